# Optimizing a Trainium2 kernel written in Bass

```python
import jax, jax.numpy as jnp
from jax import lax
import numpy as np

D_MODEL = 1024
BATCH = 32
SEQ = 2048
DEPTH = 1

HEAD_DIM = 64
N_HEADS_SWA = 8
N_KV_SWA = 2
GROUP_SWA = N_HEADS_SWA // N_KV_SWA
WINDOW = 128
N_HEADS_MOBA = 8
MOBA_BLOCK = 256
MOBA_TOPK = 3
MOBA_QCHUNK = 16
ROPE_THETA = 10000.0
N_EXPERTS = 32
TOP_K = 4
D_FF = D_MODEL
SWIGLU_LIMIT = 7.0
SWIGLU_ALPHA = 1.702
EXPERT_BLOCK = 256
LN_EPS = 1e-5
DEEPNORM_ALPHA = (2 * DEPTH) ** 0.25
DEEPNORM_BETA = (8 * DEPTH) ** -0.25

W_Q_SWA = N_HEADS_SWA * HEAD_DIM
W_KV_SWA = N_KV_SWA * HEAD_DIM
W_MOBA = N_HEADS_MOBA * HEAD_DIM
MIX_WIDTH = W_Q_SWA + W_MOBA
IN_WIDTH = W_Q_SWA + 2 * W_KV_SWA + 3 * W_MOBA
SPLITS = (W_Q_SWA, W_Q_SWA + W_KV_SWA, W_Q_SWA + 2 * W_KV_SWA,
          W_Q_SWA + 2 * W_KV_SWA + W_MOBA, W_Q_SWA + 2 * W_KV_SWA + 2 * W_MOBA)

kernel_name = 'hymba_style_swa_sink_moba_moe_deepnorm'


def layer_norm(x, g, b):
    xf = x.astype(jnp.float32)
    mu = jnp.mean(xf, axis=-1, keepdims=True)
    var = jnp.mean(jnp.square(xf - mu), axis=-1, keepdims=True)
    y = (xf - mu) * lax.rsqrt(var + LN_EPS) * g.astype(jnp.float32) + b.astype(jnp.float32)
    return y.astype(x.dtype)


def rope_tables(seq_len, dtype):
    inv_freq = 1.0 / (ROPE_THETA ** (jnp.arange(0, HEAD_DIM, 2, dtype=jnp.float32) / HEAD_DIM))
    ang = jnp.arange(seq_len, dtype=jnp.float32)[:, None] * inv_freq[None, :]
    return jnp.cos(ang)[None, :, None, :].astype(dtype), jnp.sin(ang)[None, :, None, :].astype(dtype)


def apply_rope(t, cos, sin):
    t1, t2 = jnp.split(t, 2, axis=-1)
    return jnp.concatenate([t1 * cos - t2 * sin, t2 * cos + t1 * sin], axis=-1)


def sliding_window_sink_attention(q, k, v, sinks):
    B, S = q.shape[0], q.shape[1]
    nb = S // WINDOW
    qb = q.reshape(B, nb, WINDOW, N_KV_SWA, GROUP_SWA, HEAD_DIM)

    def band(t):
        cur = t.reshape(B, nb, WINDOW, N_KV_SWA, HEAD_DIM)
        prev = jnp.pad(cur, ((0, 0), (1, 0), (0, 0), (0, 0), (0, 0)))[:, :-1]
        return jnp.concatenate([prev, cur], axis=2)

    kb, vb = band(k), band(v)
    scale = HEAD_DIM ** -0.5
    s = jnp.einsum('bnqkgd,bnckd->bkgnqc', qb, kb).astype(jnp.float32) * scale
    r = jnp.arange(WINDOW)[:, None]
    c = jnp.arange(2 * WINDOW)[None, :]
    rel = WINDOW + r - c
    band_mask = (rel >= 0) & (rel < WINDOW)
    key_pos = jnp.arange(nb)[:, None, None] * WINDOW + c[None] - WINDOW
    mask = band_mask[None] & (key_pos >= 0)
    s = jnp.where(mask, s, -jnp.inf)
    sink = sinks.astype(jnp.float32).reshape(1, N_KV_SWA, GROUP_SWA, 1, 1, 1)
    m = jnp.maximum(jnp.max(s, axis=-1, keepdims=True), sink)
    p = jnp.exp(s - m)
    denom = jnp.sum(p, axis=-1, keepdims=True) + jnp.exp(sink - m)
    p = (p / denom).astype(v.dtype)
    o = jnp.einsum('bkgnqc,bnckd->bnqkgd', p, vb)
    return o.reshape(B, S, W_Q_SWA)


def moba_attention(q, k, v):
    B, S = q.shape[0], q.shape[1]
    s_pad = -(-S // MOBA_BLOCK) * MOBA_BLOCK
    pad = ((0, 0), (0, s_pad - S), (0, 0), (0, 0))
    q, k, v = [jnp.pad(t, pad).transpose(0, 2, 1, 3) for t in (q, k, v)]
    nblk = s_pad // MOBA_BLOCK
    ksel = min(MOBA_TOPK, nblk)
    kblk = k.reshape(B, N_HEADS_MOBA, nblk, MOBA_BLOCK, HEAD_DIM)
    vblk = v.reshape(B, N_HEADS_MOBA, nblk, MOBA_BLOCK, HEAD_DIM)
    kmean = jnp.mean(kblk.astype(jnp.float32), axis=3)
    gate = jnp.einsum('bhsd,bhnd->bhsn', q.astype(jnp.float32), kmean)
    q_block = jnp.arange(s_pad) // MOBA_BLOCK
    fully_past = jnp.arange(nblk)[None, :] < q_block[:, None]
    gate = jnp.where(fully_past, gate, -jnp.inf)
    gate_vals, sel_idx = lax.top_k(gate, ksel)
    sel_valid = jnp.isfinite(gate_vals)

    nc = s_pad // MOBA_QCHUNK

    def chunked(t):
        return jnp.moveaxis(t.reshape(B, N_HEADS_MOBA, nc, MOBA_QCHUNK, *t.shape[3:]), 2, 0)

    bi = jnp.arange(B)[:, None, None, None]
    hi = jnp.arange(N_HEADS_MOBA)[None, :, None, None]
    scale = HEAD_DIM ** -0.5

    def chunk_attend(args):
        qc, ic, vc, cidx = args
        k_sel = kblk[bi, hi, ic]
        v_sel = vblk[bi, hi, ic]
        own = cidx * MOBA_QCHUNK // MOBA_BLOCK
        k_own = lax.dynamic_index_in_dim(kblk, own, axis=2, keepdims=False)
        v_own = lax.dynamic_index_in_dim(vblk, own, axis=2, keepdims=False)
        s_sel = jnp.einsum('bhqd,bhqnkd->bhqnk', qc, k_sel).astype(jnp.float32) * scale
        s_sel = jnp.where(vc[..., None], s_sel, -jnp.inf)
        s_own = jnp.einsum('bhqd,bhkd->bhqk', qc, k_own).astype(jnp.float32) * scale
        q_pos = cidx * MOBA_QCHUNK + jnp.arange(MOBA_QCHUNK)
        k_pos = own * MOBA_BLOCK + jnp.arange(MOBA_BLOCK)
        s_own = jnp.where(k_pos[None, :] <= q_pos[:, None], s_own, -jnp.inf)
        s_all = jnp.concatenate([s_sel.reshape(B, N_HEADS_MOBA, MOBA_QCHUNK, ksel * MOBA_BLOCK), s_own], axis=-1)
        p = jax.nn.softmax(s_all, axis=-1).astype(v.dtype)
        p_sel = p[..., :ksel * MOBA_BLOCK].reshape(B, N_HEADS_MOBA, MOBA_QCHUNK, ksel, MOBA_BLOCK)
        p_own = p[..., ksel * MOBA_BLOCK:]
        return (jnp.einsum('bhqnk,bhqnkd->bhqd', p_sel, v_sel)
                + jnp.einsum('bhqk,bhkd->bhqd', p_own, v_own))

    out = lax.map(chunk_attend, (chunked(q), chunked(sel_idx), chunked(sel_valid),
                                 jnp.arange(nc, dtype=jnp.int32)))
    out = jnp.moveaxis(out, 0, 2).reshape(B, N_HEADS_MOBA, s_pad, HEAD_DIM)
    return out.transpose(0, 2, 1, 3)[:, :S].reshape(B, S, W_MOBA)


def clamped_swiglu(h):
    glu = jnp.minimum(h[..., 0::2], SWIGLU_LIMIT)
    lin = jnp.clip(h[..., 1::2], -SWIGLU_LIMIT, SWIGLU_LIMIT)
    return glu * jax.nn.sigmoid(SWIGLU_ALPHA * glu) * (lin + 1.0)


def moe_ffn(h, w_router, b_router, w1, b1, w2, b2):
    T, D = h.shape
    logits = (h @ w_router + b_router).astype(jnp.float32)
    top_vals, top_idx = lax.top_k(logits, TOP_K)
    gates = jax.nn.softmax(top_vals, axis=-1)
    n_assign = T * TOP_K
    e_flat = top_idx.reshape(n_assign)
    order = jnp.argsort(e_flat)
    e_sorted = e_flat[order]
    tok_sorted = order // TOP_K
    gate_sorted = gates.reshape(n_assign)[order]
    counts = jnp.bincount(e_flat, length=N_EXPERTS)
    offsets = jnp.cumsum(counts) - counts
    padded = (counts + EXPERT_BLOCK - 1) // EXPERT_BLOCK * EXPERT_BLOCK
    padded_end = jnp.cumsum(padded)
    padded_off = padded_end - padded
    dest = padded_off[e_sorted] + (jnp.arange(n_assign) - offsets[e_sorted])
    n_blocks = -(-n_assign // EXPERT_BLOCK) + N_EXPERTS
    rows = n_blocks * EXPERT_BLOCK
    xbuf = jnp.zeros((rows, D), h.dtype).at[dest].set(h[tok_sorted])
    block_start = jnp.arange(n_blocks) * EXPERT_BLOCK
    block_expert = jnp.minimum(jnp.searchsorted(padded_end, block_start, side='right'), N_EXPERTS - 1)

    def expert_block(args):
        xb, e = args
        act = clamped_swiglu(xb @ w1[e] + b1[e])
        return act @ w2[e] + b2[e]

    ybuf = lax.map(expert_block, (xbuf.reshape(n_blocks, EXPERT_BLOCK, D), block_expert)).reshape(rows, D)
    contrib = gate_sorted.astype(h.dtype)[:, None] * ybuf[dest]
    return jnp.zeros((T, D), h.dtype).at[tok_sorted].add(contrib)


def setup_inputs(seed: int = 0) -> dict:
    key = jax.random.key(seed)
    ks = jax.random.split(key, 16)
    f32 = jnp.float32

    def nrm(k, shape, scale):
        return scale * jax.random.normal(k, shape, f32)

    x = jax.random.normal(ks[0], (BATCH, SEQ, D_MODEL), f32)
    col_scale = jnp.concatenate([
        jnp.ones((W_Q_SWA + W_KV_SWA,), f32), jnp.full((W_KV_SWA,), DEEPNORM_BETA, f32),
        jnp.ones((2 * W_MOBA,), f32), jnp.full((W_MOBA,), DEEPNORM_BETA, f32)])
    w_in = nrm(ks[1], (DEPTH, D_MODEL, IN_WIDTH), D_MODEL ** -0.5) * col_scale
    b_in = nrm(ks[2], (DEPTH, IN_WIDTH), 0.02)
    sinks = nrm(ks[3], (DEPTH, N_HEADS_SWA), 1.0)
    w_out = nrm(ks[4], (DEPTH, MIX_WIDTH, D_MODEL), MIX_WIDTH ** -0.5 * DEEPNORM_BETA)
    b_out = nrm(ks[5], (DEPTH, D_MODEL), 0.02)
    ln1_g = 1.0 + nrm(ks[6], (DEPTH, D_MODEL), 0.02)
    ln1_b = nrm(ks[7], (DEPTH, D_MODEL), 0.02)
    w_router = nrm(ks[8], (DEPTH, D_MODEL, N_EXPERTS), D_MODEL ** -0.5)
    b_router = nrm(ks[9], (DEPTH, N_EXPERTS), 0.01)
    w1 = nrm(ks[10], (DEPTH, N_EXPERTS, D_MODEL, 2 * D_FF), D_MODEL ** -0.5 * DEEPNORM_BETA)
    b1 = nrm(ks[11], (DEPTH, N_EXPERTS, 2 * D_FF), 0.02)
    w2 = nrm(ks[12], (DEPTH, N_EXPERTS, D_FF, D_MODEL), D_FF ** -0.5 * DEEPNORM_BETA)
    b2 = nrm(ks[13], (DEPTH, N_EXPERTS, D_MODEL), 0.02)
    ln2_g = 1.0 + nrm(ks[14], (DEPTH, D_MODEL), 0.02)
    ln2_b = nrm(ks[15], (DEPTH, D_MODEL), 0.02)
    return {'x': x, 'w_in': w_in, 'b_in': b_in, 'sinks': sinks, 'w_out': w_out, 'b_out': b_out,
            'ln1_g': ln1_g, 'ln1_b': ln1_b, 'w_router': w_router, 'b_router': b_router,
            'w1': w1, 'b1': b1, 'w2': w2, 'b2': b2, 'ln2_g': ln2_g, 'ln2_b': ln2_b}


def reference(x, w_in, b_in, sinks, w_out, b_out, ln1_g, ln1_b, w_router, b_router,
              w1, b1, w2, b2, ln2_g, ln2_b):
    B, S, D = x.shape
    cos, sin = rope_tables(S, x.dtype)
    for l in range(DEPTH):
        proj = x @ w_in[l] + b_in[l]
        q_a, k_a, v_a, q_b, k_b, v_b = jnp.split(proj, SPLITS, axis=-1)
        q_a = apply_rope(q_a.reshape(B, S, N_HEADS_SWA, HEAD_DIM), cos, sin)
        k_a = apply_rope(k_a.reshape(B, S, N_KV_SWA, HEAD_DIM), cos, sin)
        v_a = v_a.reshape(B, S, N_KV_SWA, HEAD_DIM)
        q_b = apply_rope(q_b.reshape(B, S, N_HEADS_MOBA, HEAD_DIM), cos, sin)
        k_b = apply_rope(k_b.reshape(B, S, N_HEADS_MOBA, HEAD_DIM), cos, sin)
        v_b = v_b.reshape(B, S, N_HEADS_MOBA, HEAD_DIM)
        o_a = sliding_window_sink_attention(q_a, k_a, v_a, sinks[l])
        o_b = moba_attention(q_b, k_b, v_b)
        mix = jnp.concatenate([o_a, o_b], axis=-1) @ w_out[l] + b_out[l]
        x = layer_norm(DEEPNORM_ALPHA * x + mix, ln1_g[l], ln1_b[l])
        moe = moe_ffn(x.reshape(B * S, D), w_router[l], b_router[l], w1[l], b1[l], w2[l], b2[l]).reshape(B, S, D)
        x = layer_norm(DEEPNORM_ALPHA * x + moe, ln2_g[l], ln2_b[l])
    return x
```

```python
import numpy as np
from contextlib import ExitStack
import concourse.bass as bass
import concourse.mybir as mybir
from concourse.bass_utils import run_bass_kernel_spmd

F32 = mybir.dt.float32
BF16 = mybir.dt.bfloat16
U32 = mybir.dt.uint32
I32 = mybir.dt.int32
AF = mybir.ActivationFunctionType
ALU = mybir.AluOpType
AX = mybir.AxisListType

D = 1024
SEQ = 2048
NE = 32
ALPHA = float(2.0 ** 0.25)
EPS = 1e-5
NEG = -30000.0
N_CORES = 8


class Res:
    __slots__ = ("name", "w", "r", "multi", "excl")

    def __init__(self, name, multi=False, excl=False):
        self.excl = excl
        self.name = name
        self.w = {}
        self.r = {}
        self.multi = multi


class Buf:
    __slots__ = ("t", "r")

    def __init__(self, t, name):
        self.t = t
        self.r = Res(name)


class Sched:
    def __init__(self, nc, stack):
        self.nc = nc
        self.stack = stack
        self.eng = {"pe": nc.tensor, "act": nc.scalar, "dve": nc.vector, "pool": nc.gpsimd, "sp": nc.sync}
        self.sem = {}
        self.cnt = {}
        self.waited = {k: {} for k in self.eng}
        for k in self.eng:
            self.sem[k] = stack.enter_context(nc.semaphore("s_" + k))
            self.cnt[k] = 0
        self.dsem = {}
        self.dcnt = {}
        self.nwait = 0
        self.nins = 0

    def _wait(self, e, deps):
        for key, (sh, val) in deps.items():
            if self.waited[e].get(key, 0) >= val:
                continue
            self.eng[e].wait_ge(sh, val)
            self.waited[e][key] = val
            self.nwait += 1

    def _deps(self, e, reads, writes, pe_order=False):
        deps = {}

        def add(key, sh, val):
            if key == "pe" and e == "pe" and not pe_order:
                return
            if key not in deps or deps[key][1] < val:
                deps[key] = (sh, val)

        for r in reads:
            for key, (sh, val) in r.w.items():
                add(key, sh, val)
            if r.excl:
                for key, (sh, val) in r.r.items():
                    if key != e:
                        add(key, sh, val)
        for w in writes:
            if not w.multi:
                for key, (sh, val) in w.w.items():
                    add(key, sh, val)
            for key, (sh, val) in w.r.items():
                add(key, sh, val)
        return deps

    def _record(self, key, sh, val, reads, writes):
        for r in reads:
            r.r[key] = (sh, val)
        for w in writes:
            if w.multi:
                w.w[key] = (sh, val)
            else:
                w.w = {key: (sh, val)}
                w.r = {}

    def op(self, e, fn, reads=(), writes=(), pe_order=False):
        deps = self._deps(e, reads, writes, pe_order)
        self._wait(e, deps)
        ins = fn(self.eng[e])
        self.cnt[e] += 1
        self.nins += 1
        ins.then_inc(self.sem[e], 1)
        self._record(e, self.sem[e], self.cnt[e], reads, writes)
        return ins

    def dma(self, q, fn, dname, reads=(), writes=()):
        if dname not in self.dsem:
            self.dsem[dname] = self.stack.enter_context(self.nc.semaphore("d_" + dname))
            self.dcnt[dname] = 0
        key = "d_" + dname
        deps = self._deps(q, reads, writes)
        deps.pop(key, None)
        self._wait(q, deps)
        ins = fn(self.eng[q])
        self.dcnt[dname] += 16
        self.nins += 1
        ins.then_inc(self.dsem[dname], 16)
        self._record(key, self.dsem[dname], self.dcnt[dname], reads, writes)
        return ins

    def barrier(self):
        allev = {}
        for k in self.eng:
            if self.cnt[k] > 0:
                allev[k] = (self.sem[k], self.cnt[k])
        for dn in self.dsem:
            if self.dcnt[dn] > 0:
                allev["d_" + dn] = (self.dsem[dn], self.dcnt[dn])
        for e in self.eng:
            deps = {k: v for k, v in allev.items() if k != e}
            self._wait(e, deps)

    def finish(self, e, resources):
        deps = {}
        for r in resources:
            for key, (sh, val) in list(r.w.items()) + list(r.r.items()):
                if key not in deps or deps[key][1] < val:
                    deps[key] = (sh, val)
        self._wait(e, deps)


def build(NSEQ, C, dbg=False):
    NT = NSEQ * 16
    NTOK = NSEQ * SEQ
    NS = C // 128
    NROW = NE * C + 1024
    nc = bass.Bass("TRN2", target_bir_lowering=False)

    def din(name, shape, dt=F32):
        return nc.dram_tensor(name, shape, dt, kind="ExternalInput").ap()

    x_d = din("x", [NTOK, D])
    w_in_d = din("w_in", [D, 2304])
    b_in_d = din("b_in", [1, 2304])
    sinks_d = din("sinks", [1, 8])
    w_out_d = din("w_out", [D, D])
    b_out_d = din("b_out", [1, D])
    ln1g_d = din("ln1_g", [1, D])
    ln1b_d = din("ln1_b", [1, D])
    w_r_d = din("w_router", [128, 8, NE])
    b_r_d = din("b_router", [1, NE])
    w1g_d = din("w1g", [NE, D, D])
    w1l_d = din("w1l", [NE, D, D])
    b1g_d = din("b1g", [128, NE, 8])
    b1l_d = din("b1l", [128, NE, 8])
    w2_d = din("w2", [NE, D, D])
    b2_d = din("b2", [NE, D])
    ln2g_d = din("ln2_g", [1, D])
    ln2b_d = din("ln2_b", [1, D])
    ropeC_d = din("ropeC", [128, 16, 64])
    ropeS_d = din("ropeS", [128, 16, 64])
    out_d = nc.dram_tensor("out", [NTOK, D], F32, kind="ExternalOutput").ap()
    x1buf = nc.dram_tensor("x1buf", [NTOK, D], F32, kind="ExternalOutput" if dbg else "Internal").ap()
    xbuf = nc.dram_tensor("xbuf", [NROW, D], BF16, kind="Internal").ap()
    ybuf = nc.dram_tensor("ybuf", [NROW, D], F32, kind="Internal").ap()
    r_x1buf, r_xbuf, r_ybuf, r_out = Res("x1buf", True), Res("xbuf", True), Res("ybuf", True), Res("out", True)

    with ExitStack() as st0:
        S = Sched(nc, st0)
        uid = [0]

        def sb(st, shape, dt=F32, name="t"):
            uid[0] += 1
            nm = "%s_%d" % (name, uid[0])
            return Buf(st.enter_context(nc.sbuf_tensor(nm, shape, dt)), nm)

        pb = []
        for i in range(8):
            if i in (5, 6):
                t = st0.enter_context(nc.psum_tensor("pb%d" % i, [128, 1024], BF16))
            else:
                t = st0.enter_context(nc.psum_tensor("pb%d" % i, [128, 512], F32))
            pb.append(Buf(t, "pb%d" % i))
            pb[-1].r.excl = True
        bigc = [0]

        def nb():
            b = pb[bigc[0] % 3]
            bigc[0] += 1
            return b

        oc = [0]

        def nob():
            b = pb[3 + oc[0] % 2]
            oc[0] += 1
            return b

        gates = sb(st0, [128, NT, 4], F32, "gates")
        dest = sb(st0, [128, NT, 4], U32, "dest")
        ident_b = sb(st0, [128, 128], BF16, "identb")
        ident_f = sb(st0, [128, 128], F32, "identf")
        ones2 = sb(st0, [2, 128], BF16, "ones2")
        m10 = sb(st0, [2, 1], F32, "m10")

        S.op("pool", lambda e: e.memset(ident_f.t[:], 0.0), writes=[ident_f.r])
        S.op("pool", lambda e: e.affine_select(out=ident_f.t[:], in_=ident_f.t[:], pattern=[[-1, 128]], compare_op=ALU.not_equal, fill=1.0, base=0, channel_multiplier=1), reads=[ident_f.r], writes=[ident_f.r])
        S.op("dve", lambda e: e.tensor_copy(out=ident_b.t[:], in_=ident_f.t[:]), reads=[ident_f.r], writes=[ident_b.r])
        S.op("dve", lambda e: e.memset(ones2.t[:], 1.0), writes=[ones2.r])
        S.op("dve", lambda e: e.memset(m10.t[:], 0.0), writes=[m10.r])
        S.op("dve", lambda e: e.memset(m10.t[0:1, :], 1.0), reads=[m10.r], writes=[m10.r])

        def make_hilo(st, dst, src_row, n, tag):
            stg = sb(st, [2, n], F32, "hl_s" + tag)
            hb = sb(st, [2, n], BF16, "hl_b" + tag)
            hf = sb(st, [2, n], F32, "hl_f" + tag)
            lo = sb(st, [2, n], F32, "hl_l" + tag)
            S.dma("sp", lambda e: e.dma_start(out=stg.t[:], in_=src_row.partition_broadcast(2)), "hl" + tag, writes=[stg.r])
            hilo_compute(dst, stg, hb, hf, lo, n)

        def hilo_compute(dst, stg, hb, hf, lo, n):
            S.op("dve", lambda e: e.tensor_copy(out=hb.t[:, 0:n], in_=stg.t[:, 0:n]), reads=[stg.r], writes=[hb.r])
            S.op("dve", lambda e: e.tensor_copy(out=hf.t[:, 0:n], in_=hb.t[:, 0:n]), reads=[hb.r], writes=[hf.r])
            S.op("dve", lambda e: e.tensor_tensor(out=lo.t[:, 0:n], in0=stg.t[:, 0:n], in1=hf.t[:, 0:n], op=ALU.subtract), reads=[stg.r, hf.r], writes=[lo.r])
            S.op("dve", lambda e: e.tensor_tensor(out=hf.t[:, 0:n], in0=hf.t[:, 0:n], in1=lo.t[:, 0:n], op=ALU.subtract), reads=[hf.r, lo.r], writes=[hf.r])
            S.op("dve", lambda e: e.scalar_tensor_tensor(out=dst.t[:, 0:n], in0=hf.t[:, 0:n], scalar=m10.t[:, 0:1], in1=lo.t[:, 0:n], op0=ALU.mult, op1=ALU.add), reads=[hf.r, lo.r, m10.r], writes=[dst.r])

        def layer_norm(st_bufs, yln, gB, bB, outb, eng_gb="pool"):
            stats, mv, lnv, rstd, nmr, xn = st_bufs
            S.op("dve", lambda e: e.bn_stats(out=stats.t[:, 0, :], in_=yln.t[:, 0:512]), reads=[yln.r], writes=[stats.r])
            S.op("dve", lambda e: e.bn_stats(out=stats.t[:, 1, :], in_=yln.t[:, 512:1024]), reads=[yln.r], writes=[stats.r])
            S.op("dve", lambda e: e.bn_aggr(out=mv.t[:], in_=stats.t[:].rearrange("p a b -> p (a b)")), reads=[stats.r], writes=[mv.r])
            S.op("act", lambda e: e.activation(out=lnv.t[:], in_=mv.t[:, 1:2], func=AF.Ln, bias=EPS, scale=1.0), reads=[mv.r], writes=[lnv.r])
            S.op("act", lambda e: e.activation(out=rstd.t[:], in_=lnv.t[:], func=AF.Exp, scale=-0.5), reads=[lnv.r], writes=[rstd.r])
            S.op("dve", lambda e: e.tensor_scalar(out=nmr.t[:], in0=mv.t[:, 0:1], scalar1=-1.0, scalar2=rstd.t[:, 0:1], op0=ALU.mult, op1=ALU.mult), reads=[mv.r, rstd.r], writes=[nmr.r])
            S.op("act", lambda e: e.activation(out=xn.t[:], in_=yln.t[:], func=AF.Identity, scale=rstd.t[:, 0:1], bias=nmr.t[:, 0:1]), reads=[yln.r, rstd.r, nmr.r], writes=[xn.r])
            S.op(eng_gb, lambda e: e.tensor_tensor(out=xn.t[:], in0=xn.t[:], in1=gB.t[:], op=ALU.mult), reads=[xn.r, gB.r], writes=[xn.r])
            S.op(eng_gb, lambda e: e.tensor_tensor(out=outb.t[:], in0=xn.t[:], in1=bB.t[:], op=ALU.add), reads=[xn.r, bB.r], writes=[outb.r])

        with ExitStack() as st:
            w_in = sb(st, [128, 8, 2304], BF16, "w_in")
            w_out = sb(st, [128, 8, D], BF16, "w_out")
            w_r = sb(st, [128, 8, NE], F32, "w_r")
            bin2 = sb(st, [2, 2304], BF16, "bin2")
            bout2 = sb(st, [2, D], BF16, "bout2")
            ropeC = sb(st, [128, 16, 64], F32, "ropeC")
            ropeS = sb(st, [128, 16, 64], F32, "ropeS")
            ln1g = sb(st, [128, D], F32, "ln1g")
            ln1b = sb(st, [128, D], F32, "ln1b")
            expsink = sb(st, [128, 8], F32, "expsink")
            brB = sb(st, [128, NE], F32, "brB")
            carryD = sb(st, [128, NE], F32, "carryD")
            carryI = sb(st, [128, NE], I32, "carryI")
            tri4 = sb(st, [128, 512], BF16, "tri4")
            atri4 = sb(st, [128, 512], BF16, "atri4")
            Lst = sb(st, [128, 128], BF16, "Lst")
            onesm = sb(st, [128, 128], BF16, "onesm")
            elig = sb(st, [128, 4, 64], F32, "elig")
            stt = ExitStack()
            trif = sb(stt, [128, 512], F32, "trif")

            w_in_v = w_in_d.rearrange("(k p) c -> p k c", p=128)
            w_out_v = w_out_d.rearrange("(k p) c -> p k c", p=128)
            for k in range(8):
                for h2 in range(2):
                    S.dma("pool", lambda e: e.dma_start(out=w_in.t[:, k, h2 * 1152:(h2 + 1) * 1152], in_=w_in_v[:, k, h2 * 1152:(h2 + 1) * 1152]), "w_in", writes=[w_in.r])
                S.dma("pool", lambda e: e.dma_start(out=w_out.t[:, k, :], in_=w_out_v[:, k, :]), "w_out", writes=[w_out.r])
            S.dma("sp", lambda e: e.dma_start(out=w_r.t[:], in_=w_r_d), "w_r", writes=[w_r.r])
            S.dma("sp", lambda e: e.dma_start(out=ropeC.t[:], in_=ropeC_d), "ropeC", writes=[ropeC.r])
            S.dma("sp", lambda e: e.dma_start(out=ropeS.t[:], in_=ropeS_d), "ropeS", writes=[ropeS.r])
            S.dma("sp", lambda e: e.dma_start(out=ln1g.t[:], in_=ln1g_d[0].partition_broadcast(128)), "ln1g", writes=[ln1g.r])
            S.dma("sp", lambda e: e.dma_start(out=ln1b.t[:], in_=ln1b_d[0].partition_broadcast(128)), "ln1b", writes=[ln1b.r])
            S.dma("sp", lambda e: e.dma_start(out=expsink.t[:], in_=sinks_d[0].partition_broadcast(128)), "sinks", writes=[expsink.r])
            S.dma("sp", lambda e: e.dma_start(out=brB.t[:], in_=b_r_d[0].partition_broadcast(128)), "brB", writes=[brB.r])
            S.op("act", lambda e: e.activation(out=expsink.t[:], in_=expsink.t[:], func=AF.Exp), reads=[expsink.r], writes=[expsink.r])
            make_hilo(stt, bin2, b_in_d[0], 2304, "bin")
            make_hilo(stt, bout2, b_out_d[0], D, "bout")
            S.op("pool", lambda e: e.iota(carryI.t[:], pattern=[[C, NE]], base=0, channel_multiplier=0), writes=[carryI.r])
            S.op("dve", lambda e: e.tensor_copy(out=carryD.t[:], in_=carryI.t[:]), reads=[carryI.r], writes=[carryD.r])
            S.op("pool", lambda e: e.memset(trif.t[:], 0.0), writes=[trif.r])
            S.op("pool", lambda e: e.affine_select(out=trif.t[:], in_=trif.t[:], pattern=[[0, 4], [1, 128]], compare_op=ALU.is_ge, fill=NEG, base=0, channel_multiplier=-1), reads=[trif.r], writes=[trif.r])
            S.op("dve", lambda e: e.tensor_copy(out=tri4.t[:], in_=trif.t[:]), reads=[trif.r], writes=[tri4.r])
            S.op("pool", lambda e: e.memset(trif.t[:], 0.0), reads=[trif.r], writes=[trif.r])
            S.op("pool", lambda e: e.affine_select(out=trif.t[:], in_=trif.t[:], pattern=[[0, 4], [-1, 128]], compare_op=ALU.is_gt, fill=NEG, base=0, channel_multiplier=1), reads=[trif.r], writes=[trif.r])
            S.op("dve", lambda e: e.tensor_copy(out=atri4.t[:], in_=trif.t[:]), reads=[trif.r], writes=[atri4.r])
            S.op("pool", lambda e: e.memset(trif.t[:, 0:128], 1.0), reads=[trif.r], writes=[trif.r])
            S.op("pool", lambda e: e.affine_select(out=trif.t[:, 0:128], in_=trif.t[:, 0:128], pattern=[[1, 128]], compare_op=ALU.is_gt, fill=0.0, base=0, channel_multiplier=-1), reads=[trif.r], writes=[trif.r])
            S.op("dve", lambda e: e.tensor_copy(out=Lst.t[:], in_=trif.t[:, 0:128]), reads=[trif.r], writes=[Lst.r])
            S.op("dve", lambda e: e.memset(onesm.t[:], 1.0), writes=[onesm.r])
            S.op("dve", lambda e: e.memset(elig.t[:], 0.0), writes=[elig.r])
            for j in range(4, 8):
                S.op("dve", lambda e: e.memset(elig.t[:, j - 4, :].rearrange("p (h b) -> p h b", h=8)[:, :, j:8], -1e30), reads=[elig.r], writes=[elig.r])

            S.barrier()
            stt.close()
            kT_a = sb(st, [128, SEQ], BF16, "kT_a")
            kT_b = sb(st, [128, 4, SEQ], BF16, "kT_b")
            Va = sb(st, [128, 16, 2, 65], BF16, "Va")
            Vb = sb(st, [128, 16, 8, 65], BF16, "Vb")
            r_kv = [Res("kv%d" % t) for t in range(16)]
            S.op("pool", lambda e: e.memset(Va.t[:], 1.0), writes=r_kv)
            S.op("pool", lambda e: e.memset(Vb.t[:], 1.0), writes=r_kv)
            kms = sb(st, [128, 4, 8], F32, "kms")
            kmT = sb(st, [128, 4, 8], BF16, "kmT")
            S.op("dve", lambda e: e.memset(kms.t[:], 0.0), writes=[kms.r])
            S.op("dve", lambda e: e.memset(kmT.t[:], 0.0), writes=[kmT.r])
            xbs = [sb(st, [128, D], BF16, "xb") for _ in range(2)]
            xTs = [sb(st, [128, D], BF16, "xT") for _ in range(2)]
            m1s = [sb(st, [128, 512], F32, "m1") for _ in range(2)]
            m2s = [sb(st, [128, 512], F32, "m2") for _ in range(2)]
            rqa = [sb(st, [128, 512], BF16, "rqa") for _ in range(2)]
            rqb = [sb(st, [128, 512], BF16, "rqb") for _ in range(2)]
            rkb = [sb(st, [128, 512], BF16, "rkb") for _ in range(2)]
            rka = [sb(st, [128, 128], BF16, "rka") for _ in range(2)]
            qTa = [sb(st, [128, 4, 512], BF16, "qTa") for _ in range(1)]
            qTb = [sb(st, [128, 4, 512], BF16, "qTb") for _ in range(1)]
            sel = [sb(st, [128, 4, 64], F32, "sel") for _ in range(2)]
            gm = sb(st, [128, 64], F32, "gm")
            top = sb(st, [128, 8, 8], F32, "top")
            pts = [sb(st, [128, 512], BF16, "pt") for _ in range(4)]
            ptc = [0]
            den = sb(st, [128, 4], F32, "den")
            rden = sb(st, [128, 4], F32, "rden")
            accs = [sb(st, [128, 4, 65], F32, "acc") for _ in range(2)]
            o_t = [sb(st, [128, 4, D], BF16, "o_t") for _ in range(1)]
            oT = sb(st, [128, D], BF16, "oT")
            xres = [sb(st, [128, D], F32, "xres") for _ in range(1)]
            yln = sb(st, [128, D], F32, "yln")
            lnb = (sb(st, [128, 2, 6], F32, "stats"), sb(st, [128, 2], F32, "mv"), sb(st, [128, 1], F32, "lnv"),
                   sb(st, [128, 1], F32, "rstd"), sb(st, [128, 1], F32, "nmr"), sb(st, [128, D], F32, "xn"))
            x1s = [sb(st, [128, D], F32, "x1") for _ in range(1)]
            x1bs = [sb(st, [128, D], BF16, "x1b") for _ in range(2)]
            x1T = sb(st, [128, D], F32, "x1T")
            lg = sb(st, [128, NE], F32, "lg")
            top8 = sb(st, [128, 8], F32, "top8")
            ntop = sb(st, [128, 1], F32, "ntop")
            gex = sb(st, [128, 4], F32, "gex")
            gsum = sb(st, [128, 1], F32, "gsum")
            Mb = sb(st, [128, NE], BF16, "Mb")
            posD = sb(st, [128, NE], F32, "posD")
            junk = sb(st, [128, NE], F32, "junk")
            destf = sb(st, [128, 4], F32, "destf")

            def npt():
                b = pts[ptc[0] % 4]
                ptc[0] += 1
                return b

            def rope(src_bank, H, t, m1, m2, out_view_fn):
                n = H * 64
                src = src_bank.t[:, 0:n].rearrange("p (h c) -> p h c", h=H)
                Cb = ropeC.t[:, t, :].unsqueeze(1).broadcast_to([128, H, 64])
                Sb1 = ropeS.t[:, t, 0:32].unsqueeze(1).broadcast_to([128, H, 32])
                Sb2 = ropeS.t[:, t, 32:64].unsqueeze(1).broadcast_to([128, H, 32])
                m1v = m1.t[:, 0:n].rearrange("p (h c) -> p h c", h=H)
                m2v = m2.t[:, 0:n].rearrange("p (h c) -> p h c", h=H)
                S.op("dve", lambda e: e.tensor_tensor(out=m1v, in0=src, in1=Cb, op=ALU.mult), reads=[src_bank.r, ropeC.r], writes=[m1.r])
                S.op("dve", lambda e: e.tensor_tensor(out=m2v[:, :, 0:32], in0=src[:, :, 32:64], in1=Sb1, op=ALU.mult), reads=[src_bank.r, ropeS.r], writes=[m2.r])
                S.op("dve", lambda e: e.tensor_tensor(out=m2v[:, :, 32:64], in0=src[:, :, 0:32], in1=Sb2, op=ALU.mult), reads=[src_bank.r, ropeS.r], writes=[m2.r])
                return m1v, m2v

            for s in range(NSEQ):
                for c in range(4):
                    cb = (s * 4 + c) % 2
                    qa_c, qb_c, sel_c, o_c = qTa[0], qTb[0], sel[cb], o_t[0]
                    for u in range(4):
                        t = 4 * c + u
                        T = s * 16 + t
                        pp = T % 2
                        xb, xT = xbs[pp], xTs[pp]
                        S.dma("pool", lambda e: e.dma_start(out=xb.t[:], in_=x_d[T * 128:(T + 1) * 128, :]), "xb%d" % pp, writes=[xb.r])
                        for k in range(8):
                            S.op("pe", lambda e: e.transpose(out=pb[5].t[:, k * 128:(k + 1) * 128], in_=xb.t[:, k * 128:(k + 1) * 128], identity=ident_b.t[:]), reads=[xb.r, ident_b.r], writes=[pb[5].r])
                        S.op("act", lambda e: e.activation(out=xT.t[:], in_=pb[5].t[:], func=AF.Copy), reads=[pb[5].r], writes=[xT.r])

                        def proj(col0, n):
                            bank = nb()
                            for k in range(8):
                                S.op("pe", lambda e: e.matmul(bank.t[:, 0:n], lhsT=xT.t[:, k * 128:(k + 1) * 128], rhs=w_in.t[:, k, col0:col0 + n], start=(k == 0), stop=False), reads=[xT.r, w_in.r], writes=[bank.r])
                            S.op("pe", lambda e: e.matmul(bank.t[:, 0:n], lhsT=ones2.t[:, :], rhs=bin2.t[:, col0:col0 + n], start=False, stop=True), reads=[ones2.r, bin2.r], writes=[bank.r])
                            return bank

                        bank = proj(0, 512)
                        m1v, m2v = rope(bank, 8, t, m1s[0], m2s[0], None)
                        ov = rqa[pp].t[:].rearrange("p (i two c) -> p two i c", i=4, two=2)
                        S.op("pool", lambda e: e.tensor_tensor(out=ov, in0=m1s[0].t[:].rearrange("p (two i c) -> p two i c", two=2, i=4), in1=m2s[0].t[:].rearrange("p (two i c) -> p two i c", two=2, i=4), op=ALU.add), reads=[m1s[0].r, m2s[0].r], writes=[rqa[pp].r])
                        bank = proj(768, 512)
                        rope(bank, 8, t, m1s[1], m2s[1], None)
                        S.op("pool", lambda e: e.tensor_tensor(out=rqb[pp].t[:], in0=m1s[1].t[:], in1=m2s[1].t[:], op=ALU.add), reads=[m1s[1].r, m2s[1].r], writes=[rqb[pp].r])
                        for i in range(4):
                            S.op("pe", lambda e: e.transpose(out=pb[6].t[:, i * 128:(i + 1) * 128], in_=rqa[pp].t[:, i * 128:(i + 1) * 128], identity=ident_b.t[:]), reads=[rqa[pp].r, ident_b.r], writes=[pb[6].r])
                        for i in range(4):
                            S.op("pe", lambda e: e.transpose(out=pb[6].t[:, 512 + i * 128:512 + (i + 1) * 128], in_=rqb[pp].t[:, i * 128:(i + 1) * 128], identity=ident_b.t[:]), reads=[rqb[pp].r, ident_b.r], writes=[pb[6].r])
                        S.op("act", lambda e: e.activation(out=qa_c.t[:, u, :], in_=pb[6].t[:, 0:512], func=AF.Copy), reads=[pb[6].r], writes=[qa_c.r])
                        S.op("act", lambda e: e.activation(out=qb_c.t[:, :, u * 128:(u + 1) * 128], in_=pb[6].t[:, 512:1024].rearrange("p (i q) -> p i q", i=4), func=AF.Copy), reads=[pb[6].r], writes=[qb_c.r])
                        bank = proj(1280, 512)
                        rope(bank, 8, t, m1s[0], m2s[0], None)
                        S.op("pool", lambda e: e.tensor_tensor(out=rkb[pp].t[:], in0=m1s[0].t[:], in1=m2s[0].t[:], op=ALU.add), reads=[m1s[0].r, m2s[0].r], writes=[rkb[pp].r])
                        bank = proj(512, 256)
                        rope(bank, 2, t, m1s[1], m2s[1], None)
                        S.op("pool", lambda e: e.tensor_tensor(out=rka[pp].t[:], in0=m1s[1].t[:, 0:128], in1=m2s[1].t[:, 0:128], op=ALU.add), reads=[m1s[1].r, m2s[1].r], writes=[rka[pp].r])
                        S.op("act", lambda e: e.activation(out=Va.t[:, t, :, 0:64], in_=bank.t[:, 128:256].rearrange("p (h c) -> p h c", h=2), func=AF.Copy), reads=[bank.r], writes=[r_kv[t]])
                        for i in range(4):
                            S.op("pe", lambda e: e.transpose(out=pb[5].t[:, i * 128:(i + 1) * 128], in_=rkb[pp].t[:, i * 128:(i + 1) * 128], identity=ident_b.t[:]), reads=[rkb[pp].r, ident_b.r], writes=[pb[5].r])
                        S.op("pe", lambda e: e.transpose(out=pb[5].t[:, 512:640], in_=rka[pp].t[:, 0:128], identity=ident_b.t[:]), reads=[rka[pp].r, ident_b.r], writes=[pb[5].r])
                        S.op("act", lambda e: e.activation(out=kT_b.t[:, :, t * 128:(t + 1) * 128], in_=pb[5].t[:, 0:512].rearrange("p (i q) -> p i q", i=4), func=AF.Copy), reads=[pb[5].r], writes=[r_kv[t]])
                        S.op("act", lambda e: e.activation(out=kT_a.t[:, t * 128:(t + 1) * 128], in_=pb[5].t[:, 512:640], func=AF.Copy), reads=[pb[5].r], writes=[r_kv[t]])
                        bank = proj(1792, 512)
                        S.op("act", lambda e: e.activation(out=Vb.t[:, t, :, 0:64], in_=bank.t[:, 0:512].rearrange("p (h c) -> p h c", h=8), func=AF.Copy), reads=[bank.r], writes=[r_kv[t]])

                    kvc = [r_kv[4 * c + u] for u in range(4)]
                    S.op("dve", lambda e: e.tensor_reduce(out=kms.t[:, :, 2 * c:2 * c + 2], in_=kT_b.t[:, :, c * 512:(c + 1) * 512].rearrange("p i (b k) -> p i b k", b=2), axis=AX.X, op=ALU.add), reads=kvc, writes=[kms.r])
                    S.op("act", lambda e: e.activation(out=kmT.t[:, :, 2 * c:2 * c + 2], in_=kms.t[:, :, 2 * c:2 * c + 2], func=AF.Copy, scale=1.0 / 256.0), reads=[kms.r], writes=[kmT.r])
                    if c >= 2:
                        for u in range(4):
                            for h in range(8):
                                i, ph = h // 2, (h % 2) * 64
                                S.op("pe", lambda e: e.matmul(pb[7].t[:, (u * 8 + h) * 8:(u * 8 + h) * 8 + 8], lhsT=qb_c.t[ph:ph + 64, i, u * 128:(u + 1) * 128], rhs=kmT.t[ph:ph + 64, i, 0:8], start=True, stop=True), reads=[qb_c.r, kmT.r], writes=[pb[7].r], pe_order=True)
                        for u in range(4):
                            j = 2 * c + u // 2
                            S.op("dve", lambda e: e.tensor_tensor(out=gm.t[:], in0=pb[7].t[:, u * 64:(u + 1) * 64], in1=elig.t[:, j - 4, :], op=ALU.add), reads=[pb[7].r, elig.r], writes=[gm.r])
                            for h in range(8):
                                S.op("dve", lambda e: e.max(out=top.t[:, h, :], in_=gm.t[:, h * 8:(h + 1) * 8]), reads=[gm.r], writes=[top.r])
                            S.op("dve", lambda e: e.tensor_tensor(out=sel_c.t[:, u, :].rearrange("p (h b) -> p h b", h=8), in0=gm.t[:].rearrange("p (h b) -> p h b", h=8), in1=top.t[:, :, 2:3].broadcast_to([128, 8, 8]), op=ALU.is_ge), reads=[gm.r, top.r], writes=[sel_c.r])

                    for u in range(4):
                        qt = 4 * c + u
                        for g in range(2):
                            ph = g * 64
                            pt_prev = None
                            if qt >= 1:
                                bank = nb()
                                S.op("pe", lambda e: e.matmul(bank.t[:, 0:512], lhsT=kT_a.t[ph:ph + 64, (qt - 1) * 128:qt * 128], rhs=qa_c.t[ph:ph + 64, u, :], start=True, stop=False), reads=[r_kv[qt - 1], qa_c.r], writes=[bank.r])
                                S.op("pe", lambda e: e.matmul(bank.t[:, 0:512], lhsT=ident_b.t[:], rhs=atri4.t[:], start=False, stop=True), reads=[ident_b.r, atri4.r], writes=[bank.r])
                                pt_prev = npt()
                                S.op("act", lambda e: e.activation(out=pt_prev.t[:], in_=bank.t[:, 0:512], func=AF.Exp, scale=0.125), reads=[bank.r], writes=[pt_prev.r])
                            bank = nb()
                            S.op("pe", lambda e: e.matmul(bank.t[:, 0:512], lhsT=kT_a.t[ph:ph + 64, qt * 128:(qt + 1) * 128], rhs=qa_c.t[ph:ph + 64, u, :], start=True, stop=False), reads=[r_kv[qt], qa_c.r], writes=[bank.r])
                            S.op("pe", lambda e: e.matmul(bank.t[:, 0:512], lhsT=ident_b.t[:], rhs=tri4.t[:], start=False, stop=True), reads=[ident_b.r, tri4.r], writes=[bank.r])
                            pt_cur = npt()
                            S.op("act", lambda e: e.activation(out=pt_cur.t[:], in_=bank.t[:, 0:512], func=AF.Exp, scale=0.125), reads=[bank.r], writes=[pt_cur.r])
                            ob = nob()
                            for hh in range(4):
                                reg = ob.t[:, hh * 65:(hh + 1) * 65]
                                if pt_prev is not None:
                                    S.op("pe", lambda e: e.matmul(reg, lhsT=pt_prev.t[:, hh * 128:(hh + 1) * 128], rhs=Va.t[:, qt - 1, g, :], start=True, stop=False), reads=[pt_prev.r, r_kv[qt - 1]], writes=[ob.r])
                                S.op("pe", lambda e: e.matmul(reg, lhsT=pt_cur.t[:, hh * 128:(hh + 1) * 128], rhs=Va.t[:, qt, g, :], start=(pt_prev is None), stop=True), reads=[pt_cur.r, r_kv[qt]], writes=[ob.r])
                            ov = ob.t[:, 0:260].rearrange("p (h c) -> p h c", h=4)
                            S.op("dve", lambda e: e.tensor_tensor(out=den.t[:].unsqueeze(2), in0=ov[:, :, 64:65], in1=expsink.t[:, 4 * g:4 * g + 4].unsqueeze(2), op=ALU.add), reads=[ob.r, expsink.r], writes=[den.r])
                            S.op("dve", lambda e: e.reciprocal(out=rden.t[:], in_=den.t[:]), reads=[den.r], writes=[rden.r])
                            S.op("dve", lambda e: e.tensor_tensor(out=o_c.t[:, u, g * 256:(g + 1) * 256].rearrange("p (h c) -> p h c", h=4), in0=ov[:, :, 0:64], in1=rden.t[:].unsqueeze(2).broadcast_to([128, 4, 64]), op=ALU.mult), reads=[ob.r, rden.r], writes=[o_c.r])

                    for h in range(8):
                        i, ph = h // 2, (h % 2) * 64
                        acc = accs[h % 2]
                        for blk in range(2 * c + 2):
                            ptk = {}
                            for kt in (2 * blk, 2 * blk + 1):
                                r = kt - 4 * c
                                q0 = max(r, 0) * 128
                                n = 512 - q0
                                bank = nb()
                                S.op("pe", lambda e: e.matmul(bank.t[:, 0:n], lhsT=kT_b.t[ph:ph + 64, i, kt * 128:(kt + 1) * 128], rhs=qb_c.t[ph:ph + 64, i, q0:512], start=True, stop=(r < 0)), reads=[r_kv[kt], qb_c.r], writes=[bank.r])
                                if r >= 0:
                                    S.op("pe", lambda e: e.matmul(bank.t[:, 0:128], lhsT=ident_b.t[:], rhs=tri4.t[:, 0:128], start=False, stop=True), reads=[ident_b.r, tri4.r], writes=[bank.r])
                                p = npt()
                                S.op("act", lambda e: e.activation(out=p.t[:, q0:512], in_=bank.t[:, 0:n], func=AF.Exp, scale=0.125), reads=[bank.r], writes=[p.r])
                                ptk[kt] = p
                            ob = nob()
                            us = []
                            for u in range(4):
                                kts = [kt for kt in (2 * blk, 2 * blk + 1) if kt <= 4 * c + u]
                                if not kts:
                                    continue
                                us.append(u)
                                for ki, kt in enumerate(kts):
                                    S.op("pe", lambda e: e.matmul(ob.t[:, u * 65:(u + 1) * 65], lhsT=ptk[kt].t[:, u * 128:(u + 1) * 128], rhs=Vb.t[:, kt, h, :], start=(ki == 0), stop=(ki == len(kts) - 1)), reads=[ptk[kt].r, r_kv[kt]], writes=[ob.r])
                            first = (blk == 0)
                            need_sel = [(blk != 2 * c + u // 2) and (2 * c + u // 2 >= 4) for u in us]
                            if not any(need_sel):
                                u0, u1 = us[0], us[-1] + 1
                                if first:
                                    S.op("dve", lambda e: e.tensor_copy(out=acc.t[:, u0:u1, :], in_=ob.t[:, u0 * 65:u1 * 65].rearrange("p (u c) -> p u c", c=65)), reads=[ob.r], writes=[acc.r])
                                else:
                                    S.op("dve", lambda e: e.tensor_tensor(out=acc.t[:, u0:u1, :], in0=ob.t[:, u0 * 65:u1 * 65].rearrange("p (u c) -> p u c", c=65), in1=acc.t[:, u0:u1, :], op=ALU.add), reads=[ob.r, acc.r], writes=[acc.r])
                            else:
                                for u, ns in zip(us, need_sel):
                                    reg = ob.t[:, u * 65:(u + 1) * 65]
                                    sc = sel_c.t[:, u, h * 8 + blk:h * 8 + blk + 1]
                                    if ns and first:
                                        S.op("dve", lambda e: e.tensor_scalar(out=acc.t[:, u, :], in0=reg, scalar1=sc, scalar2=None, op0=ALU.mult), reads=[ob.r, sel_c.r], writes=[acc.r])
                                    elif ns:
                                        S.op("dve", lambda e: e.scalar_tensor_tensor(out=acc.t[:, u, :], in0=reg, scalar=sc, in1=acc.t[:, u, :], op0=ALU.mult, op1=ALU.add), reads=[ob.r, sel_c.r, acc.r], writes=[acc.r])
                                    elif first:
                                        S.op("dve", lambda e: e.tensor_copy(out=acc.t[:, u, :], in_=reg), reads=[ob.r], writes=[acc.r])
                                    else:
                                        S.op("dve", lambda e: e.tensor_tensor(out=acc.t[:, u, :], in0=reg, in1=acc.t[:, u, :], op=ALU.add), reads=[ob.r, acc.r], writes=[acc.r])
                        S.op("dve", lambda e: e.reciprocal(out=rden.t[:].unsqueeze(2), in_=acc.t[:, :, 64:65]), reads=[acc.r], writes=[rden.r])
                        S.op("dve", lambda e: e.tensor_tensor(out=o_c.t[:, :, 512 + h * 64:512 + (h + 1) * 64], in0=acc.t[:, :, 0:64], in1=rden.t[:].unsqueeze(2).broadcast_to([128, 4, 64]), op=ALU.mult), reads=[acc.r, rden.r], writes=[o_c.r])

                    for u in range(4):
                        t = 4 * c + u
                        T = s * 16 + t
                        pp = T % 2
                        xr, x1, x1b = xres[0], x1s[0], x1bs[pp]
                        S.dma("sp", lambda e: e.dma_start(out=xr.t[:], in_=x_d[T * 128:(T + 1) * 128, :]), "xres0", writes=[xr.r])
                        for k in range(8):
                            S.op("pe", lambda e: e.transpose(out=pb[6].t[:, k * 128:(k + 1) * 128], in_=o_c.t[:, u, k * 128:(k + 1) * 128], identity=ident_b.t[:]), reads=[o_c.r, ident_b.r], writes=[pb[6].r])
                        S.op("act", lambda e: e.activation(out=oT.t[:], in_=pb[6].t[:], func=AF.Copy), reads=[pb[6].r], writes=[oT.r])
                        for hf in range(2):
                            bank = nb()
                            for k in range(8):
                                S.op("pe", lambda e: e.matmul(bank.t[:, 0:512], lhsT=oT.t[:, k * 128:(k + 1) * 128], rhs=w_out.t[:, k, hf * 512:(hf + 1) * 512], start=(k == 0), stop=False), reads=[oT.r, w_out.r], writes=[bank.r])
                            S.op("pe", lambda e: e.matmul(bank.t[:, 0:512], lhsT=ones2.t[:, :], rhs=bout2.t[:, hf * 512:(hf + 1) * 512], start=False, stop=True), reads=[ones2.r, bout2.r], writes=[bank.r])
                            S.op("dve", lambda e: e.scalar_tensor_tensor(out=yln.t[:, hf * 512:(hf + 1) * 512], in0=xr.t[:, hf * 512:(hf + 1) * 512], scalar=ALPHA, in1=bank.t[:, 0:512], op0=ALU.mult, op1=ALU.add), reads=[xr.r, bank.r], writes=[yln.r])
                        layer_norm(lnb, yln, ln1g, ln1b, x1)
                        S.dma("sp", lambda e: e.dma_start(out=x1buf[T * 128:(T + 1) * 128, :], in_=x1.t[:]), "x1st0", reads=[x1.r], writes=[r_x1buf])
                        S.op("act", lambda e: e.activation(out=x1b.t[:], in_=x1.t[:], func=AF.Copy), reads=[x1.r], writes=[x1b.r])
                        for rr in range(2):
                            for k in range(4):
                                kk = rr * 4 + k
                                S.op("pe", lambda e: e.transpose(out=pb[7].t[:, k * 128:(k + 1) * 128], in_=x1.t[:, kk * 128:(kk + 1) * 128], identity=ident_f.t[:]), reads=[x1.r, ident_f.r], writes=[pb[7].r])
                            S.op("act", lambda e: e.activation(out=x1T.t[:, rr * 512:(rr + 1) * 512], in_=pb[7].t[:, 0:512], func=AF.Copy), reads=[pb[7].r], writes=[x1T.r])
                        for k in range(8):
                            S.op("pe", lambda e: e.matmul(pb[7].t[:, 0:NE], lhsT=x1T.t[:, k * 128:(k + 1) * 128], rhs=w_r.t[:, k, :], start=(k == 0), stop=(k == 7)), reads=[x1T.r, w_r.r], writes=[pb[7].r])
                        S.op("dve", lambda e: e.tensor_tensor(out=lg.t[:], in0=pb[7].t[:, 0:NE], in1=brB.t[:], op=ALU.add), reads=[pb[7].r, brB.r], writes=[lg.r])
                        S.op("dve", lambda e: e.max(out=top8.t[:], in_=lg.t[:]), reads=[lg.r], writes=[top8.r])
                        S.op("dve", lambda e: e.tensor_scalar(out=ntop.t[:], in0=top8.t[:, 0:1], scalar1=-1.0, scalar2=None, op0=ALU.mult), reads=[top8.r], writes=[ntop.r])
                        S.op("act", lambda e: e.activation(out=gex.t[:], in_=top8.t[:, 0:4], func=AF.Exp, bias=ntop.t[:, 0:1], scale=1.0, accum_out=gsum.t[:, 0:1]), reads=[top8.r, ntop.r], writes=[gex.r, gsum.r])
                        S.op("dve", lambda e: e.reciprocal(out=gsum.t[:], in_=gsum.t[:]), reads=[gsum.r], writes=[gsum.r])
                        S.op("dve", lambda e: e.tensor_scalar(out=gates.t[:, T, :], in0=gex.t[:], scalar1=gsum.t[:, 0:1], scalar2=None, op0=ALU.mult), reads=[gex.r, gsum.r], writes=[gates.r])
                        S.op("dve", lambda e: e.tensor_scalar(out=Mb.t[:], in0=lg.t[:], scalar1=top8.t[:, 3:4], scalar2=None, op0=ALU.is_ge), reads=[lg.r, top8.r], writes=[Mb.r])
                        S.op("pe", lambda e: e.matmul(pb[7].t[:, 64:64 + NE], lhsT=Lst.t[:], rhs=Mb.t[:], start=True, stop=True), reads=[Lst.r, Mb.r], writes=[pb[7].r])
                        S.op("pe", lambda e: e.matmul(pb[7].t[:, 128:128 + NE], lhsT=onesm.t[:], rhs=Mb.t[:], start=True, stop=True), reads=[onesm.r, Mb.r], writes=[pb[7].r])
                        S.op("dve", lambda e: e.tensor_tensor(out=posD.t[:], in0=pb[7].t[:, 64:64 + NE], in1=carryD.t[:], op=ALU.add), reads=[pb[7].r, carryD.r], writes=[posD.r])
                        S.op("dve", lambda e: e.tensor_tensor(out=carryD.t[:], in0=pb[7].t[:, 128:128 + NE], in1=carryD.t[:], op=ALU.add), reads=[pb[7].r, carryD.r], writes=[carryD.r])
                        for k in range(4):
                            S.op("dve", lambda e: e.scalar_tensor_tensor(out=junk.t[:], in0=lg.t[:], scalar=top8.t[:, k:k + 1], in1=posD.t[:], op0=ALU.is_equal, op1=ALU.mult, accum_out=destf.t[:, k:k + 1]), reads=[lg.r, top8.r, posD.r], writes=[junk.r, destf.r])
                        S.op("dve", lambda e: e.tensor_copy(out=dest.t[:, T, :], in_=destf.t[:]), reads=[destf.r], writes=[dest.r])
                        for k in range(4):
                            S.dma("pool", lambda e: e.indirect_dma_start(out=xbuf, out_offset=bass.IndirectOffsetOnAxis(ap=dest.t[:, T, k:k + 1], axis=0), in_=x1b.t[:], in_offset=None), "x1b%d" % pp, reads=[x1b.r, dest.r], writes=[r_xbuf])
            S.barrier()

        if dbg == "A":
            S.finish("sp", [r_x1buf])
            S.barrier()
            return nc

        with ExitStack() as st:
            b1g = sb(st, [128, NE, 8], F32, "b1g")
            b1l = sb(st, [128, NE, 8], F32, "b1l")
            S.dma("sp", lambda e: e.dma_start(out=b1g.t[:], in_=b1g_d), "b1g", writes=[b1g.r])
            S.dma("sp", lambda e: e.dma_start(out=b1l.t[:], in_=b1l_d), "b1l", writes=[b1l.r])
            wg = [sb(st, [128, 8, D], BF16, "wg") for _ in range(2)]
            wl = [sb(st, [128, 8, D], BF16, "wl") for _ in range(2)]
            w2 = [sb(st, [128, 8, D], BF16, "w2") for _ in range(2)]
            b2B = [sb(st, [128, D], F32, "b2B") for _ in range(2)]
            xeT = [sb(st, [128, 8, C], BF16, "xeT") for _ in range(2)]
            xss = [sb(st, [128, D], BF16, "xs") for _ in range(4)]
            actT = [sb(st, [128, 8, 512], BF16, "actT") for _ in range(2)]
            glu = [sb(st, [128, 512], F32, "glu") for _ in range(2)]
            sg = [sb(st, [128, 512], F32, "sg") for _ in range(2)]
            la = [sb(st, [128, 512], F32, "la") for _ in range(2)]
            ysb = [sb(st, [128, D], F32, "ysb") for _ in range(2)]
            xsc = [0]
            ysc = [0]
            hc = [0]
            chunks = []
            q0 = 0
            while q0 < C:
                n = min(512, C - q0)
                chunks.append((q0, n))
                q0 += n

            def load_w(e_):
                pp = e_ % 2
                for (wb, wd, nm) in ((wg[pp], w1g_d, "wg"), (wl[pp], w1l_d, "wl"), (w2[pp], w2_d, "w2")):
                    src = wd[e_].rearrange("(k p) f -> p k f", p=128)
                    for k in range(8):
                        S.dma("pool", lambda e: e.dma_start(out=wb.t[:, k, :], in_=src[:, k, :]), "%s%d" % (nm, pp), writes=[wb.r])
                S.dma("sp", lambda e: e.dma_start(out=b2B[pp].t[:], in_=b2_d[e_].partition_broadcast(128)), "b2B%d" % pp, writes=[b2B[pp].r])

            def prep_x(e_):
                pp = e_ % 2
                for s_ in range(NS):
                    xs = xss[xsc[0] % 4]
                    xsc[0] += 1
                    row0 = e_ * C + s_ * 128
                    S.dma("sp", lambda e: e.dma_start(out=xs.t[:], in_=xbuf[row0:row0 + 128, :]), "xs%d" % ((xsc[0] - 1) % 4), reads=[r_xbuf], writes=[xs.r])
                    for k in range(8):
                        S.op("pe", lambda e: e.transpose(out=pb[5].t[:, k * 128:(k + 1) * 128], in_=xs.t[:, k * 128:(k + 1) * 128], identity=ident_b.t[:]), reads=[xs.r, ident_b.r], writes=[pb[5].r])
                    S.op("act", lambda e: e.activation(out=xeT[pp].t[:, :, s_ * 128:(s_ + 1) * 128], in_=pb[5].t[:].rearrange("p (k q) -> p k q", k=8), func=AF.Copy), reads=[pb[5].r], writes=[xeT[pp].r])

            load_w(0)
            prep_x(0)
            for e_ in range(NE):
                pp = e_ % 2
                if e_ + 1 < NE:
                    load_w(e_ + 1)
                for ci, (q0, n) in enumerate(chunks):
                    aT = actT[ci % 2]
                    for j in range(8):
                        hb = hc[0] % 2
                        hc[0] += 1
                        bg, bl = pb[hb * 2], pb[hb * 2 + 1]
                        for k in range(8):
                            S.op("pe", lambda e: e.matmul(bg.t[:, 0:n], lhsT=wg[pp].t[:, k, j * 128:(j + 1) * 128], rhs=xeT[pp].t[:, k, q0:q0 + n], start=(k == 0), stop=(k == 7)), reads=[wg[pp].r, xeT[pp].r], writes=[bg.r])
                        for k in range(8):
                            S.op("pe", lambda e: e.matmul(bl.t[:, 0:n], lhsT=wl[pp].t[:, k, j * 128:(j + 1) * 128], rhs=xeT[pp].t[:, k, q0:q0 + n], start=(k == 0), stop=(k == 7)), reads=[wl[pp].r, xeT[pp].r], writes=[bl.r])
                        S.op("dve", lambda e: e.tensor_scalar(out=glu[hb].t[:, 0:n], in0=bg.t[:, 0:n], scalar1=b1g.t[:, e_, j:j + 1], scalar2=7.0, op0=ALU.add, op1=ALU.min), reads=[bg.r, b1g.r], writes=[glu[hb].r])
                        S.op("act", lambda e: e.activation(out=sg[hb].t[:, 0:n], in_=glu[hb].t[:, 0:n], func=AF.Sigmoid, scale=1.702), reads=[glu[hb].r], writes=[sg[hb].r])
                        S.op("dve", lambda e: e.tensor_scalar(out=la[hb].t[:, 0:n], in0=bl.t[:, 0:n], scalar1=b1l.t[:, e_, j:j + 1], scalar2=7.0, op0=ALU.add, op1=ALU.min), reads=[bl.r, b1l.r], writes=[la[hb].r])
                        S.op("pool", lambda e: e.tensor_scalar(out=la[hb].t[:, 0:n], in0=la[hb].t[:, 0:n], scalar1=-7.0, scalar2=1.0, op0=ALU.max, op1=ALU.add), reads=[la[hb].r], writes=[la[hb].r])
                        S.op("pool", lambda e: e.tensor_tensor(out=sg[hb].t[:, 0:n], in0=glu[hb].t[:, 0:n], in1=sg[hb].t[:, 0:n], op=ALU.mult), reads=[glu[hb].r, sg[hb].r], writes=[sg[hb].r])
                        S.op("dve", lambda e: e.tensor_tensor(out=aT.t[:, j, 0:n], in0=sg[hb].t[:, 0:n], in1=la[hb].t[:, 0:n], op=ALU.mult), reads=[sg[hb].r, la[hb].r], writes=[aT.r])
                    for sl in range(n // 128):
                        yb = ysb[ysc[0] % 2]
                        yi = ysc[0] % 2
                        ysc[0] += 1
                        for hf in range(2):
                            bank = pb[4] if hf == 0 else pb[7]
                            for j in range(8):
                                S.op("pe", lambda e: e.matmul(bank.t[:, 0:512], lhsT=aT.t[:, j, sl * 128:(sl + 1) * 128], rhs=w2[pp].t[:, j, hf * 512:(hf + 1) * 512], start=(j == 0), stop=(j == 7)), reads=[aT.r, w2[pp].r], writes=[bank.r])
                            S.op("dve", lambda e: e.tensor_tensor(out=yb.t[:, hf * 512:(hf + 1) * 512], in0=bank.t[:, 0:512], in1=b2B[pp].t[:, hf * 512:(hf + 1) * 512], op=ALU.add), reads=[bank.r, b2B[pp].r], writes=[yb.r])
                        row0 = e_ * C + q0 + sl * 128
                        S.dma("sp", lambda e: e.dma_start(out=ybuf[row0:row0 + 128, :], in_=yb.t[:]), "ysb%d" % yi, reads=[yb.r], writes=[r_ybuf])
                if e_ + 1 < NE:
                    prep_x(e_ + 1)
            S.barrier()

        with ExitStack() as st:
            ln2g = sb(st, [128, D], F32, "ln2g")
            ln2b = sb(st, [128, D], F32, "ln2b")
            S.dma("sp", lambda e: e.dma_start(out=ln2g.t[:], in_=ln2g_d[0].partition_broadcast(128)), "ln2g", writes=[ln2g.r])
            S.dma("sp", lambda e: e.dma_start(out=ln2b.t[:], in_=ln2b_d[0].partition_broadcast(128)), "ln2b", writes=[ln2b.r])
            ygs = [[sb(st, [128, D], F32, "yg") for _ in range(4)] for _ in range(2)]
            x1r = [sb(st, [128, D], F32, "x1r") for _ in range(2)]
            accC = [sb(st, [128, D], F32, "accC") for _ in range(2)]
            outb = [sb(st, [128, D], F32, "outb") for _ in range(2)]
            lnb2 = (sb(st, [128, 2, 6], F32, "stats2"), sb(st, [128, 2], F32, "mv2"), sb(st, [128, 1], F32, "lnv2"),
                    sb(st, [128, 1], F32, "rstd2"), sb(st, [128, 1], F32, "nmr2"), sb(st, [128, D], F32, "xn2"))
            for T in range(NT):
                pp = T % 2
                S.dma("sp", lambda e: e.dma_start(out=x1r[pp].t[:], in_=x1buf[T * 128:(T + 1) * 128, :]), "x1r%d" % pp, reads=[r_x1buf], writes=[x1r[pp].r])
                for k in range(4):
                    yg = ygs[pp][k]
                    S.dma("pool", lambda e: e.indirect_dma_start(out=yg.t[:], out_offset=None, in_=ybuf, in_offset=bass.IndirectOffsetOnAxis(ap=dest.t[:, T, k:k + 1], axis=0)), "yg%d_%d" % (pp, k), reads=[r_ybuf, dest.r], writes=[yg.r])
                a = accC[pp]
                S.op("act", lambda e: e.activation(out=a.t[:], in_=x1r[pp].t[:], func=AF.Copy, scale=ALPHA), reads=[x1r[pp].r], writes=[a.r])
                for k in range(4):
                    yg = ygs[pp][k]
                    S.op("dve", lambda e: e.scalar_tensor_tensor(out=a.t[:], in0=yg.t[:], scalar=gates.t[:, T, k:k + 1], in1=a.t[:], op0=ALU.mult, op1=ALU.add), reads=[yg.r, gates.r, a.r], writes=[a.r])
                layer_norm(lnb2, a, ln2g, ln2b, outb[pp])
                S.dma("sp", lambda e: e.dma_start(out=out_d[T * 128:(T + 1) * 128, :], in_=outb[pp].t[:]), "outb%d" % pp, reads=[outb[pp].r], writes=[r_out])
            S.finish("sp", [r_out])
            S.barrier()
    return nc


def rope_tables():
    inv = 1.0 / (10000.0 ** (np.arange(0, 64, 2, dtype=np.float32) / 64.0))
    ang = np.arange(SEQ, dtype=np.float32)[:, None] * inv[None, :].astype(np.float32)
    cos = np.cos(ang).astype(np.float32)
    sin = np.sin(ang).astype(np.float32)
    return np.concatenate([cos, cos], 1), np.concatenate([-sin, sin], 1)


def prep_weights(w_in, b_in, sinks, w_out, b_out, ln1_g, ln1_b, w_router, b_router, w1, b1, w2, b2, ln2_g, ln2_b):
    f = lambda a: np.ascontiguousarray(np.asarray(a, dtype=np.float32))
    rc, rs = rope_tables()
    w1 = np.asarray(w1)[0]
    b1 = np.asarray(b1)[0]
    m = {
        "w_in": f(w_in[0]), "b_in": f(b_in[0]).reshape(1, -1), "sinks": f(sinks[0]).reshape(1, -1),
        "w_out": f(w_out[0]), "b_out": f(b_out[0]).reshape(1, -1),
        "ln1_g": f(ln1_g[0]).reshape(1, -1), "ln1_b": f(ln1_b[0]).reshape(1, -1),
        "w_router": f(np.asarray(w_router[0]).reshape(8, 128, NE).transpose(1, 0, 2)), "b_router": f(b_router[0]).reshape(1, -1),
        "w1g": f(w1[:, :, 0::2]), "w1l": f(w1[:, :, 1::2]),
        "b1g": f(b1[:, 0::2].reshape(NE, 8, 128).transpose(2, 0, 1)),
        "b1l": f(b1[:, 1::2].reshape(NE, 8, 128).transpose(2, 0, 1)),
        "w2": f(w2[0]), "b2": f(b2[0]),
        "ln2_g": f(ln2_g[0]).reshape(1, -1), "ln2_b": f(ln2_b[0]).reshape(1, -1),
        "ropeC": f(rc.reshape(16, 128, 64).transpose(1, 0, 2)), "ropeS": f(rs.reshape(16, 128, 64).transpose(1, 0, 2)),
    }
    return m


def kernel(x, w_in, b_in, sinks, w_out, b_out, ln1_g, ln1_b, w_router, b_router, w1, b1, w2, b2, ln2_g, ln2_b):
    x = np.asarray(x, dtype=np.float32)
    B = x.shape[0]
    nseq = B // N_CORES
    wm = prep_weights(w_in, b_in, sinks, w_out, b_out, ln1_g, ln1_b, w_router, b_router, w1, b1, w2, b2, ln2_g, ln2_b)
    nc = build(nseq, 1536)
    in_maps = []
    for c in range(N_CORES):
        m = dict(wm)
        m["x"] = np.ascontiguousarray(x[c * nseq:(c + 1) * nseq].reshape(nseq * SEQ, D))
        in_maps.append(m)
    res = run_bass_kernel_spmd(nc, in_maps, core_ids=list(range(N_CORES)))
    out = np.concatenate([r["out"].reshape(nseq, SEQ, D) for r in res.results], axis=0)
    return out.astype(np.float32)
```

```python
import numpy as np
from contextlib import ExitStack
import concourse.bass as bass
import concourse.mybir as mybir
from concourse.bass_utils import run_bass_kernel_spmd

F32 = mybir.dt.float32
BF16 = mybir.dt.bfloat16
U32 = mybir.dt.uint32
I32 = mybir.dt.int32
AF = mybir.ActivationFunctionType
ALU = mybir.AluOpType
AX = mybir.AxisListType

D = 1024
SEQ = 2048
NE = 32
ALPHA = float(2.0 ** 0.25)
EPS = 1e-5
NEG = -30000.0
SINV = float(1.0 / 1.702)
UMAX = float(11.914 / (1.0 + np.exp(-11.914)))
N_CORES = 8


class Res:
    __slots__ = ("name", "w", "r", "multi", "excl")

    def __init__(self, name, multi=False, excl=False):
        self.excl = excl
        self.name = name
        self.w = {}
        self.r = {}
        self.multi = multi


class Buf:
    __slots__ = ("t", "r")

    def __init__(self, t, name):
        self.t = t
        self.r = Res(name)


class Sched:
    def __init__(self, nc, stack):
        self.nc = nc
        self.stack = stack
        self.eng = {"pe": nc.tensor, "act": nc.scalar, "dve": nc.vector, "pool": nc.gpsimd, "sp": nc.sync}
        self.sem = {}
        self.cnt = {}
        self.waited = {k: {} for k in self.eng}
        for k in self.eng:
            self.sem[k] = stack.enter_context(nc.semaphore("s_" + k))
            self.cnt[k] = 0
        self.dsem = {}
        self.dcnt = {}
        self.nwait = 0
        self.nins = 0

    def _wait(self, e, deps):
        for key, (sh, val) in deps.items():
            if self.waited[e].get(key, 0) >= val:
                continue
            self.eng[e].wait_ge(sh, val)
            self.waited[e][key] = val
            self.nwait += 1

    def _deps(self, e, reads, writes, pe_order=False):
        deps = {}

        def add(key, sh, val):
            if key == "pe" and e == "pe" and not pe_order:
                return
            if key not in deps or deps[key][1] < val:
                deps[key] = (sh, val)

        for r in reads:
            for key, (sh, val) in r.w.items():
                add(key, sh, val)
            if r.excl:
                for key, (sh, val) in r.r.items():
                    if key != e:
                        add(key, sh, val)
        for w in writes:
            if not w.multi:
                for key, (sh, val) in w.w.items():
                    add(key, sh, val)
            for key, (sh, val) in w.r.items():
                add(key, sh, val)
        return deps

    def _record(self, key, sh, val, reads, writes):
        for r in reads:
            r.r[key] = (sh, val)
        for w in writes:
            if w.multi:
                w.w[key] = (sh, val)
            else:
                w.w = {key: (sh, val)}
                w.r = {}

    def op(self, e, fn, reads=(), writes=(), pe_order=False):
        deps = self._deps(e, reads, writes, pe_order)
        self._wait(e, deps)
        ins = fn(self.eng[e])
        self.cnt[e] += 1
        self.nins += 1
        ins.then_inc(self.sem[e], 1)
        self._record(e, self.sem[e], self.cnt[e], reads, writes)
        return ins

    def dma(self, q, fn, dname, reads=(), writes=()):
        if dname not in self.dsem:
            self.dsem[dname] = self.stack.enter_context(self.nc.semaphore("d_" + dname))
            self.dcnt[dname] = 0
        key = "d_" + dname
        deps = self._deps(q, reads, writes)
        deps.pop(key, None)
        self._wait(q, deps)
        ins = fn(self.eng[q])
        self.dcnt[dname] += 16
        self.nins += 1
        ins.then_inc(self.dsem[dname], 16)
        self._record(key, self.dsem[dname], self.dcnt[dname], reads, writes)
        return ins

    def barrier(self):
        allev = {}
        for k in self.eng:
            if self.cnt[k] > 0:
                allev[k] = (self.sem[k], self.cnt[k])
        for dn in self.dsem:
            if self.dcnt[dn] > 0:
                allev["d_" + dn] = (self.dsem[dn], self.dcnt[dn])
        for e in self.eng:
            deps = {k: v for k, v in allev.items() if k != e}
            self._wait(e, deps)

    def finish(self, e, resources):
        deps = {}
        for r in resources:
            for key, (sh, val) in list(r.w.items()) + list(r.r.items()):
                if key not in deps or deps[key][1] < val:
                    deps[key] = (sh, val)
        self._wait(e, deps)


def build(NSEQ, C, dbg=False):
    NT = NSEQ * 16
    NTOK = NSEQ * SEQ
    NS = C // 128
    NROW = NE * C + 1024
    nc = bass.Bass("TRN2", target_bir_lowering=False)

    def din(name, shape, dt=F32):
        return nc.dram_tensor(name, shape, dt, kind="ExternalInput").ap()

    x_d = din("x", [NTOK, D])
    w_in_d = din("w_in", [D, 2304])
    b_in_d = din("b_in", [1, 2304])
    sinks_d = din("sinks", [1, 8])
    w_out_d = din("w_out", [D, D])
    b_out_d = din("b_out", [1, D])
    ln1g_d = din("ln1_g", [1, D])
    ln1b_d = din("ln1_b", [1, D])
    w_r_d = din("w_router", [128, 8, NE])
    b_r_d = din("b_router", [1, NE])
    w1g_d = din("w1g", [NE, D, D])
    w1l_d = din("w1l", [NE, D, D])
    b1g_d = din("b1g", [128, NE, 8])
    b1l_d = din("b1l", [128, NE, 8])
    w2_d = din("w2", [NE, D, D])
    b2_d = din("b2", [NE, D])
    ln2g_d = din("ln2_g", [1, D])
    ln2b_d = din("ln2_b", [1, D])
    ropeC_d = din("ropeC", [128, 16, 64])
    ropeS_d = din("ropeS", [128, 16, 64])
    out_d = nc.dram_tensor("out", [NTOK, D], F32, kind="ExternalOutput").ap()
    x1buf = nc.dram_tensor("x1buf", [NTOK, D], F32, kind="ExternalOutput" if dbg else "Internal").ap()
    xbuf = nc.dram_tensor("xbuf", [NROW, D], BF16, kind="Internal").ap()
    ybuf = nc.dram_tensor("ybuf", [NROW, D], F32, kind="Internal").ap()
    r_x1buf, r_xbuf, r_ybuf, r_out = Res("x1buf", True), Res("xbuf", True), Res("ybuf", True), Res("out", True)

    with ExitStack() as st0:
        S = Sched(nc, st0)
        uid = [0]

        def sb(st, shape, dt=F32, name="t"):
            uid[0] += 1
            nm = "%s_%d" % (name, uid[0])
            return Buf(st.enter_context(nc.sbuf_tensor(nm, shape, dt)), nm)

        pb = []
        for i in range(8):
            if i in (5, 6):
                t = st0.enter_context(nc.psum_tensor("pb%d" % i, [128, 1024], BF16))
            else:
                t = st0.enter_context(nc.psum_tensor("pb%d" % i, [128, 512], F32))
            pb.append(Buf(t, "pb%d" % i))
            pb[-1].r.excl = True
        bigc = [0]

        def nb():
            b = pb[bigc[0] % 3]
            bigc[0] += 1
            return b

        oc = [0]

        def nob():
            b = pb[3 + oc[0] % 2]
            oc[0] += 1
            return b

        gates = sb(st0, [128, NT, 4], F32, "gates")
        dest = sb(st0, [128, NT, 4], U32, "dest")
        ident_b = sb(st0, [128, 128], BF16, "identb")
        ident_f = sb(st0, [128, 128], F32, "identf")
        ones2 = sb(st0, [2, 128], BF16, "ones2")
        m10 = sb(st0, [2, 1], F32, "m10")

        S.op("pool", lambda e: e.memset(ident_f.t[:], 0.0), writes=[ident_f.r])
        S.op("pool", lambda e: e.affine_select(out=ident_f.t[:], in_=ident_f.t[:], pattern=[[-1, 128]], compare_op=ALU.not_equal, fill=1.0, base=0, channel_multiplier=1), reads=[ident_f.r], writes=[ident_f.r])
        S.op("dve", lambda e: e.tensor_copy(out=ident_b.t[:], in_=ident_f.t[:]), reads=[ident_f.r], writes=[ident_b.r])
        S.op("dve", lambda e: e.memset(ones2.t[:], 1.0), writes=[ones2.r])
        S.op("dve", lambda e: e.memset(m10.t[:], 0.0), writes=[m10.r])
        S.op("dve", lambda e: e.memset(m10.t[0:1, :], 1.0), reads=[m10.r], writes=[m10.r])

        def make_hilo(st, dst, src_row, n, tag):
            stg = sb(st, [2, n], F32, "hl_s" + tag)
            hb = sb(st, [2, n], BF16, "hl_b" + tag)
            hf = sb(st, [2, n], F32, "hl_f" + tag)
            lo = sb(st, [2, n], F32, "hl_l" + tag)
            S.dma("sp", lambda e: e.dma_start(out=stg.t[:], in_=src_row.partition_broadcast(2)), "hl" + tag, writes=[stg.r])
            hilo_compute(dst, stg, hb, hf, lo, n)

        def hilo_compute(dst, stg, hb, hf, lo, n):
            S.op("dve", lambda e: e.tensor_copy(out=hb.t[:, 0:n], in_=stg.t[:, 0:n]), reads=[stg.r], writes=[hb.r])
            S.op("dve", lambda e: e.tensor_copy(out=hf.t[:, 0:n], in_=hb.t[:, 0:n]), reads=[hb.r], writes=[hf.r])
            S.op("dve", lambda e: e.tensor_tensor(out=lo.t[:, 0:n], in0=stg.t[:, 0:n], in1=hf.t[:, 0:n], op=ALU.subtract), reads=[stg.r, hf.r], writes=[lo.r])
            S.op("dve", lambda e: e.tensor_tensor(out=hf.t[:, 0:n], in0=hf.t[:, 0:n], in1=lo.t[:, 0:n], op=ALU.subtract), reads=[hf.r, lo.r], writes=[hf.r])
            S.op("dve", lambda e: e.scalar_tensor_tensor(out=dst.t[:, 0:n], in0=hf.t[:, 0:n], scalar=m10.t[:, 0:1], in1=lo.t[:, 0:n], op0=ALU.mult, op1=ALU.add), reads=[hf.r, lo.r, m10.r], writes=[dst.r])

        def layer_norm(st_bufs, yln, gB, bB, outb, eng_gb="pool"):
            stats, mv, lnv, rstd, nmr, xn = st_bufs
            S.op("dve", lambda e: e.bn_stats(out=stats.t[:, 0, :], in_=yln.t[:, 0:512]), reads=[yln.r], writes=[stats.r])
            S.op("dve", lambda e: e.bn_stats(out=stats.t[:, 1, :], in_=yln.t[:, 512:1024]), reads=[yln.r], writes=[stats.r])
            S.op("dve", lambda e: e.bn_aggr(out=mv.t[:], in_=stats.t[:].rearrange("p a b -> p (a b)")), reads=[stats.r], writes=[mv.r])
            S.op("act", lambda e: e.activation(out=lnv.t[:], in_=mv.t[:, 1:2], func=AF.Ln, bias=EPS, scale=1.0), reads=[mv.r], writes=[lnv.r])
            S.op("act", lambda e: e.activation(out=rstd.t[:], in_=lnv.t[:], func=AF.Exp, scale=-0.5), reads=[lnv.r], writes=[rstd.r])
            S.op("dve", lambda e: e.tensor_scalar(out=nmr.t[:], in0=mv.t[:, 0:1], scalar1=-1.0, scalar2=rstd.t[:, 0:1], op0=ALU.mult, op1=ALU.mult), reads=[mv.r, rstd.r], writes=[nmr.r])
            S.op("act", lambda e: e.activation(out=xn.t[:], in_=yln.t[:], func=AF.Identity, scale=rstd.t[:, 0:1], bias=nmr.t[:, 0:1]), reads=[yln.r, rstd.r, nmr.r], writes=[xn.r])
            S.op(eng_gb, lambda e: e.tensor_tensor(out=xn.t[:], in0=xn.t[:], in1=gB.t[:], op=ALU.mult), reads=[xn.r, gB.r], writes=[xn.r])
            S.op(eng_gb, lambda e: e.tensor_tensor(out=outb.t[:], in0=xn.t[:], in1=bB.t[:], op=ALU.add), reads=[xn.r, bB.r], writes=[outb.r])

        with ExitStack() as st:
            w_in = sb(st, [128, 8, 2304], BF16, "w_in")
            w_out = sb(st, [128, 8, D], BF16, "w_out")
            w_r = sb(st, [128, 8, NE], F32, "w_r")
            bin2 = sb(st, [2, 2304], BF16, "bin2")
            bout2 = sb(st, [2, D], BF16, "bout2")
            ropeC = sb(st, [128, 16, 64], F32, "ropeC")
            ropeS = sb(st, [128, 16, 64], F32, "ropeS")
            ln1g = sb(st, [128, D], F32, "ln1g")
            ln1b = sb(st, [128, D], F32, "ln1b")
            expsink = sb(st, [128, 8], F32, "expsink")
            brB = sb(st, [128, NE], F32, "brB")
            carryD = sb(st, [128, NE], F32, "carryD")
            carryI = sb(st, [128, NE], I32, "carryI")
            tri4 = sb(st, [128, 512], BF16, "tri4")
            atri4 = sb(st, [128, 512], BF16, "atri4")
            Lst = sb(st, [128, 128], BF16, "Lst")
            onesm = sb(st, [128, 128], BF16, "onesm")
            elig = sb(st, [128, 4, 64], F32, "elig")
            stt = ExitStack()
            trif = sb(stt, [128, 512], F32, "trif")

            w_in_v = w_in_d.rearrange("(k p) c -> p k c", p=128)
            w_out_v = w_out_d.rearrange("(k p) c -> p k c", p=128)
            for k in range(8):
                for h2 in range(2):
                    S.dma("pool", lambda e: e.dma_start(out=w_in.t[:, k, h2 * 1152:(h2 + 1) * 1152], in_=w_in_v[:, k, h2 * 1152:(h2 + 1) * 1152]), "w_in", writes=[w_in.r])
                S.dma("pool", lambda e: e.dma_start(out=w_out.t[:, k, :], in_=w_out_v[:, k, :]), "w_out", writes=[w_out.r])
            S.dma("sp", lambda e: e.dma_start(out=w_r.t[:], in_=w_r_d), "w_r", writes=[w_r.r])
            S.dma("sp", lambda e: e.dma_start(out=ropeC.t[:], in_=ropeC_d), "ropeC", writes=[ropeC.r])
            S.dma("sp", lambda e: e.dma_start(out=ropeS.t[:], in_=ropeS_d), "ropeS", writes=[ropeS.r])
            S.dma("sp", lambda e: e.dma_start(out=ln1g.t[:], in_=ln1g_d[0].partition_broadcast(128)), "ln1g", writes=[ln1g.r])
            S.dma("sp", lambda e: e.dma_start(out=ln1b.t[:], in_=ln1b_d[0].partition_broadcast(128)), "ln1b", writes=[ln1b.r])
            S.dma("sp", lambda e: e.dma_start(out=expsink.t[:], in_=sinks_d[0].partition_broadcast(128)), "sinks", writes=[expsink.r])
            S.dma("sp", lambda e: e.dma_start(out=brB.t[:], in_=b_r_d[0].partition_broadcast(128)), "brB", writes=[brB.r])
            S.op("act", lambda e: e.activation(out=expsink.t[:], in_=expsink.t[:], func=AF.Exp), reads=[expsink.r], writes=[expsink.r])
            make_hilo(stt, bin2, b_in_d[0], 2304, "bin")
            make_hilo(stt, bout2, b_out_d[0], D, "bout")
            S.op("pool", lambda e: e.iota(carryI.t[:], pattern=[[C, NE]], base=0, channel_multiplier=0), writes=[carryI.r])
            S.op("dve", lambda e: e.tensor_copy(out=carryD.t[:], in_=carryI.t[:]), reads=[carryI.r], writes=[carryD.r])
            S.op("pool", lambda e: e.memset(trif.t[:], 0.0), writes=[trif.r])
            S.op("pool", lambda e: e.affine_select(out=trif.t[:], in_=trif.t[:], pattern=[[0, 4], [1, 128]], compare_op=ALU.is_ge, fill=NEG, base=0, channel_multiplier=-1), reads=[trif.r], writes=[trif.r])
            S.op("dve", lambda e: e.tensor_copy(out=tri4.t[:], in_=trif.t[:]), reads=[trif.r], writes=[tri4.r])
            S.op("pool", lambda e: e.memset(trif.t[:], 0.0), reads=[trif.r], writes=[trif.r])
            S.op("pool", lambda e: e.affine_select(out=trif.t[:], in_=trif.t[:], pattern=[[0, 4], [-1, 128]], compare_op=ALU.is_gt, fill=NEG, base=0, channel_multiplier=1), reads=[trif.r], writes=[trif.r])
            S.op("dve", lambda e: e.tensor_copy(out=atri4.t[:], in_=trif.t[:]), reads=[trif.r], writes=[atri4.r])
            S.op("pool", lambda e: e.memset(trif.t[:, 0:128], 1.0), reads=[trif.r], writes=[trif.r])
            S.op("pool", lambda e: e.affine_select(out=trif.t[:, 0:128], in_=trif.t[:, 0:128], pattern=[[1, 128]], compare_op=ALU.is_gt, fill=0.0, base=0, channel_multiplier=-1), reads=[trif.r], writes=[trif.r])
            S.op("dve", lambda e: e.tensor_copy(out=Lst.t[:], in_=trif.t[:, 0:128]), reads=[trif.r], writes=[Lst.r])
            S.op("dve", lambda e: e.memset(onesm.t[:], 1.0), writes=[onesm.r])
            S.op("dve", lambda e: e.memset(elig.t[:], 0.0), writes=[elig.r])
            for j in range(4, 8):
                S.op("dve", lambda e: e.memset(elig.t[:, j - 4, :].rearrange("p (h b) -> p h b", h=8)[:, :, j:8], -1e30), reads=[elig.r], writes=[elig.r])

            S.barrier()
            stt.close()
            kT_a = sb(st, [128, SEQ], BF16, "kT_a")
            kT_b = sb(st, [128, 4, SEQ], BF16, "kT_b")
            Va = sb(st, [128, 16, 2, 65], BF16, "Va")
            Vb = sb(st, [128, 16, 8, 65], BF16, "Vb")
            r_kv = [Res("kv%d" % t) for t in range(16)]
            S.op("pool", lambda e: e.memset(Va.t[:], 1.0), writes=r_kv)
            S.op("pool", lambda e: e.memset(Vb.t[:], 1.0), writes=r_kv)
            kms = sb(st, [128, 4, 8], F32, "kms")
            kmT = sb(st, [128, 4, 8], BF16, "kmT")
            S.op("dve", lambda e: e.memset(kms.t[:], 0.0), writes=[kms.r])
            S.op("dve", lambda e: e.memset(kmT.t[:], 0.0), writes=[kmT.r])
            xbs = [sb(st, [128, D], BF16, "xb") for _ in range(2)]
            xTs = [sb(st, [128, D], BF16, "xT") for _ in range(2)]
            m1s = [sb(st, [128, 512], F32, "m1") for _ in range(2)]
            m2s = [sb(st, [128, 512], F32, "m2") for _ in range(2)]
            rqa = [sb(st, [128, 512], BF16, "rqa") for _ in range(2)]
            rqb = [sb(st, [128, 512], BF16, "rqb") for _ in range(2)]
            rkb = [sb(st, [128, 512], BF16, "rkb") for _ in range(2)]
            rka = [sb(st, [128, 128], BF16, "rka") for _ in range(2)]
            qTa = [sb(st, [128, 4, 512], BF16, "qTa") for _ in range(1)]
            qTb = [sb(st, [128, 4, 512], BF16, "qTb") for _ in range(1)]
            sel = [sb(st, [128, 4, 64], F32, "sel") for _ in range(2)]
            gm = sb(st, [128, 64], F32, "gm")
            top = sb(st, [128, 8, 8], F32, "top")
            pts = [sb(st, [128, 512], BF16, "pt") for _ in range(4)]
            ptc = [0]
            den = sb(st, [128, 4], F32, "den")
            rden = sb(st, [128, 4], F32, "rden")
            accs = [sb(st, [128, 4, 65], F32, "acc") for _ in range(2)]
            o_t = [sb(st, [128, 4, D], BF16, "o_t") for _ in range(1)]
            oT = sb(st, [128, D], BF16, "oT")
            xres = [sb(st, [128, D], F32, "xres") for _ in range(1)]
            yln = sb(st, [128, D], F32, "yln")
            lnb = (sb(st, [128, 2, 6], F32, "stats"), sb(st, [128, 2], F32, "mv"), sb(st, [128, 1], F32, "lnv"),
                   sb(st, [128, 1], F32, "rstd"), sb(st, [128, 1], F32, "nmr"), sb(st, [128, D], F32, "xn"))
            x1s = [sb(st, [128, D], F32, "x1") for _ in range(1)]
            x1bs = [sb(st, [128, D], BF16, "x1b") for _ in range(2)]
            x1T = sb(st, [128, D], F32, "x1T")
            lg = sb(st, [128, NE], F32, "lg")
            top8 = sb(st, [128, 8], F32, "top8")
            ntop = sb(st, [128, 1], F32, "ntop")
            gex = sb(st, [128, 4], F32, "gex")
            gsum = sb(st, [128, 1], F32, "gsum")
            Mb = sb(st, [128, NE], BF16, "Mb")
            posD = sb(st, [128, NE], F32, "posD")
            junk = sb(st, [128, NE], F32, "junk")
            destf = sb(st, [128, 4], F32, "destf")

            def npt():
                b = pts[ptc[0] % 4]
                ptc[0] += 1
                return b

            def rope(src_bank, H, t, m1, m2, out_view_fn):
                n = H * 64
                src = src_bank.t[:, 0:n].rearrange("p (h c) -> p h c", h=H)
                Cb = ropeC.t[:, t, :].unsqueeze(1).broadcast_to([128, H, 64])
                Sb1 = ropeS.t[:, t, 0:32].unsqueeze(1).broadcast_to([128, H, 32])
                Sb2 = ropeS.t[:, t, 32:64].unsqueeze(1).broadcast_to([128, H, 32])
                m1v = m1.t[:, 0:n].rearrange("p (h c) -> p h c", h=H)
                m2v = m2.t[:, 0:n].rearrange("p (h c) -> p h c", h=H)
                S.op("dve", lambda e: e.tensor_tensor(out=m1v, in0=src, in1=Cb, op=ALU.mult), reads=[src_bank.r, ropeC.r], writes=[m1.r])
                S.op("dve", lambda e: e.tensor_tensor(out=m2v[:, :, 0:32], in0=src[:, :, 32:64], in1=Sb1, op=ALU.mult), reads=[src_bank.r, ropeS.r], writes=[m2.r])
                S.op("dve", lambda e: e.tensor_tensor(out=m2v[:, :, 32:64], in0=src[:, :, 0:32], in1=Sb2, op=ALU.mult), reads=[src_bank.r, ropeS.r], writes=[m2.r])
                return m1v, m2v

            for s in range(NSEQ):
                for c in range(4):
                    cb = (s * 4 + c) % 2
                    qa_c, qb_c, sel_c, o_c = qTa[0], qTb[0], sel[cb], o_t[0]
                    for u in range(4):
                        t = 4 * c + u
                        T = s * 16 + t
                        pp = T % 2
                        xb, xT = xbs[pp], xTs[pp]
                        S.dma("pool", lambda e: e.dma_start(out=xb.t[:], in_=x_d[T * 128:(T + 1) * 128, :]), "xb%d" % pp, writes=[xb.r])
                        for k in range(8):
                            S.op("pe", lambda e: e.transpose(out=pb[5].t[:, k * 128:(k + 1) * 128], in_=xb.t[:, k * 128:(k + 1) * 128], identity=ident_b.t[:]), reads=[xb.r, ident_b.r], writes=[pb[5].r])
                        S.op("act", lambda e: e.activation(out=xT.t[:], in_=pb[5].t[:], func=AF.Copy), reads=[pb[5].r], writes=[xT.r])

                        def proj(col0, n):
                            bank = nb()
                            for k in range(8):
                                S.op("pe", lambda e: e.matmul(bank.t[:, 0:n], lhsT=xT.t[:, k * 128:(k + 1) * 128], rhs=w_in.t[:, k, col0:col0 + n], start=(k == 0), stop=False), reads=[xT.r, w_in.r], writes=[bank.r])
                            S.op("pe", lambda e: e.matmul(bank.t[:, 0:n], lhsT=ones2.t[:, :], rhs=bin2.t[:, col0:col0 + n], start=False, stop=True), reads=[ones2.r, bin2.r], writes=[bank.r])
                            return bank

                        bank = proj(0, 512)
                        m1v, m2v = rope(bank, 8, t, m1s[0], m2s[0], None)
                        ov = rqa[pp].t[:].rearrange("p (i two c) -> p two i c", i=4, two=2)
                        S.op("pool", lambda e: e.tensor_tensor(out=ov, in0=m1s[0].t[:].rearrange("p (two i c) -> p two i c", two=2, i=4), in1=m2s[0].t[:].rearrange("p (two i c) -> p two i c", two=2, i=4), op=ALU.add), reads=[m1s[0].r, m2s[0].r], writes=[rqa[pp].r])
                        bank = proj(768, 512)
                        rope(bank, 8, t, m1s[1], m2s[1], None)
                        S.op("pool", lambda e: e.tensor_tensor(out=rqb[pp].t[:], in0=m1s[1].t[:], in1=m2s[1].t[:], op=ALU.add), reads=[m1s[1].r, m2s[1].r], writes=[rqb[pp].r])
                        for i in range(4):
                            S.op("pe", lambda e: e.transpose(out=pb[6].t[:, i * 128:(i + 1) * 128], in_=rqa[pp].t[:, i * 128:(i + 1) * 128], identity=ident_b.t[:]), reads=[rqa[pp].r, ident_b.r], writes=[pb[6].r])
                        for i in range(4):
                            S.op("pe", lambda e: e.transpose(out=pb[6].t[:, 512 + i * 128:512 + (i + 1) * 128], in_=rqb[pp].t[:, i * 128:(i + 1) * 128], identity=ident_b.t[:]), reads=[rqb[pp].r, ident_b.r], writes=[pb[6].r])
                        S.op("act", lambda e: e.activation(out=qa_c.t[:, u, :], in_=pb[6].t[:, 0:512], func=AF.Copy), reads=[pb[6].r], writes=[qa_c.r])
                        S.op("act", lambda e: e.activation(out=qb_c.t[:, :, u * 128:(u + 1) * 128], in_=pb[6].t[:, 512:1024].rearrange("p (i q) -> p i q", i=4), func=AF.Copy), reads=[pb[6].r], writes=[qb_c.r])
                        bank = proj(1280, 512)
                        rope(bank, 8, t, m1s[0], m2s[0], None)
                        S.op("pool", lambda e: e.tensor_tensor(out=rkb[pp].t[:], in0=m1s[0].t[:], in1=m2s[0].t[:], op=ALU.add), reads=[m1s[0].r, m2s[0].r], writes=[rkb[pp].r])
                        bank = proj(512, 256)
                        rope(bank, 2, t, m1s[1], m2s[1], None)
                        S.op("pool", lambda e: e.tensor_tensor(out=rka[pp].t[:], in0=m1s[1].t[:, 0:128], in1=m2s[1].t[:, 0:128], op=ALU.add), reads=[m1s[1].r, m2s[1].r], writes=[rka[pp].r])
                        S.op("act", lambda e: e.activation(out=Va.t[:, t, :, 0:64], in_=bank.t[:, 128:256].rearrange("p (h c) -> p h c", h=2), func=AF.Copy), reads=[bank.r], writes=[r_kv[t]])
                        for i in range(4):
                            S.op("pe", lambda e: e.transpose(out=pb[5].t[:, i * 128:(i + 1) * 128], in_=rkb[pp].t[:, i * 128:(i + 1) * 128], identity=ident_b.t[:]), reads=[rkb[pp].r, ident_b.r], writes=[pb[5].r])
                        S.op("pe", lambda e: e.transpose(out=pb[5].t[:, 512:640], in_=rka[pp].t[:, 0:128], identity=ident_b.t[:]), reads=[rka[pp].r, ident_b.r], writes=[pb[5].r])
                        S.op("act", lambda e: e.activation(out=kT_b.t[:, :, t * 128:(t + 1) * 128], in_=pb[5].t[:, 0:512].rearrange("p (i q) -> p i q", i=4), func=AF.Copy), reads=[pb[5].r], writes=[r_kv[t]])
                        S.op("act", lambda e: e.activation(out=kT_a.t[:, t * 128:(t + 1) * 128], in_=pb[5].t[:, 512:640], func=AF.Copy), reads=[pb[5].r], writes=[r_kv[t]])
                        bank = proj(1792, 512)
                        S.op("act", lambda e: e.activation(out=Vb.t[:, t, :, 0:64], in_=bank.t[:, 0:512].rearrange("p (h c) -> p h c", h=8), func=AF.Copy), reads=[bank.r], writes=[r_kv[t]])

                    kvc = [r_kv[4 * c + u] for u in range(4)]
                    S.op("dve", lambda e: e.tensor_reduce(out=kms.t[:, :, 2 * c:2 * c + 2], in_=kT_b.t[:, :, c * 512:(c + 1) * 512].rearrange("p i (b k) -> p i b k", b=2), axis=AX.X, op=ALU.add), reads=kvc, writes=[kms.r])
                    S.op("act", lambda e: e.activation(out=kmT.t[:, :, 2 * c:2 * c + 2], in_=kms.t[:, :, 2 * c:2 * c + 2], func=AF.Copy, scale=1.0 / 256.0), reads=[kms.r], writes=[kmT.r])
                    if c >= 2:
                        for u in range(4):
                            for h in range(8):
                                i, ph = h // 2, (h % 2) * 64
                                S.op("pe", lambda e: e.matmul(pb[7].t[:, (u * 8 + h) * 8:(u * 8 + h) * 8 + 8], lhsT=qb_c.t[ph:ph + 64, i, u * 128:(u + 1) * 128], rhs=kmT.t[ph:ph + 64, i, 0:8], start=True, stop=True), reads=[qb_c.r, kmT.r], writes=[pb[7].r], pe_order=True)
                        for u in range(4):
                            j = 2 * c + u // 2
                            S.op("dve", lambda e: e.tensor_tensor(out=gm.t[:], in0=pb[7].t[:, u * 64:(u + 1) * 64], in1=elig.t[:, j - 4, :], op=ALU.add), reads=[pb[7].r, elig.r], writes=[gm.r])
                            for h in range(8):
                                S.op("dve", lambda e: e.max(out=top.t[:, h, :], in_=gm.t[:, h * 8:(h + 1) * 8]), reads=[gm.r], writes=[top.r])
                            S.op("dve", lambda e: e.tensor_tensor(out=sel_c.t[:, u, :].rearrange("p (h b) -> p h b", h=8), in0=gm.t[:].rearrange("p (h b) -> p h b", h=8), in1=top.t[:, :, 2:3].broadcast_to([128, 8, 8]), op=ALU.is_ge), reads=[gm.r, top.r], writes=[sel_c.r])

                    for u in range(4):
                        qt = 4 * c + u
                        for g in range(2):
                            ph = g * 64
                            pt_prev = None
                            if qt >= 1:
                                bank = nb()
                                S.op("pe", lambda e: e.matmul(bank.t[:, 0:512], lhsT=kT_a.t[ph:ph + 64, (qt - 1) * 128:qt * 128], rhs=qa_c.t[ph:ph + 64, u, :], start=True, stop=False), reads=[r_kv[qt - 1], qa_c.r], writes=[bank.r])
                                S.op("pe", lambda e: e.matmul(bank.t[:, 0:512], lhsT=ident_b.t[:], rhs=atri4.t[:], start=False, stop=True), reads=[ident_b.r, atri4.r], writes=[bank.r])
                                pt_prev = npt()
                                S.op("act", lambda e: e.activation(out=pt_prev.t[:], in_=bank.t[:, 0:512], func=AF.Exp, scale=0.125), reads=[bank.r], writes=[pt_prev.r])
                            bank = nb()
                            S.op("pe", lambda e: e.matmul(bank.t[:, 0:512], lhsT=kT_a.t[ph:ph + 64, qt * 128:(qt + 1) * 128], rhs=qa_c.t[ph:ph + 64, u, :], start=True, stop=False), reads=[r_kv[qt], qa_c.r], writes=[bank.r])
                            S.op("pe", lambda e: e.matmul(bank.t[:, 0:512], lhsT=ident_b.t[:], rhs=tri4.t[:], start=False, stop=True), reads=[ident_b.r, tri4.r], writes=[bank.r])
                            pt_cur = npt()
                            S.op("act", lambda e: e.activation(out=pt_cur.t[:], in_=bank.t[:, 0:512], func=AF.Exp, scale=0.125), reads=[bank.r], writes=[pt_cur.r])
                            ob = nob()
                            for hh in range(4):
                                reg = ob.t[:, hh * 65:(hh + 1) * 65]
                                if pt_prev is not None:
                                    S.op("pe", lambda e: e.matmul(reg, lhsT=pt_prev.t[:, hh * 128:(hh + 1) * 128], rhs=Va.t[:, qt - 1, g, :], start=True, stop=False), reads=[pt_prev.r, r_kv[qt - 1]], writes=[ob.r])
                                S.op("pe", lambda e: e.matmul(reg, lhsT=pt_cur.t[:, hh * 128:(hh + 1) * 128], rhs=Va.t[:, qt, g, :], start=(pt_prev is None), stop=True), reads=[pt_cur.r, r_kv[qt]], writes=[ob.r])
                            ov = ob.t[:, 0:260].rearrange("p (h c) -> p h c", h=4)
                            S.op("dve", lambda e: e.tensor_tensor(out=den.t[:].unsqueeze(2), in0=ov[:, :, 64:65], in1=expsink.t[:, 4 * g:4 * g + 4].unsqueeze(2), op=ALU.add), reads=[ob.r, expsink.r], writes=[den.r])
                            S.op("dve", lambda e: e.reciprocal(out=rden.t[:], in_=den.t[:]), reads=[den.r], writes=[rden.r])
                            S.op("dve", lambda e: e.tensor_tensor(out=o_c.t[:, u, g * 256:(g + 1) * 256].rearrange("p (h c) -> p h c", h=4), in0=ov[:, :, 0:64], in1=rden.t[:].unsqueeze(2).broadcast_to([128, 4, 64]), op=ALU.mult), reads=[ob.r, rden.r], writes=[o_c.r])

                    for h in range(8):
                        i, ph = h // 2, (h % 2) * 64
                        acc = accs[h % 2]
                        for blk in range(2 * c + 2):
                            ptk = {}
                            for kt in (2 * blk, 2 * blk + 1):
                                r = kt - 4 * c
                                q0 = max(r, 0) * 128
                                n = 512 - q0
                                bank = nb()
                                S.op("pe", lambda e: e.matmul(bank.t[:, 0:n], lhsT=kT_b.t[ph:ph + 64, i, kt * 128:(kt + 1) * 128], rhs=qb_c.t[ph:ph + 64, i, q0:512], start=True, stop=(r < 0)), reads=[r_kv[kt], qb_c.r], writes=[bank.r])
                                if r >= 0:
                                    S.op("pe", lambda e: e.matmul(bank.t[:, 0:128], lhsT=ident_b.t[:], rhs=tri4.t[:, 0:128], start=False, stop=True), reads=[ident_b.r, tri4.r], writes=[bank.r])
                                p = npt()
                                S.op("act", lambda e: e.activation(out=p.t[:, q0:512], in_=bank.t[:, 0:n], func=AF.Exp, scale=0.125), reads=[bank.r], writes=[p.r])
                                ptk[kt] = p
                            ob = nob()
                            us = []
                            for u in range(4):
                                kts = [kt for kt in (2 * blk, 2 * blk + 1) if kt <= 4 * c + u]
                                if not kts:
                                    continue
                                us.append(u)
                                for ki, kt in enumerate(kts):
                                    S.op("pe", lambda e: e.matmul(ob.t[:, u * 65:(u + 1) * 65], lhsT=ptk[kt].t[:, u * 128:(u + 1) * 128], rhs=Vb.t[:, kt, h, :], start=(ki == 0), stop=(ki == len(kts) - 1)), reads=[ptk[kt].r, r_kv[kt]], writes=[ob.r])
                            first = (blk == 0)
                            need_sel = [(blk != 2 * c + u // 2) and (2 * c + u // 2 >= 4) for u in us]
                            if not any(need_sel):
                                u0, u1 = us[0], us[-1] + 1
                                if first:
                                    S.op("dve", lambda e: e.tensor_copy(out=acc.t[:, u0:u1, :], in_=ob.t[:, u0 * 65:u1 * 65].rearrange("p (u c) -> p u c", c=65)), reads=[ob.r], writes=[acc.r])
                                else:
                                    S.op("dve", lambda e: e.tensor_tensor(out=acc.t[:, u0:u1, :], in0=ob.t[:, u0 * 65:u1 * 65].rearrange("p (u c) -> p u c", c=65), in1=acc.t[:, u0:u1, :], op=ALU.add), reads=[ob.r, acc.r], writes=[acc.r])
                            else:
                                for u, ns in zip(us, need_sel):
                                    reg = ob.t[:, u * 65:(u + 1) * 65]
                                    sc = sel_c.t[:, u, h * 8 + blk:h * 8 + blk + 1]
                                    if ns and first:
                                        S.op("dve", lambda e: e.tensor_scalar(out=acc.t[:, u, :], in0=reg, scalar1=sc, scalar2=None, op0=ALU.mult), reads=[ob.r, sel_c.r], writes=[acc.r])
                                    elif ns:
                                        S.op("dve", lambda e: e.scalar_tensor_tensor(out=acc.t[:, u, :], in0=reg, scalar=sc, in1=acc.t[:, u, :], op0=ALU.mult, op1=ALU.add), reads=[ob.r, sel_c.r, acc.r], writes=[acc.r])
                                    elif first:
                                        S.op("dve", lambda e: e.tensor_copy(out=acc.t[:, u, :], in_=reg), reads=[ob.r], writes=[acc.r])
                                    else:
                                        S.op("dve", lambda e: e.tensor_tensor(out=acc.t[:, u, :], in0=reg, in1=acc.t[:, u, :], op=ALU.add), reads=[ob.r, acc.r], writes=[acc.r])
                        S.op("dve", lambda e: e.reciprocal(out=rden.t[:].unsqueeze(2), in_=acc.t[:, :, 64:65]), reads=[acc.r], writes=[rden.r])
                        S.op("dve", lambda e: e.tensor_tensor(out=o_c.t[:, :, 512 + h * 64:512 + (h + 1) * 64], in0=acc.t[:, :, 0:64], in1=rden.t[:].unsqueeze(2).broadcast_to([128, 4, 64]), op=ALU.mult), reads=[acc.r, rden.r], writes=[o_c.r])

                    for u in range(4):
                        t = 4 * c + u
                        T = s * 16 + t
                        pp = T % 2
                        xr, x1, x1b = xres[0], x1s[0], x1bs[pp]
                        S.dma("sp", lambda e: e.dma_start(out=xr.t[:], in_=x_d[T * 128:(T + 1) * 128, :]), "xres0", writes=[xr.r])
                        for k in range(8):
                            S.op("pe", lambda e: e.transpose(out=pb[6].t[:, k * 128:(k + 1) * 128], in_=o_c.t[:, u, k * 128:(k + 1) * 128], identity=ident_b.t[:]), reads=[o_c.r, ident_b.r], writes=[pb[6].r])
                        S.op("act", lambda e: e.activation(out=oT.t[:], in_=pb[6].t[:], func=AF.Copy), reads=[pb[6].r], writes=[oT.r])
                        for hf in range(2):
                            bank = nb()
                            for k in range(8):
                                S.op("pe", lambda e: e.matmul(bank.t[:, 0:512], lhsT=oT.t[:, k * 128:(k + 1) * 128], rhs=w_out.t[:, k, hf * 512:(hf + 1) * 512], start=(k == 0), stop=False), reads=[oT.r, w_out.r], writes=[bank.r])
                            S.op("pe", lambda e: e.matmul(bank.t[:, 0:512], lhsT=ones2.t[:, :], rhs=bout2.t[:, hf * 512:(hf + 1) * 512], start=False, stop=True), reads=[ones2.r, bout2.r], writes=[bank.r])
                            S.op("dve", lambda e: e.scalar_tensor_tensor(out=yln.t[:, hf * 512:(hf + 1) * 512], in0=xr.t[:, hf * 512:(hf + 1) * 512], scalar=ALPHA, in1=bank.t[:, 0:512], op0=ALU.mult, op1=ALU.add), reads=[xr.r, bank.r], writes=[yln.r])
                        layer_norm(lnb, yln, ln1g, ln1b, x1)
                        S.dma("sp", lambda e: e.dma_start(out=x1buf[T * 128:(T + 1) * 128, :], in_=x1.t[:]), "x1st0", reads=[x1.r], writes=[r_x1buf])
                        S.op("act", lambda e: e.activation(out=x1b.t[:], in_=x1.t[:], func=AF.Copy), reads=[x1.r], writes=[x1b.r])
                        for rr in range(2):
                            for k in range(4):
                                kk = rr * 4 + k
                                S.op("pe", lambda e: e.transpose(out=pb[7].t[:, k * 128:(k + 1) * 128], in_=x1.t[:, kk * 128:(kk + 1) * 128], identity=ident_f.t[:]), reads=[x1.r, ident_f.r], writes=[pb[7].r])
                            S.op("act", lambda e: e.activation(out=x1T.t[:, rr * 512:(rr + 1) * 512], in_=pb[7].t[:, 0:512], func=AF.Copy), reads=[pb[7].r], writes=[x1T.r])
                        for k in range(8):
                            S.op("pe", lambda e: e.matmul(pb[7].t[:, 0:NE], lhsT=x1T.t[:, k * 128:(k + 1) * 128], rhs=w_r.t[:, k, :], start=(k == 0), stop=(k == 7)), reads=[x1T.r, w_r.r], writes=[pb[7].r])
                        S.op("dve", lambda e: e.tensor_tensor(out=lg.t[:], in0=pb[7].t[:, 0:NE], in1=brB.t[:], op=ALU.add), reads=[pb[7].r, brB.r], writes=[lg.r])
                        S.op("dve", lambda e: e.max(out=top8.t[:], in_=lg.t[:]), reads=[lg.r], writes=[top8.r])
                        S.op("dve", lambda e: e.tensor_scalar(out=ntop.t[:], in0=top8.t[:, 0:1], scalar1=-1.0, scalar2=None, op0=ALU.mult), reads=[top8.r], writes=[ntop.r])
                        S.op("act", lambda e: e.activation(out=gex.t[:], in_=top8.t[:, 0:4], func=AF.Exp, bias=ntop.t[:, 0:1], scale=1.0, accum_out=gsum.t[:, 0:1]), reads=[top8.r, ntop.r], writes=[gex.r, gsum.r])
                        S.op("dve", lambda e: e.reciprocal(out=gsum.t[:], in_=gsum.t[:]), reads=[gsum.r], writes=[gsum.r])
                        S.op("dve", lambda e: e.tensor_scalar(out=gates.t[:, T, :], in0=gex.t[:], scalar1=gsum.t[:, 0:1], scalar2=None, op0=ALU.mult), reads=[gex.r, gsum.r], writes=[gates.r])
                        S.op("dve", lambda e: e.tensor_scalar(out=Mb.t[:], in0=lg.t[:], scalar1=top8.t[:, 3:4], scalar2=None, op0=ALU.is_ge), reads=[lg.r, top8.r], writes=[Mb.r])
                        S.op("pe", lambda e: e.matmul(pb[7].t[:, 64:64 + NE], lhsT=Lst.t[:], rhs=Mb.t[:], start=True, stop=True), reads=[Lst.r, Mb.r], writes=[pb[7].r])
                        S.op("pe", lambda e: e.matmul(pb[7].t[:, 128:128 + NE], lhsT=onesm.t[:], rhs=Mb.t[:], start=True, stop=True), reads=[onesm.r, Mb.r], writes=[pb[7].r])
                        S.op("dve", lambda e: e.tensor_tensor(out=posD.t[:], in0=pb[7].t[:, 64:64 + NE], in1=carryD.t[:], op=ALU.add), reads=[pb[7].r, carryD.r], writes=[posD.r])
                        S.op("dve", lambda e: e.tensor_tensor(out=carryD.t[:], in0=pb[7].t[:, 128:128 + NE], in1=carryD.t[:], op=ALU.add), reads=[pb[7].r, carryD.r], writes=[carryD.r])
                        for k in range(4):
                            S.op("dve", lambda e: e.scalar_tensor_tensor(out=junk.t[:], in0=lg.t[:], scalar=top8.t[:, k:k + 1], in1=posD.t[:], op0=ALU.is_equal, op1=ALU.mult, accum_out=destf.t[:, k:k + 1]), reads=[lg.r, top8.r, posD.r], writes=[junk.r, destf.r])
                        S.op("dve", lambda e: e.tensor_copy(out=dest.t[:, T, :], in_=destf.t[:]), reads=[destf.r], writes=[dest.r])
                        for k in range(4):
                            S.dma("pool", lambda e: e.indirect_dma_start(out=xbuf, out_offset=bass.IndirectOffsetOnAxis(ap=dest.t[:, T, k:k + 1], axis=0), in_=x1b.t[:], in_offset=None), "x1b%d" % pp, reads=[x1b.r, dest.r], writes=[r_xbuf])
            S.barrier()

        if dbg == "A":
            S.finish("sp", [r_x1buf])
            S.barrier()
            return nc

        with ExitStack() as st:
            b1g = sb(st, [128, NE, 8], F32, "b1g")
            b1l = sb(st, [128, NE, 8], F32, "b1l")
            S.dma("sp", lambda e: e.dma_start(out=b1g.t[:], in_=b1g_d), "b1g", writes=[b1g.r])
            S.dma("sp", lambda e: e.dma_start(out=b1l.t[:], in_=b1l_d), "b1l", writes=[b1l.r])
            S.op("dve", lambda e: e.tensor_scalar(out=b1g.t[:], in0=b1g.t[:], scalar1=1.702, scalar2=None, op0=ALU.mult), reads=[b1g.r], writes=[b1g.r])
            S.op("dve", lambda e: e.tensor_scalar(out=b1l.t[:], in0=b1l.t[:], scalar1=7.0, scalar2=SINV, op0=ALU.add, op1=ALU.mult), reads=[b1l.r], writes=[b1l.r])
            wg = [sb(st, [128, 8, D], BF16, "wg") for _ in range(2)]
            wl = [sb(st, [128, 8, D], BF16, "wl") for _ in range(2)]
            w2 = [sb(st, [128, 8, D], BF16, "w2") for _ in range(2)]
            b2B = [sb(st, [128, D], F32, "b2B") for _ in range(2)]
            xeT = [sb(st, [128, 8, C], BF16, "xeT") for _ in range(2)]
            xss = [sb(st, [128, D], BF16, "xs") for _ in range(4)]
            actT = [sb(st, [128, 8, 512], BF16, "actT") for _ in range(2)]
            glu = [sb(st, [128, 512], F32, "glu") for _ in range(2)]
            sg = [sb(st, [128, 512], F32, "sg") for _ in range(2)]
            la = [sb(st, [128, 512], F32, "la") for _ in range(2)]
            ysb = [sb(st, [128, D], F32, "ysb") for _ in range(2)]
            xsc = [0]
            ysc = [0]
            hc = [0]
            chunks = []
            q0 = 0
            while q0 < C:
                n = min(512, C - q0)
                chunks.append((q0, n))
                q0 += n

            def load_w(e_):
                pp = e_ % 2
                for (wb, wd, nm) in ((wg[pp], w1g_d, "wg"), (wl[pp], w1l_d, "wl"), (w2[pp], w2_d, "w2")):
                    src = wd[e_].rearrange("(k p) f -> p k f", p=128)
                    for k in range(8):
                        S.dma("pool", lambda e: e.dma_start(out=wb.t[:, k, :], in_=src[:, k, :]), "%s%d" % (nm, pp), writes=[wb.r])
                S.dma("sp", lambda e: e.dma_start(out=b2B[pp].t[:], in_=b2_d[e_].partition_broadcast(128)), "b2B%d" % pp, writes=[b2B[pp].r])

            def prep_x(e_):
                pp = e_ % 2
                for s_ in range(NS):
                    xs = xss[xsc[0] % 4]
                    xsc[0] += 1
                    row0 = e_ * C + s_ * 128
                    S.dma("sp", lambda e: e.dma_start(out=xs.t[:], in_=xbuf[row0:row0 + 128, :]), "xs%d" % ((xsc[0] - 1) % 4), reads=[r_xbuf], writes=[xs.r])
                    for k in range(8):
                        S.op("pe", lambda e: e.transpose(out=pb[5].t[:, k * 128:(k + 1) * 128], in_=xs.t[:, k * 128:(k + 1) * 128], identity=ident_b.t[:]), reads=[xs.r, ident_b.r], writes=[pb[5].r])
                    S.op("act", lambda e: e.activation(out=xeT[pp].t[:, :, s_ * 128:(s_ + 1) * 128], in_=pb[5].t[:].rearrange("p (k q) -> p k q", k=8), func=AF.Copy), reads=[pb[5].r], writes=[xeT[pp].r])

            load_w(0)
            prep_x(0)
            for e_ in range(NE):
                pp = e_ % 2
                if e_ + 1 < NE:
                    load_w(e_ + 1)
                for ci, (q0, n) in enumerate(chunks):
                    aT = actT[ci % 2]
                    for j in range(8):
                        hb = hc[0] % 2
                        hc[0] += 1
                        bg, bl = pb[hb * 2], pb[hb * 2 + 1]
                        for k in range(8):
                            S.op("pe", lambda e: e.matmul(bg.t[:, 0:n], lhsT=wg[pp].t[:, k, j * 128:(j + 1) * 128], rhs=xeT[pp].t[:, k, q0:q0 + n], start=(k == 0), stop=(k == 7)), reads=[wg[pp].r, xeT[pp].r], writes=[bg.r])
                        for k in range(8):
                            S.op("pe", lambda e: e.matmul(bl.t[:, 0:n], lhsT=wl[pp].t[:, k, j * 128:(j + 1) * 128], rhs=xeT[pp].t[:, k, q0:q0 + n], start=(k == 0), stop=(k == 7)), reads=[wl[pp].r, xeT[pp].r], writes=[bl.r])
                        S.op("act", lambda e: e.activation(out=glu[hb].t[:, 0:n], in_=bg.t[:, 0:n], func=AF.Silu, scale=1.702, bias=b1g.t[:, e_, j:j + 1]), reads=[bg.r, b1g.r], writes=[glu[hb].r])
                        S.op("act", lambda e: e.activation(out=la[hb].t[:, 0:n], in_=bl.t[:, 0:n], func=AF.Relu, scale=SINV, bias=b1l.t[:, e_, j:j + 1]), reads=[bl.r, b1l.r], writes=[la[hb].r])
                        S.op("dve", lambda e: e.tensor_scalar(out=sg[hb].t[:, 0:n], in0=la[hb].t[:, 0:n], scalar1=14.0 * SINV, scalar2=-6.0 * SINV, op0=ALU.min, op1=ALU.add), reads=[la[hb].r], writes=[sg[hb].r])
                        S.op("dve", lambda e: e.scalar_tensor_tensor(out=aT.t[:, j, 0:n], in0=glu[hb].t[:, 0:n], scalar=UMAX, in1=sg[hb].t[:, 0:n], op0=ALU.min, op1=ALU.mult), reads=[glu[hb].r, sg[hb].r], writes=[aT.r])
                    for sl in range(n // 128):
                        yb = ysb[ysc[0] % 2]
                        yi = ysc[0] % 2
                        ysc[0] += 1
                        for hf in range(2):
                            bank = pb[4] if hf == 0 else pb[7]
                            for j in range(8):
                                S.op("pe", lambda e: e.matmul(bank.t[:, 0:512], lhsT=aT.t[:, j, sl * 128:(sl + 1) * 128], rhs=w2[pp].t[:, j, hf * 512:(hf + 1) * 512], start=(j == 0), stop=(j == 7)), reads=[aT.r, w2[pp].r], writes=[bank.r])
                            S.op("dve", lambda e: e.tensor_tensor(out=yb.t[:, hf * 512:(hf + 1) * 512], in0=bank.t[:, 0:512], in1=b2B[pp].t[:, hf * 512:(hf + 1) * 512], op=ALU.add), reads=[bank.r, b2B[pp].r], writes=[yb.r])
                        row0 = e_ * C + q0 + sl * 128
                        S.dma("sp", lambda e: e.dma_start(out=ybuf[row0:row0 + 128, :], in_=yb.t[:]), "ysb%d" % yi, reads=[yb.r], writes=[r_ybuf])
                if e_ + 1 < NE:
                    prep_x(e_ + 1)
            S.barrier()

        with ExitStack() as st:
            ln2g = sb(st, [128, D], F32, "ln2g")
            ln2b = sb(st, [128, D], F32, "ln2b")
            S.dma("sp", lambda e: e.dma_start(out=ln2g.t[:], in_=ln2g_d[0].partition_broadcast(128)), "ln2g", writes=[ln2g.r])
            S.dma("sp", lambda e: e.dma_start(out=ln2b.t[:], in_=ln2b_d[0].partition_broadcast(128)), "ln2b", writes=[ln2b.r])
            ygs = [[sb(st, [128, D], F32, "yg") for _ in range(4)] for _ in range(3)]
            x1r = [sb(st, [128, D], F32, "x1r") for _ in range(3)]
            accC = [sb(st, [128, D], F32, "accC") for _ in range(3)]
            outb = [sb(st, [128, D], F32, "outb") for _ in range(3)]
            lnb2 = (sb(st, [128, 2, 6], F32, "stats2"), sb(st, [128, 2], F32, "mv2"), sb(st, [128, 1], F32, "lnv2"),
                    sb(st, [128, 1], F32, "rstd2"), sb(st, [128, 1], F32, "nmr2"), sb(st, [128, D], F32, "xn2"))
            for T in range(NT):
                pp = T % 3
                S.dma("sp", lambda e: e.dma_start(out=x1r[pp].t[:], in_=x1buf[T * 128:(T + 1) * 128, :]), "x1r%d" % pp, reads=[r_x1buf], writes=[x1r[pp].r])
                for k in range(4):
                    yg = ygs[pp][k]
                    S.dma("pool", lambda e: e.indirect_dma_start(out=yg.t[:], out_offset=None, in_=ybuf, in_offset=bass.IndirectOffsetOnAxis(ap=dest.t[:, T, k:k + 1], axis=0)), "yg%d_%d" % (pp, k), reads=[r_ybuf, dest.r], writes=[yg.r])
                a = accC[pp]
                S.op("act", lambda e: e.activation(out=a.t[:], in_=x1r[pp].t[:], func=AF.Copy, scale=ALPHA), reads=[x1r[pp].r], writes=[a.r])
                for k in range(4):
                    yg = ygs[pp][k]
                    S.op("dve", lambda e: e.scalar_tensor_tensor(out=a.t[:], in0=yg.t[:], scalar=gates.t[:, T, k:k + 1], in1=a.t[:], op0=ALU.mult, op1=ALU.add), reads=[yg.r, gates.r, a.r], writes=[a.r])
                layer_norm(lnb2, a, ln2g, ln2b, outb[pp])
                S.dma("sp", lambda e: e.dma_start(out=out_d[T * 128:(T + 1) * 128, :], in_=outb[pp].t[:]), "outb%d" % pp, reads=[outb[pp].r], writes=[r_out])
            S.finish("sp", [r_out])
            S.barrier()
    return nc


def rope_tables():
    inv = 1.0 / (10000.0 ** (np.arange(0, 64, 2, dtype=np.float32) / 64.0))
    ang = np.arange(SEQ, dtype=np.float32)[:, None] * inv[None, :].astype(np.float32)
    cos = np.cos(ang).astype(np.float32)
    sin = np.sin(ang).astype(np.float32)
    return np.concatenate([cos, cos], 1), np.concatenate([-sin, sin], 1)


def prep_weights(w_in, b_in, sinks, w_out, b_out, ln1_g, ln1_b, w_router, b_router, w1, b1, w2, b2, ln2_g, ln2_b):
    f = lambda a: np.ascontiguousarray(np.asarray(a, dtype=np.float32))
    rc, rs = rope_tables()
    w1 = np.asarray(w1)[0]
    b1 = np.asarray(b1)[0]
    m = {
        "w_in": f(w_in[0]), "b_in": f(b_in[0]).reshape(1, -1), "sinks": f(sinks[0]).reshape(1, -1),
        "w_out": f(w_out[0]), "b_out": f(b_out[0]).reshape(1, -1),
        "ln1_g": f(ln1_g[0]).reshape(1, -1), "ln1_b": f(ln1_b[0]).reshape(1, -1),
        "w_router": f(np.asarray(w_router[0]).reshape(8, 128, NE).transpose(1, 0, 2)), "b_router": f(b_router[0]).reshape(1, -1),
        "w1g": f(w1[:, :, 0::2]), "w1l": f(w1[:, :, 1::2]),
        "b1g": f(b1[:, 0::2].reshape(NE, 8, 128).transpose(2, 0, 1)),
        "b1l": f(b1[:, 1::2].reshape(NE, 8, 128).transpose(2, 0, 1)),
        "w2": f(w2[0]), "b2": f(b2[0]),
        "ln2_g": f(ln2_g[0]).reshape(1, -1), "ln2_b": f(ln2_b[0]).reshape(1, -1),
        "ropeC": f(rc.reshape(16, 128, 64).transpose(1, 0, 2)), "ropeS": f(rs.reshape(16, 128, 64).transpose(1, 0, 2)),
    }
    return m


def kernel(x, w_in, b_in, sinks, w_out, b_out, ln1_g, ln1_b, w_router, b_router, w1, b1, w2, b2, ln2_g, ln2_b):
    x = np.asarray(x, dtype=np.float32)
    B = x.shape[0]
    nseq = B // N_CORES
    wm = prep_weights(w_in, b_in, sinks, w_out, b_out, ln1_g, ln1_b, w_router, b_router, w1, b1, w2, b2, ln2_g, ln2_b)
    nc = build(nseq, 1536)
    in_maps = []
    for c in range(N_CORES):
        m = dict(wm)
        m["x"] = np.ascontiguousarray(x[c * nseq:(c + 1) * nseq].reshape(nseq * SEQ, D))
        in_maps.append(m)
    res = run_bass_kernel_spmd(nc, in_maps, core_ids=list(range(N_CORES)))
    out = np.concatenate([r["out"].reshape(nseq, SEQ, D) for r in res.results], axis=0)
    return out.astype(np.float32)
```

```python
import numpy as np
from contextlib import ExitStack
import concourse.bass as bass
import concourse.mybir as mybir
from concourse.bass_utils import run_bass_kernel_spmd

F32 = mybir.dt.float32
BF16 = mybir.dt.bfloat16
U32 = mybir.dt.uint32
I32 = mybir.dt.int32
AF = mybir.ActivationFunctionType
ALU = mybir.AluOpType
AX = mybir.AxisListType

D = 1024
SEQ = 2048
NE = 32
ALPHA = float(2.0 ** 0.25)
EPS = 1e-5
NEG = -30000.0
SINV = float(1.0 / 1.702)
UMAX = float(11.914 / (1.0 + np.exp(-11.914)))
N_CORES = 8


class Res:
    __slots__ = ("name", "w", "r", "multi", "excl")

    def __init__(self, name, multi=False, excl=False):
        self.excl = excl
        self.name = name
        self.w = {}
        self.r = {}
        self.multi = multi


class Buf:
    __slots__ = ("t", "r")

    def __init__(self, t, name):
        self.t = t
        self.r = Res(name)


class Sched:
    def __init__(self, nc, stack):
        self.nc = nc
        self.stack = stack
        self.eng = {"pe": nc.tensor, "act": nc.scalar, "dve": nc.vector, "pool": nc.gpsimd, "sp": nc.sync}
        self.sem = {}
        self.cnt = {}
        self.waited = {k: {} for k in self.eng}
        for k in self.eng:
            self.sem[k] = stack.enter_context(nc.semaphore("s_" + k))
            self.cnt[k] = 0
        self.dsem = {}
        self.dcnt = {}
        self.nwait = 0
        self.nins = 0

    def _wait(self, e, deps):
        for key, (sh, val) in deps.items():
            if self.waited[e].get(key, 0) >= val:
                continue
            self.eng[e].wait_ge(sh, val)
            self.waited[e][key] = val
            self.nwait += 1

    def _deps(self, e, reads, writes, pe_order=False):
        deps = {}

        def add(key, sh, val):
            if key == "pe" and e == "pe" and not pe_order:
                return
            if key not in deps or deps[key][1] < val:
                deps[key] = (sh, val)

        for r in reads:
            for key, (sh, val) in r.w.items():
                add(key, sh, val)
            if r.excl:
                for key, (sh, val) in r.r.items():
                    if key != e:
                        add(key, sh, val)
        for w in writes:
            if not w.multi:
                for key, (sh, val) in w.w.items():
                    add(key, sh, val)
            for key, (sh, val) in w.r.items():
                add(key, sh, val)
        return deps

    def _record(self, key, sh, val, reads, writes):
        for r in reads:
            r.r[key] = (sh, val)
        for w in writes:
            if w.multi:
                w.w[key] = (sh, val)
            else:
                w.w = {key: (sh, val)}
                w.r = {}

    def op(self, e, fn, reads=(), writes=(), pe_order=False):
        deps = self._deps(e, reads, writes, pe_order)
        self._wait(e, deps)
        ins = fn(self.eng[e])
        self.cnt[e] += 1
        self.nins += 1
        ins.then_inc(self.sem[e], 1)
        self._record(e, self.sem[e], self.cnt[e], reads, writes)
        return ins

    def dma(self, q, fn, dname, reads=(), writes=()):
        if dname not in self.dsem:
            self.dsem[dname] = self.stack.enter_context(self.nc.semaphore("d_" + dname))
            self.dcnt[dname] = 0
        key = "d_" + dname
        deps = self._deps(q, reads, writes)
        deps.pop(key, None)
        self._wait(q, deps)
        ins = fn(self.eng[q])
        self.dcnt[dname] += 16
        self.nins += 1
        ins.then_inc(self.dsem[dname], 16)
        self._record(key, self.dsem[dname], self.dcnt[dname], reads, writes)
        return ins

    def barrier(self):
        allev = {}
        for k in self.eng:
            if self.cnt[k] > 0:
                allev[k] = (self.sem[k], self.cnt[k])
        for dn in self.dsem:
            if self.dcnt[dn] > 0:
                allev["d_" + dn] = (self.dsem[dn], self.dcnt[dn])
        for e in self.eng:
            deps = {k: v for k, v in allev.items() if k != e}
            self._wait(e, deps)

    def finish(self, e, resources):
        deps = {}
        for r in resources:
            for key, (sh, val) in list(r.w.items()) + list(r.r.items()):
                if key not in deps or deps[key][1] < val:
                    deps[key] = (sh, val)
        self._wait(e, deps)


def build(NSEQ, C, dbg=False):
    NT = NSEQ * 16
    NTOK = NSEQ * SEQ
    NS = C // 128
    NROW = NE * C + 1024
    nc = bass.Bass("TRN2", target_bir_lowering=False)

    def din(name, shape, dt=F32):
        return nc.dram_tensor(name, shape, dt, kind="ExternalInput").ap()

    x_d = din("x", [NTOK, D])
    w_in_d = din("w_in", [D, 2304])
    b_in_d = din("b_in", [1, 2304])
    sinks_d = din("sinks", [1, 8])
    w_out_d = din("w_out", [D, D])
    b_out_d = din("b_out", [1, D])
    ln1g_d = din("ln1_g", [1, D])
    ln1b_d = din("ln1_b", [1, D])
    w_r_d = din("w_router", [128, 8, NE])
    b_r_d = din("b_router", [1, NE])
    w1g_d = din("w1g", [NE, D, D])
    w1l_d = din("w1l", [NE, D, D])
    b1g_d = din("b1g", [128, NE, 8])
    b1l_d = din("b1l", [128, NE, 8])
    w2_d = din("w2", [NE, D, D])
    b2_d = din("b2", [NE, D])
    ln2g_d = din("ln2_g", [1, D])
    ln2b_d = din("ln2_b", [1, D])
    ropeC_d = din("ropeC", [128, 16, 64])
    ropeS_d = din("ropeS", [128, 16, 64])
    out_d = nc.dram_tensor("out", [NTOK, D], F32, kind="ExternalOutput").ap()
    x1buf = nc.dram_tensor("x1buf", [NTOK, D], F32, kind="ExternalOutput" if dbg else "Internal").ap()
    xbuf = nc.dram_tensor("xbuf", [NROW, D], BF16, kind="Internal").ap()
    ybuf = nc.dram_tensor("ybuf", [NROW, D], F32, kind="Internal").ap()
    r_x1buf, r_xbuf, r_ybuf, r_out = Res("x1buf", True), Res("xbuf", True), Res("ybuf", True), Res("out", True)

    with ExitStack() as st0:
        S = Sched(nc, st0)
        uid = [0]

        def sb(st, shape, dt=F32, name="t"):
            uid[0] += 1
            nm = "%s_%d" % (name, uid[0])
            return Buf(st.enter_context(nc.sbuf_tensor(nm, shape, dt)), nm)

        pb = []
        for i in range(8):
            if i in (5, 6):
                t = st0.enter_context(nc.psum_tensor("pb%d" % i, [128, 1024], BF16))
            else:
                t = st0.enter_context(nc.psum_tensor("pb%d" % i, [128, 512], F32))
            pb.append(Buf(t, "pb%d" % i))
            pb[-1].r.excl = True
        bigc = [0]

        def nb():
            b = pb[bigc[0] % 3]
            bigc[0] += 1
            return b

        bigs = [0]

        def nbs():
            b = pb[(0, 1, 2, 7)[bigs[0] % 4]]
            bigs[0] += 1
            return b

        oc = [0]

        def nob():
            b = pb[3 + oc[0] % 2]
            oc[0] += 1
            return b

        gates = sb(st0, [128, NT, 4], F32, "gates")
        dest = sb(st0, [128, NT, 4], U32, "dest")
        ident_b = sb(st0, [128, 128], BF16, "identb")
        ident_f = sb(st0, [128, 128], F32, "identf")
        ones2 = sb(st0, [2, 128], BF16, "ones2")
        m10 = sb(st0, [2, 1], F32, "m10")

        S.op("pool", lambda e: e.memset(ident_f.t[:], 0.0), writes=[ident_f.r])
        S.op("pool", lambda e: e.affine_select(out=ident_f.t[:], in_=ident_f.t[:], pattern=[[-1, 128]], compare_op=ALU.not_equal, fill=1.0, base=0, channel_multiplier=1), reads=[ident_f.r], writes=[ident_f.r])
        S.op("dve", lambda e: e.tensor_copy(out=ident_b.t[:], in_=ident_f.t[:]), reads=[ident_f.r], writes=[ident_b.r])
        S.op("dve", lambda e: e.memset(ones2.t[:], 1.0), writes=[ones2.r])
        S.op("dve", lambda e: e.memset(m10.t[:], 0.0), writes=[m10.r])
        S.op("dve", lambda e: e.memset(m10.t[0:1, :], 1.0), reads=[m10.r], writes=[m10.r])

        def make_hilo(st, dst, src_row, n, tag):
            stg = sb(st, [2, n], F32, "hl_s" + tag)
            hb = sb(st, [2, n], BF16, "hl_b" + tag)
            hf = sb(st, [2, n], F32, "hl_f" + tag)
            lo = sb(st, [2, n], F32, "hl_l" + tag)
            S.dma("sp", lambda e: e.dma_start(out=stg.t[:], in_=src_row.partition_broadcast(2)), "hl" + tag, writes=[stg.r])
            hilo_compute(dst, stg, hb, hf, lo, n)

        def hilo_compute(dst, stg, hb, hf, lo, n):
            S.op("dve", lambda e: e.tensor_copy(out=hb.t[:, 0:n], in_=stg.t[:, 0:n]), reads=[stg.r], writes=[hb.r])
            S.op("dve", lambda e: e.tensor_copy(out=hf.t[:, 0:n], in_=hb.t[:, 0:n]), reads=[hb.r], writes=[hf.r])
            S.op("dve", lambda e: e.tensor_tensor(out=lo.t[:, 0:n], in0=stg.t[:, 0:n], in1=hf.t[:, 0:n], op=ALU.subtract), reads=[stg.r, hf.r], writes=[lo.r])
            S.op("dve", lambda e: e.tensor_tensor(out=hf.t[:, 0:n], in0=hf.t[:, 0:n], in1=lo.t[:, 0:n], op=ALU.subtract), reads=[hf.r, lo.r], writes=[hf.r])
            S.op("dve", lambda e: e.scalar_tensor_tensor(out=dst.t[:, 0:n], in0=hf.t[:, 0:n], scalar=m10.t[:, 0:1], in1=lo.t[:, 0:n], op0=ALU.mult, op1=ALU.add), reads=[hf.r, lo.r, m10.r], writes=[dst.r])

        def layer_norm(st_bufs, yln, gB, bB, outb, eng_gb="pool"):
            stats, mv, lnv, rstd, nmr, xn = st_bufs
            S.op("dve", lambda e: e.bn_stats(out=stats.t[:, 0, :], in_=yln.t[:, 0:512]), reads=[yln.r], writes=[stats.r])
            S.op("dve", lambda e: e.bn_stats(out=stats.t[:, 1, :], in_=yln.t[:, 512:1024]), reads=[yln.r], writes=[stats.r])
            S.op("dve", lambda e: e.bn_aggr(out=mv.t[:], in_=stats.t[:].rearrange("p a b -> p (a b)")), reads=[stats.r], writes=[mv.r])
            S.op("act", lambda e: e.activation(out=lnv.t[:], in_=mv.t[:, 1:2], func=AF.Ln, bias=EPS, scale=1.0), reads=[mv.r], writes=[lnv.r])
            S.op("act", lambda e: e.activation(out=rstd.t[:], in_=lnv.t[:], func=AF.Exp, scale=-0.5), reads=[lnv.r], writes=[rstd.r])
            S.op("dve", lambda e: e.tensor_scalar(out=nmr.t[:], in0=mv.t[:, 0:1], scalar1=-1.0, scalar2=rstd.t[:, 0:1], op0=ALU.mult, op1=ALU.mult), reads=[mv.r, rstd.r], writes=[nmr.r])
            S.op("act", lambda e: e.activation(out=xn.t[:], in_=yln.t[:], func=AF.Identity, scale=rstd.t[:, 0:1], bias=nmr.t[:, 0:1]), reads=[yln.r, rstd.r, nmr.r], writes=[xn.r])
            S.op(eng_gb, lambda e: e.tensor_tensor(out=xn.t[:], in0=xn.t[:], in1=gB.t[:], op=ALU.mult), reads=[xn.r, gB.r], writes=[xn.r])
            S.op(eng_gb, lambda e: e.tensor_tensor(out=outb.t[:], in0=xn.t[:], in1=bB.t[:], op=ALU.add), reads=[xn.r, bB.r], writes=[outb.r])

        with ExitStack() as st:
            w_in = sb(st, [128, 8, 2304], BF16, "w_in")
            w_out = sb(st, [128, 8, D], BF16, "w_out")
            w_r = sb(st, [128, 8, NE], F32, "w_r")
            bin2 = sb(st, [2, 2304], BF16, "bin2")
            bout2 = sb(st, [2, D], BF16, "bout2")
            ropeC = sb(st, [128, 16, 64], F32, "ropeC")
            ropeS = sb(st, [128, 16, 64], F32, "ropeS")
            ln1g = sb(st, [128, D], F32, "ln1g")
            ln1b = sb(st, [128, D], F32, "ln1b")
            expsink = sb(st, [128, 8], F32, "expsink")
            brB = sb(st, [128, NE], F32, "brB")
            carryD = sb(st, [128, NE], F32, "carryD")
            carryI = sb(st, [128, NE], I32, "carryI")
            tri4 = sb(st, [128, 512], BF16, "tri4")
            atri4 = sb(st, [128, 512], BF16, "atri4")
            Lst = sb(st, [128, 128], BF16, "Lst")
            onesm = sb(st, [128, 128], BF16, "onesm")
            elig = sb(st, [128, 4, 64], F32, "elig")
            stt = ExitStack()
            trif = sb(stt, [128, 512], F32, "trif")

            w_in_v = w_in_d.rearrange("(k p) c -> p k c", p=128)
            w_out_v = w_out_d.rearrange("(k p) c -> p k c", p=128)
            for k in range(8):
                for h2 in range(2):
                    S.dma("pool", lambda e: e.dma_start(out=w_in.t[:, k, h2 * 1152:(h2 + 1) * 1152], in_=w_in_v[:, k, h2 * 1152:(h2 + 1) * 1152]), "w_in", writes=[w_in.r])
                S.dma("pool", lambda e: e.dma_start(out=w_out.t[:, k, :], in_=w_out_v[:, k, :]), "w_out", writes=[w_out.r])
            S.dma("sp", lambda e: e.dma_start(out=w_r.t[:], in_=w_r_d), "w_r", writes=[w_r.r])
            S.dma("sp", lambda e: e.dma_start(out=ropeC.t[:], in_=ropeC_d), "ropeC", writes=[ropeC.r])
            S.dma("sp", lambda e: e.dma_start(out=ropeS.t[:], in_=ropeS_d), "ropeS", writes=[ropeS.r])
            S.dma("sp", lambda e: e.dma_start(out=ln1g.t[:], in_=ln1g_d[0].partition_broadcast(128)), "ln1g", writes=[ln1g.r])
            S.dma("sp", lambda e: e.dma_start(out=ln1b.t[:], in_=ln1b_d[0].partition_broadcast(128)), "ln1b", writes=[ln1b.r])
            S.dma("sp", lambda e: e.dma_start(out=expsink.t[:], in_=sinks_d[0].partition_broadcast(128)), "sinks", writes=[expsink.r])
            S.dma("sp", lambda e: e.dma_start(out=brB.t[:], in_=b_r_d[0].partition_broadcast(128)), "brB", writes=[brB.r])
            S.op("act", lambda e: e.activation(out=expsink.t[:], in_=expsink.t[:], func=AF.Exp), reads=[expsink.r], writes=[expsink.r])
            make_hilo(stt, bin2, b_in_d[0], 2304, "bin")
            make_hilo(stt, bout2, b_out_d[0], D, "bout")
            S.op("pool", lambda e: e.iota(carryI.t[:], pattern=[[C, NE]], base=0, channel_multiplier=0), writes=[carryI.r])
            S.op("dve", lambda e: e.tensor_copy(out=carryD.t[:], in_=carryI.t[:]), reads=[carryI.r], writes=[carryD.r])
            S.op("pool", lambda e: e.memset(trif.t[:], 0.0), writes=[trif.r])
            S.op("pool", lambda e: e.affine_select(out=trif.t[:], in_=trif.t[:], pattern=[[0, 4], [1, 128]], compare_op=ALU.is_ge, fill=NEG, base=0, channel_multiplier=-1), reads=[trif.r], writes=[trif.r])
            S.op("dve", lambda e: e.tensor_copy(out=tri4.t[:], in_=trif.t[:]), reads=[trif.r], writes=[tri4.r])
            S.op("pool", lambda e: e.memset(trif.t[:], 0.0), reads=[trif.r], writes=[trif.r])
            S.op("pool", lambda e: e.affine_select(out=trif.t[:], in_=trif.t[:], pattern=[[0, 4], [-1, 128]], compare_op=ALU.is_gt, fill=NEG, base=0, channel_multiplier=1), reads=[trif.r], writes=[trif.r])
            S.op("dve", lambda e: e.tensor_copy(out=atri4.t[:], in_=trif.t[:]), reads=[trif.r], writes=[atri4.r])
            S.op("pool", lambda e: e.memset(trif.t[:, 0:128], 1.0), reads=[trif.r], writes=[trif.r])
            S.op("pool", lambda e: e.affine_select(out=trif.t[:, 0:128], in_=trif.t[:, 0:128], pattern=[[1, 128]], compare_op=ALU.is_gt, fill=0.0, base=0, channel_multiplier=-1), reads=[trif.r], writes=[trif.r])
            S.op("dve", lambda e: e.tensor_copy(out=Lst.t[:], in_=trif.t[:, 0:128]), reads=[trif.r], writes=[Lst.r])
            S.op("dve", lambda e: e.memset(onesm.t[:], 1.0), writes=[onesm.r])
            S.op("dve", lambda e: e.memset(elig.t[:], 0.0), writes=[elig.r])
            for j in range(4, 8):
                S.op("dve", lambda e: e.memset(elig.t[:, j - 4, :].rearrange("p (h b) -> p h b", h=8)[:, :, j:8], -1e30), reads=[elig.r], writes=[elig.r])

            S.barrier()
            stt.close()
            kT_a = sb(st, [128, SEQ], BF16, "kT_a")
            kT_b = sb(st, [128, 4, SEQ], BF16, "kT_b")
            Va = sb(st, [128, 16, 2, 65], BF16, "Va")
            Vb = sb(st, [128, 16, 8, 65], BF16, "Vb")
            r_kv = [Res("kv%d" % t) for t in range(16)]
            S.op("pool", lambda e: e.memset(Va.t[:], 1.0), writes=r_kv)
            S.op("pool", lambda e: e.memset(Vb.t[:], 1.0), writes=r_kv)
            kms = sb(st, [128, 4, 8], F32, "kms")
            kmT = sb(st, [128, 4, 8], BF16, "kmT")
            S.op("dve", lambda e: e.memset(kms.t[:], 0.0), writes=[kms.r])
            S.op("dve", lambda e: e.memset(kmT.t[:], 0.0), writes=[kmT.r])
            xbs = [sb(st, [128, D], BF16, "xb") for _ in range(2)]
            xTs = [sb(st, [128, D], BF16, "xT") for _ in range(2)]
            m1s = [sb(st, [128, 512], F32, "m1") for _ in range(2)]
            m2s = [sb(st, [128, 512], F32, "m2") for _ in range(2)]
            rqa = [sb(st, [128, 512], BF16, "rqa") for _ in range(2)]
            rqb = [sb(st, [128, 512], BF16, "rqb") for _ in range(2)]
            rkb = [sb(st, [128, 512], BF16, "rkb") for _ in range(2)]
            rka = [sb(st, [128, 128], BF16, "rka") for _ in range(2)]
            qTa = [sb(st, [128, 4, 512], BF16, "qTa") for _ in range(1)]
            qTb = [sb(st, [128, 4, 512], BF16, "qTb") for _ in range(1)]
            sel = [sb(st, [128, 4, 64], F32, "sel") for _ in range(2)]
            gm = sb(st, [128, 64], F32, "gm")
            top = sb(st, [128, 8, 8], F32, "top")
            pts = [sb(st, [128, 512], BF16, "pt") for _ in range(6)]
            ptc = [0]
            den = sb(st, [128, 4], F32, "den")
            rden = sb(st, [128, 4], F32, "rden")
            accs = [sb(st, [128, 4, 65], F32, "acc") for _ in range(2)]
            o_t = [sb(st, [128, 4, D], BF16, "o_t") for _ in range(1)]
            oT = sb(st, [128, D], BF16, "oT")
            xres = [sb(st, [128, D], F32, "xres") for _ in range(1)]
            ylns = [sb(st, [128, D], F32, "yln") for _ in range(2)]
            lnb = (sb(st, [128, 2, 6], F32, "stats"), sb(st, [128, 2], F32, "mv"), sb(st, [128, 1], F32, "lnv"),
                   sb(st, [128, 1], F32, "rstd"), sb(st, [128, 1], F32, "nmr"), sb(st, [128, D], F32, "xn"))
            x1s = [sb(st, [128, D], F32, "x1") for _ in range(2)]
            x1bs = [sb(st, [128, D], BF16, "x1b") for _ in range(2)]
            x1T = sb(st, [128, D], F32, "x1T")
            lg = sb(st, [128, NE], F32, "lg")
            top8 = sb(st, [128, 8], F32, "top8")
            ntop = sb(st, [128, 1], F32, "ntop")
            gex = sb(st, [128, 4], F32, "gex")
            gsum = sb(st, [128, 1], F32, "gsum")
            Mb = sb(st, [128, NE], BF16, "Mb")
            posD = sb(st, [128, NE], F32, "posD")
            junk = sb(st, [128, NE], F32, "junk")
            destf = sb(st, [128, 4], F32, "destf")

            def npt():
                b = pts[ptc[0] % 6]
                ptc[0] += 1
                return b

            def rope(src_bank, H, t, m1, m2, out_view_fn):
                n = H * 64
                src = src_bank.t[:, 0:n].rearrange("p (h c) -> p h c", h=H)
                Cb = ropeC.t[:, t, :].unsqueeze(1).broadcast_to([128, H, 64])
                Sb1 = ropeS.t[:, t, 0:32].unsqueeze(1).broadcast_to([128, H, 32])
                Sb2 = ropeS.t[:, t, 32:64].unsqueeze(1).broadcast_to([128, H, 32])
                m1v = m1.t[:, 0:n].rearrange("p (h c) -> p h c", h=H)
                m2v = m2.t[:, 0:n].rearrange("p (h c) -> p h c", h=H)
                S.op("dve", lambda e: e.tensor_tensor(out=m1v, in0=src, in1=Cb, op=ALU.mult), reads=[src_bank.r, ropeC.r], writes=[m1.r])
                S.op("dve", lambda e: e.tensor_tensor(out=m2v[:, :, 0:32], in0=src[:, :, 32:64], in1=Sb1, op=ALU.mult), reads=[src_bank.r, ropeS.r], writes=[m2.r])
                S.op("dve", lambda e: e.tensor_tensor(out=m2v[:, :, 32:64], in0=src[:, :, 0:32], in1=Sb2, op=ALU.mult), reads=[src_bank.r, ropeS.r], writes=[m2.r])
                return m1v, m2v

            for s in range(NSEQ):
                for c in range(4):
                    cb = (s * 4 + c) % 2
                    qa_c, qb_c, sel_c, o_c = qTa[0], qTb[0], sel[cb], o_t[0]
                    for u in range(4):
                        t = 4 * c + u
                        T = s * 16 + t
                        pp = T % 2
                        xb, xT = xbs[pp], xTs[pp]
                        S.dma("pool", lambda e: e.dma_start(out=xb.t[:], in_=x_d[T * 128:(T + 1) * 128, :]), "xb%d" % pp, writes=[xb.r])
                        for k in range(8):
                            S.op("pe", lambda e: e.transpose(out=pb[5].t[:, k * 128:(k + 1) * 128], in_=xb.t[:, k * 128:(k + 1) * 128], identity=ident_b.t[:]), reads=[xb.r, ident_b.r], writes=[pb[5].r])
                        S.op("act", lambda e: e.activation(out=xT.t[:], in_=pb[5].t[:], func=AF.Copy), reads=[pb[5].r], writes=[xT.r])

                        def proj(col0, n):
                            bank = nb()
                            for k in range(8):
                                S.op("pe", lambda e: e.matmul(bank.t[:, 0:n], lhsT=xT.t[:, k * 128:(k + 1) * 128], rhs=w_in.t[:, k, col0:col0 + n], start=(k == 0), stop=False), reads=[xT.r, w_in.r], writes=[bank.r])
                            S.op("pe", lambda e: e.matmul(bank.t[:, 0:n], lhsT=ones2.t[:, :], rhs=bin2.t[:, col0:col0 + n], start=False, stop=True), reads=[ones2.r, bin2.r], writes=[bank.r])
                            return bank

                        bank = proj(0, 512)
                        m1v, m2v = rope(bank, 8, t, m1s[0], m2s[0], None)
                        ov = rqa[pp].t[:].rearrange("p (i two c) -> p two i c", i=4, two=2)
                        S.op("pool", lambda e: e.tensor_tensor(out=ov, in0=m1s[0].t[:].rearrange("p (two i c) -> p two i c", two=2, i=4), in1=m2s[0].t[:].rearrange("p (two i c) -> p two i c", two=2, i=4), op=ALU.add), reads=[m1s[0].r, m2s[0].r], writes=[rqa[pp].r])
                        bank = proj(768, 512)
                        rope(bank, 8, t, m1s[1], m2s[1], None)
                        S.op("pool", lambda e: e.tensor_tensor(out=rqb[pp].t[:], in0=m1s[1].t[:], in1=m2s[1].t[:], op=ALU.add), reads=[m1s[1].r, m2s[1].r], writes=[rqb[pp].r])
                        bank = proj(1280, 512)
                        rope(bank, 8, t, m1s[0], m2s[0], None)
                        S.op("pool", lambda e: e.tensor_tensor(out=rkb[pp].t[:], in0=m1s[0].t[:], in1=m2s[0].t[:], op=ALU.add), reads=[m1s[0].r, m2s[0].r], writes=[rkb[pp].r])
                        for i in range(4):
                            S.op("pe", lambda e: e.transpose(out=pb[6].t[:, i * 128:(i + 1) * 128], in_=rqa[pp].t[:, i * 128:(i + 1) * 128], identity=ident_b.t[:]), reads=[rqa[pp].r, ident_b.r], writes=[pb[6].r])
                        for i in range(4):
                            S.op("pe", lambda e: e.transpose(out=pb[6].t[:, 512 + i * 128:512 + (i + 1) * 128], in_=rqb[pp].t[:, i * 128:(i + 1) * 128], identity=ident_b.t[:]), reads=[rqb[pp].r, ident_b.r], writes=[pb[6].r])
                        S.op("act", lambda e: e.activation(out=qa_c.t[:, u, :], in_=pb[6].t[:, 0:512], func=AF.Copy), reads=[pb[6].r], writes=[qa_c.r])
                        S.op("act", lambda e: e.activation(out=qb_c.t[:, :, u * 128:(u + 1) * 128], in_=pb[6].t[:, 512:1024].rearrange("p (i q) -> p i q", i=4), func=AF.Copy), reads=[pb[6].r], writes=[qb_c.r])
                        bank = proj(512, 256)
                        rope(bank, 2, t, m1s[1], m2s[1], None)
                        S.op("pool", lambda e: e.tensor_tensor(out=rka[pp].t[:], in0=m1s[1].t[:, 0:128], in1=m2s[1].t[:, 0:128], op=ALU.add), reads=[m1s[1].r, m2s[1].r], writes=[rka[pp].r])
                        S.op("act", lambda e: e.activation(out=Va.t[:, t, :, 0:64], in_=bank.t[:, 128:256].rearrange("p (h c) -> p h c", h=2), func=AF.Copy), reads=[bank.r], writes=[r_kv[t]])
                        bank = proj(1792, 512)
                        S.op("act", lambda e: e.activation(out=Vb.t[:, t, :, 0:64], in_=bank.t[:, 0:512].rearrange("p (h c) -> p h c", h=8), func=AF.Copy), reads=[bank.r], writes=[r_kv[t]])

                        for i in range(4):
                            S.op("pe", lambda e: e.transpose(out=pb[5].t[:, i * 128:(i + 1) * 128], in_=rkb[pp].t[:, i * 128:(i + 1) * 128], identity=ident_b.t[:]), reads=[rkb[pp].r, ident_b.r], writes=[pb[5].r])
                        S.op("pe", lambda e: e.transpose(out=pb[5].t[:, 512:640], in_=rka[pp].t[:, 0:128], identity=ident_b.t[:]), reads=[rka[pp].r, ident_b.r], writes=[pb[5].r])
                        S.op("act", lambda e: e.activation(out=kT_b.t[:, :, t * 128:(t + 1) * 128], in_=pb[5].t[:, 0:512].rearrange("p (i q) -> p i q", i=4), func=AF.Copy), reads=[pb[5].r], writes=[r_kv[t]])
                        S.op("act", lambda e: e.activation(out=kT_a.t[:, t * 128:(t + 1) * 128], in_=pb[5].t[:, 512:640], func=AF.Copy), reads=[pb[5].r], writes=[r_kv[t]])
                    kvc = [r_kv[4 * c + u] for u in range(4)]
                    S.op("dve", lambda e: e.tensor_reduce(out=kms.t[:, :, 2 * c:2 * c + 2], in_=kT_b.t[:, :, c * 512:(c + 1) * 512].rearrange("p i (b k) -> p i b k", b=2), axis=AX.X, op=ALU.add), reads=kvc, writes=[kms.r])
                    S.op("act", lambda e: e.activation(out=kmT.t[:, :, 2 * c:2 * c + 2], in_=kms.t[:, :, 2 * c:2 * c + 2], func=AF.Copy, scale=1.0 / 256.0), reads=[kms.r], writes=[kmT.r])
                    if c >= 2:
                        for u in range(4):
                            for h in range(8):
                                i, ph = h // 2, (h % 2) * 64
                                S.op("pe", lambda e: e.matmul(pb[7].t[:, (u * 8 + h) * 8:(u * 8 + h) * 8 + 8], lhsT=qb_c.t[ph:ph + 64, i, u * 128:(u + 1) * 128], rhs=kmT.t[ph:ph + 64, i, 0:8], start=True, stop=True), reads=[qb_c.r, kmT.r], writes=[pb[7].r], pe_order=True)
                        for u in range(4):
                            j = 2 * c + u // 2
                            S.op("dve", lambda e: e.tensor_tensor(out=gm.t[:], in0=pb[7].t[:, u * 64:(u + 1) * 64], in1=elig.t[:, j - 4, :], op=ALU.add), reads=[pb[7].r, elig.r], writes=[gm.r])
                            for h in range(8):
                                S.op("dve", lambda e: e.max(out=top.t[:, h, :], in_=gm.t[:, h * 8:(h + 1) * 8]), reads=[gm.r], writes=[top.r])
                            S.op("dve", lambda e: e.tensor_tensor(out=sel_c.t[:, u, :].rearrange("p (h b) -> p h b", h=8), in0=gm.t[:].rearrange("p (h b) -> p h b", h=8), in1=top.t[:, :, 2:3].broadcast_to([128, 8, 8]), op=ALU.is_ge), reads=[gm.r, top.r], writes=[sel_c.r])

                    def swa_s1(u, g):
                        qt = 4 * c + u
                        ph = g * 64
                        pt_prev = None
                        if qt >= 1:
                            bank = nbs()
                            S.op("pe", lambda e: e.matmul(bank.t[:, 0:512], lhsT=kT_a.t[ph:ph + 64, (qt - 1) * 128:qt * 128], rhs=qa_c.t[ph:ph + 64, u, :], start=True, stop=False), reads=[r_kv[qt - 1], qa_c.r], writes=[bank.r])
                            S.op("pe", lambda e: e.matmul(bank.t[:, 0:512], lhsT=ident_b.t[:], rhs=atri4.t[:], start=False, stop=True), reads=[ident_b.r, atri4.r], writes=[bank.r])
                            pt_prev = npt()
                            S.op("act", lambda e: e.activation(out=pt_prev.t[:], in_=bank.t[:, 0:512], func=AF.Exp, scale=0.125), reads=[bank.r], writes=[pt_prev.r])
                        bank = nbs()
                        S.op("pe", lambda e: e.matmul(bank.t[:, 0:512], lhsT=kT_a.t[ph:ph + 64, qt * 128:(qt + 1) * 128], rhs=qa_c.t[ph:ph + 64, u, :], start=True, stop=False), reads=[r_kv[qt], qa_c.r], writes=[bank.r])
                        S.op("pe", lambda e: e.matmul(bank.t[:, 0:512], lhsT=ident_b.t[:], rhs=tri4.t[:], start=False, stop=True), reads=[ident_b.r, tri4.r], writes=[bank.r])
                        pt_cur = npt()
                        S.op("act", lambda e: e.activation(out=pt_cur.t[:], in_=bank.t[:, 0:512], func=AF.Exp, scale=0.125), reads=[bank.r], writes=[pt_cur.r])
                        return pt_prev, pt_cur

                    def swa_s2(u, g, pt_prev, pt_cur):
                        qt = 4 * c + u
                        ob = nob()
                        for hh in range(4):
                            reg = ob.t[:, hh * 65:(hh + 1) * 65]
                            if pt_prev is not None:
                                S.op("pe", lambda e: e.matmul(reg, lhsT=pt_prev.t[:, hh * 128:(hh + 1) * 128], rhs=Va.t[:, qt - 1, g, :], start=True, stop=False), reads=[pt_prev.r, r_kv[qt - 1]], writes=[ob.r])
                            S.op("pe", lambda e: e.matmul(reg, lhsT=pt_cur.t[:, hh * 128:(hh + 1) * 128], rhs=Va.t[:, qt, g, :], start=(pt_prev is None), stop=True), reads=[pt_cur.r, r_kv[qt]], writes=[ob.r])
                        ov = ob.t[:, 0:260].rearrange("p (h c) -> p h c", h=4)
                        S.op("dve", lambda e: e.tensor_tensor(out=den.t[:].unsqueeze(2), in0=ov[:, :, 64:65], in1=expsink.t[:, 4 * g:4 * g + 4].unsqueeze(2), op=ALU.add), reads=[ob.r, expsink.r], writes=[den.r])
                        S.op("dve", lambda e: e.reciprocal(out=rden.t[:], in_=den.t[:]), reads=[den.r], writes=[rden.r])
                        S.op("dve", lambda e: e.tensor_tensor(out=o_c.t[:, u, g * 256:(g + 1) * 256].rearrange("p (h c) -> p h c", h=4), in0=ov[:, :, 0:64], in1=rden.t[:].unsqueeze(2).broadcast_to([128, 4, 64]), op=ALU.mult), reads=[ob.r, rden.r], writes=[o_c.r])

                    def moba_s1(h, blk):
                        i, ph = h // 2, (h % 2) * 64
                        ptk = {}
                        for kt in (2 * blk, 2 * blk + 1):
                            r = kt - 4 * c
                            q0 = max(r, 0) * 128
                            n = 512 - q0
                            bank = nbs()
                            S.op("pe", lambda e: e.matmul(bank.t[:, 0:n], lhsT=kT_b.t[ph:ph + 64, i, kt * 128:(kt + 1) * 128], rhs=qb_c.t[ph:ph + 64, i, q0:512], start=True, stop=(r < 0)), reads=[r_kv[kt], qb_c.r], writes=[bank.r])
                            if r >= 0:
                                S.op("pe", lambda e: e.matmul(bank.t[:, 0:128], lhsT=ident_b.t[:], rhs=tri4.t[:, 0:128], start=False, stop=True), reads=[ident_b.r, tri4.r], writes=[bank.r])
                            p = npt()
                            S.op("act", lambda e: e.activation(out=p.t[:, q0:512], in_=bank.t[:, 0:n], func=AF.Exp, scale=0.125), reads=[bank.r], writes=[p.r])
                            ptk[kt] = p
                        return ptk

                    def moba_s2(h, blk, ptk):
                        acc = accs[h % 2]
                        ob = nob()
                        us = []
                        for u in range(4):
                            kts = [kt for kt in (2 * blk, 2 * blk + 1) if kt <= 4 * c + u]
                            if not kts:
                                continue
                            us.append(u)
                            for ki, kt in enumerate(kts):
                                S.op("pe", lambda e: e.matmul(ob.t[:, u * 65:(u + 1) * 65], lhsT=ptk[kt].t[:, u * 128:(u + 1) * 128], rhs=Vb.t[:, kt, h, :], start=(ki == 0), stop=(ki == len(kts) - 1)), reads=[ptk[kt].r, r_kv[kt]], writes=[ob.r])
                        first = (blk == 0)
                        need_sel = [(blk != 2 * c + u // 2) and (2 * c + u // 2 >= 4) for u in us]
                        if not any(need_sel):
                            u0, u1 = us[0], us[-1] + 1
                            if first:
                                S.op("dve", lambda e: e.tensor_copy(out=acc.t[:, u0:u1, :], in_=ob.t[:, u0 * 65:u1 * 65].rearrange("p (u c) -> p u c", c=65)), reads=[ob.r], writes=[acc.r])
                            else:
                                S.op("dve", lambda e: e.tensor_tensor(out=acc.t[:, u0:u1, :], in0=ob.t[:, u0 * 65:u1 * 65].rearrange("p (u c) -> p u c", c=65), in1=acc.t[:, u0:u1, :], op=ALU.add), reads=[ob.r, acc.r], writes=[acc.r])
                        else:
                            for u, ns in zip(us, need_sel):
                                reg = ob.t[:, u * 65:(u + 1) * 65]
                                sc = sel_c.t[:, u, h * 8 + blk:h * 8 + blk + 1]
                                if ns and first:
                                    S.op("dve", lambda e: e.tensor_scalar(out=acc.t[:, u, :], in0=reg, scalar1=sc, scalar2=None, op0=ALU.mult), reads=[ob.r, sel_c.r], writes=[acc.r])
                                elif ns:
                                    S.op("dve", lambda e: e.scalar_tensor_tensor(out=acc.t[:, u, :], in0=reg, scalar=sc, in1=acc.t[:, u, :], op0=ALU.mult, op1=ALU.add), reads=[ob.r, sel_c.r, acc.r], writes=[acc.r])
                                elif first:
                                    S.op("dve", lambda e: e.tensor_copy(out=acc.t[:, u, :], in_=reg), reads=[ob.r], writes=[acc.r])
                                else:
                                    S.op("dve", lambda e: e.tensor_tensor(out=acc.t[:, u, :], in0=reg, in1=acc.t[:, u, :], op=ALU.add), reads=[ob.r, acc.r], writes=[acc.r])
                        if blk == 2 * c + 1:
                            S.op("dve", lambda e: e.reciprocal(out=rden.t[:].unsqueeze(2), in_=acc.t[:, :, 64:65]), reads=[acc.r], writes=[rden.r])
                            S.op("dve", lambda e: e.tensor_tensor(out=o_c.t[:, :, 512 + h * 64:512 + (h + 1) * 64], in0=acc.t[:, :, 0:64], in1=rden.t[:].unsqueeze(2).broadcast_to([128, 4, 64]), op=ALU.mult), reads=[acc.r, rden.r], writes=[o_c.r])

                    items = [("s", u, g) for u in range(4) for g in range(2)] + [("m", h, blk) for h in range(8) for blk in range(2 * c + 2)]

                    def s1(it):
                        return swa_s1(it[1], it[2]) if it[0] == "s" else moba_s1(it[1], it[2])

                    def s2(it, st1):
                        if it[0] == "s":
                            swa_s2(it[1], it[2], st1[0], st1[1])
                        else:
                            moba_s2(it[1], it[2], st1)

                    nxt = s1(items[0])
                    for ii, it in enumerate(items):
                        cur = nxt
                        if ii + 1 < len(items):
                            nxt = s1(items[ii + 1])
                        s2(it, cur)

                    def a5_s1(u):
                        T = s * 16 + 4 * c + u
                        xr, yl = xres[0], ylns[T % 2]
                        S.dma("sp", lambda e: e.dma_start(out=xr.t[:], in_=x_d[T * 128:(T + 1) * 128, :]), "xres0", writes=[xr.r])
                        for k in range(8):
                            S.op("pe", lambda e: e.transpose(out=pb[6].t[:, k * 128:(k + 1) * 128], in_=o_c.t[:, u, k * 128:(k + 1) * 128], identity=ident_b.t[:]), reads=[o_c.r, ident_b.r], writes=[pb[6].r])
                        S.op("act", lambda e: e.activation(out=oT.t[:], in_=pb[6].t[:], func=AF.Copy), reads=[pb[6].r], writes=[oT.r])
                        for hf in range(2):
                            bank = nb()
                            for k in range(8):
                                S.op("pe", lambda e: e.matmul(bank.t[:, 0:512], lhsT=oT.t[:, k * 128:(k + 1) * 128], rhs=w_out.t[:, k, hf * 512:(hf + 1) * 512], start=(k == 0), stop=False), reads=[oT.r, w_out.r], writes=[bank.r])
                            S.op("pe", lambda e: e.matmul(bank.t[:, 0:512], lhsT=ones2.t[:, :], rhs=bout2.t[:, hf * 512:(hf + 1) * 512], start=False, stop=True), reads=[ones2.r, bout2.r], writes=[bank.r])
                            S.op("dve", lambda e: e.scalar_tensor_tensor(out=yl.t[:, hf * 512:(hf + 1) * 512], in0=xr.t[:, hf * 512:(hf + 1) * 512], scalar=ALPHA, in1=bank.t[:, 0:512], op0=ALU.mult, op1=ALU.add), reads=[xr.r, bank.r], writes=[yl.r])

                    def a5_s2(u):
                        T = s * 16 + 4 * c + u
                        pp = T % 2
                        yl, x1, x1b = ylns[pp], x1s[pp], x1bs[pp]
                        layer_norm(lnb, yl, ln1g, ln1b, x1)
                        S.dma("sp", lambda e: e.dma_start(out=x1buf[T * 128:(T + 1) * 128, :], in_=x1.t[:]), "x1st%d" % pp, reads=[x1.r], writes=[r_x1buf])
                        S.op("act", lambda e: e.activation(out=x1b.t[:], in_=x1.t[:], func=AF.Copy), reads=[x1.r], writes=[x1b.r])
                        for rr in range(2):
                            for k in range(4):
                                kk = rr * 4 + k
                                S.op("pe", lambda e: e.transpose(out=pb[7].t[:, k * 128:(k + 1) * 128], in_=x1.t[:, kk * 128:(kk + 1) * 128], identity=ident_f.t[:]), reads=[x1.r, ident_f.r], writes=[pb[7].r])
                            S.op("act", lambda e: e.activation(out=x1T.t[:, rr * 512:(rr + 1) * 512], in_=pb[7].t[:, 0:512], func=AF.Copy), reads=[pb[7].r], writes=[x1T.r])
                        for k in range(8):
                            S.op("pe", lambda e: e.matmul(pb[7].t[:, 0:NE], lhsT=x1T.t[:, k * 128:(k + 1) * 128], rhs=w_r.t[:, k, :], start=(k == 0), stop=(k == 7)), reads=[x1T.r, w_r.r], writes=[pb[7].r])
                        S.op("dve", lambda e: e.tensor_tensor(out=lg.t[:], in0=pb[7].t[:, 0:NE], in1=brB.t[:], op=ALU.add), reads=[pb[7].r, brB.r], writes=[lg.r])
                        S.op("dve", lambda e: e.max(out=top8.t[:], in_=lg.t[:]), reads=[lg.r], writes=[top8.r])
                        S.op("dve", lambda e: e.tensor_scalar(out=ntop.t[:], in0=top8.t[:, 0:1], scalar1=-1.0, scalar2=None, op0=ALU.mult), reads=[top8.r], writes=[ntop.r])
                        S.op("act", lambda e: e.activation(out=gex.t[:], in_=top8.t[:, 0:4], func=AF.Exp, bias=ntop.t[:, 0:1], scale=1.0, accum_out=gsum.t[:, 0:1]), reads=[top8.r, ntop.r], writes=[gex.r, gsum.r])
                        S.op("dve", lambda e: e.reciprocal(out=gsum.t[:], in_=gsum.t[:]), reads=[gsum.r], writes=[gsum.r])
                        S.op("dve", lambda e: e.tensor_scalar(out=gates.t[:, T, :], in0=gex.t[:], scalar1=gsum.t[:, 0:1], scalar2=None, op0=ALU.mult), reads=[gex.r, gsum.r], writes=[gates.r])
                        S.op("dve", lambda e: e.tensor_scalar(out=Mb.t[:], in0=lg.t[:], scalar1=top8.t[:, 3:4], scalar2=None, op0=ALU.is_ge), reads=[lg.r, top8.r], writes=[Mb.r])
                        S.op("pe", lambda e: e.matmul(pb[7].t[:, 64:64 + NE], lhsT=Lst.t[:], rhs=Mb.t[:], start=True, stop=True), reads=[Lst.r, Mb.r], writes=[pb[7].r])
                        S.op("pe", lambda e: e.matmul(pb[7].t[:, 128:128 + NE], lhsT=onesm.t[:], rhs=Mb.t[:], start=True, stop=True), reads=[onesm.r, Mb.r], writes=[pb[7].r])
                        S.op("dve", lambda e: e.tensor_tensor(out=posD.t[:], in0=pb[7].t[:, 64:64 + NE], in1=carryD.t[:], op=ALU.add), reads=[pb[7].r, carryD.r], writes=[posD.r])
                        S.op("dve", lambda e: e.tensor_tensor(out=carryD.t[:], in0=pb[7].t[:, 128:128 + NE], in1=carryD.t[:], op=ALU.add), reads=[pb[7].r, carryD.r], writes=[carryD.r])
                        for k in range(4):
                            S.op("dve", lambda e: e.scalar_tensor_tensor(out=junk.t[:], in0=lg.t[:], scalar=top8.t[:, k:k + 1], in1=posD.t[:], op0=ALU.is_equal, op1=ALU.mult, accum_out=destf.t[:, k:k + 1]), reads=[lg.r, top8.r, posD.r], writes=[junk.r, destf.r])
                        S.op("dve", lambda e: e.tensor_copy(out=dest.t[:, T, :], in_=destf.t[:]), reads=[destf.r], writes=[dest.r])
                        for k in range(4):
                            S.dma("pool", lambda e: e.indirect_dma_start(out=xbuf, out_offset=bass.IndirectOffsetOnAxis(ap=dest.t[:, T, k:k + 1], axis=0), in_=x1b.t[:], in_offset=None), "x1b%d" % pp, reads=[x1b.r, dest.r], writes=[r_xbuf])

                    a5_s1(0)
                    for u in range(4):
                        if u + 1 < 4:
                            a5_s1(u + 1)
                        a5_s2(u)
            S.barrier()

        if dbg == "A":
            S.finish("sp", [r_x1buf])
            S.barrier()
            return nc

        with ExitStack() as st:
            b1g = sb(st, [128, NE, 8], F32, "b1g")
            b1l = sb(st, [128, NE, 8], F32, "b1l")
            S.dma("sp", lambda e: e.dma_start(out=b1g.t[:], in_=b1g_d), "b1g", writes=[b1g.r])
            S.dma("sp", lambda e: e.dma_start(out=b1l.t[:], in_=b1l_d), "b1l", writes=[b1l.r])
            S.op("dve", lambda e: e.tensor_scalar(out=b1g.t[:], in0=b1g.t[:], scalar1=1.702, scalar2=None, op0=ALU.mult), reads=[b1g.r], writes=[b1g.r])
            S.op("dve", lambda e: e.tensor_scalar(out=b1l.t[:], in0=b1l.t[:], scalar1=7.0, scalar2=SINV, op0=ALU.add, op1=ALU.mult), reads=[b1l.r], writes=[b1l.r])
            wg = [sb(st, [128, 8, D], BF16, "wg") for _ in range(2)]
            wl = [sb(st, [128, 8, D], BF16, "wl") for _ in range(2)]
            w2 = [sb(st, [128, 8, D], BF16, "w2") for _ in range(2)]
            b2B = [sb(st, [128, D], F32, "b2B") for _ in range(2)]
            xeT = [sb(st, [128, 8, C], BF16, "xeT") for _ in range(2)]
            xss = [sb(st, [128, D], BF16, "xs") for _ in range(4)]
            actT = [sb(st, [128, 8, 512], BF16, "actT") for _ in range(2)]
            glu = [sb(st, [128, 512], F32, "glu") for _ in range(2)]
            sg = [sb(st, [128, 512], F32, "sg") for _ in range(2)]
            la = [sb(st, [128, 512], F32, "la") for _ in range(2)]
            ysb = [sb(st, [128, D], F32, "ysb") for _ in range(2)]
            xsc = [0]
            ysc = [0]
            hc = [0]
            chunks = []
            q0 = 0
            while q0 < C:
                n = min(512, C - q0)
                chunks.append((q0, n))
                q0 += n

            def load_w(e_):
                pp = e_ % 2
                for (wb, wd, nm) in ((wg[pp], w1g_d, "wg"), (wl[pp], w1l_d, "wl"), (w2[pp], w2_d, "w2")):
                    src = wd[e_].rearrange("(k p) f -> p k f", p=128)
                    for k in range(8):
                        S.dma("pool", lambda e: e.dma_start(out=wb.t[:, k, :], in_=src[:, k, :]), "%s%d" % (nm, pp), writes=[wb.r])
                S.dma("sp", lambda e: e.dma_start(out=b2B[pp].t[:], in_=b2_d[e_].partition_broadcast(128)), "b2B%d" % pp, writes=[b2B[pp].r])

            def prep_x(e_):
                pp = e_ % 2
                for s_ in range(NS):
                    xs = xss[xsc[0] % 4]
                    xsc[0] += 1
                    row0 = e_ * C + s_ * 128
                    S.dma("sp", lambda e: e.dma_start(out=xs.t[:], in_=xbuf[row0:row0 + 128, :]), "xs%d" % ((xsc[0] - 1) % 4), reads=[r_xbuf], writes=[xs.r])
                    for k in range(8):
                        S.op("pe", lambda e: e.transpose(out=pb[5].t[:, k * 128:(k + 1) * 128], in_=xs.t[:, k * 128:(k + 1) * 128], identity=ident_b.t[:]), reads=[xs.r, ident_b.r], writes=[pb[5].r])
                    S.op("act", lambda e: e.activation(out=xeT[pp].t[:, :, s_ * 128:(s_ + 1) * 128], in_=pb[5].t[:].rearrange("p (k q) -> p k q", k=8), func=AF.Copy), reads=[pb[5].r], writes=[xeT[pp].r])

            load_w(0)
            prep_x(0)
            for e_ in range(NE):
                pp = e_ % 2
                if e_ + 1 < NE:
                    load_w(e_ + 1)
                for ci, (q0, n) in enumerate(chunks):
                    aT = actT[ci % 2]
                    for j in range(8):
                        hb = hc[0] % 2
                        hc[0] += 1
                        bg, bl = pb[hb * 2], pb[hb * 2 + 1]
                        for k in range(8):
                            S.op("pe", lambda e: e.matmul(bg.t[:, 0:n], lhsT=wg[pp].t[:, k, j * 128:(j + 1) * 128], rhs=xeT[pp].t[:, k, q0:q0 + n], start=(k == 0), stop=(k == 7)), reads=[wg[pp].r, xeT[pp].r], writes=[bg.r])
                        for k in range(8):
                            S.op("pe", lambda e: e.matmul(bl.t[:, 0:n], lhsT=wl[pp].t[:, k, j * 128:(j + 1) * 128], rhs=xeT[pp].t[:, k, q0:q0 + n], start=(k == 0), stop=(k == 7)), reads=[wl[pp].r, xeT[pp].r], writes=[bl.r])
                        S.op("act", lambda e: e.activation(out=glu[hb].t[:, 0:n], in_=bg.t[:, 0:n], func=AF.Silu, scale=1.702, bias=b1g.t[:, e_, j:j + 1]), reads=[bg.r, b1g.r], writes=[glu[hb].r])
                        S.op("act", lambda e: e.activation(out=la[hb].t[:, 0:n], in_=bl.t[:, 0:n], func=AF.Relu, scale=SINV, bias=b1l.t[:, e_, j:j + 1]), reads=[bl.r, b1l.r], writes=[la[hb].r])
                        S.op("dve", lambda e: e.tensor_scalar(out=sg[hb].t[:, 0:n], in0=la[hb].t[:, 0:n], scalar1=14.0 * SINV, scalar2=-6.0 * SINV, op0=ALU.min, op1=ALU.add), reads=[la[hb].r], writes=[sg[hb].r])
                        S.op("dve", lambda e: e.scalar_tensor_tensor(out=aT.t[:, j, 0:n], in0=glu[hb].t[:, 0:n], scalar=UMAX, in1=sg[hb].t[:, 0:n], op0=ALU.min, op1=ALU.mult), reads=[glu[hb].r, sg[hb].r], writes=[aT.r])
                    for sl in range(n // 128):
                        yb = ysb[ysc[0] % 2]
                        yi = ysc[0] % 2
                        ysc[0] += 1
                        for hf in range(2):
                            bank = pb[4] if hf == 0 else pb[7]
                            for j in range(8):
                                S.op("pe", lambda e: e.matmul(bank.t[:, 0:512], lhsT=aT.t[:, j, sl * 128:(sl + 1) * 128], rhs=w2[pp].t[:, j, hf * 512:(hf + 1) * 512], start=(j == 0), stop=(j == 7)), reads=[aT.r, w2[pp].r], writes=[bank.r])
                            S.op("dve", lambda e: e.tensor_tensor(out=yb.t[:, hf * 512:(hf + 1) * 512], in0=bank.t[:, 0:512], in1=b2B[pp].t[:, hf * 512:(hf + 1) * 512], op=ALU.add), reads=[bank.r, b2B[pp].r], writes=[yb.r])
                        row0 = e_ * C + q0 + sl * 128
                        S.dma("sp", lambda e: e.dma_start(out=ybuf[row0:row0 + 128, :], in_=yb.t[:]), "ysb%d" % yi, reads=[yb.r], writes=[r_ybuf])
                if e_ + 1 < NE:
                    prep_x(e_ + 1)
            S.barrier()

        with ExitStack() as st:
            ln2g = sb(st, [128, D], F32, "ln2g")
            ln2b = sb(st, [128, D], F32, "ln2b")
            S.dma("sp", lambda e: e.dma_start(out=ln2g.t[:], in_=ln2g_d[0].partition_broadcast(128)), "ln2g", writes=[ln2g.r])
            S.dma("sp", lambda e: e.dma_start(out=ln2b.t[:], in_=ln2b_d[0].partition_broadcast(128)), "ln2b", writes=[ln2b.r])
            ygs = [[sb(st, [128, D], F32, "yg") for _ in range(4)] for _ in range(3)]
            x1r = [sb(st, [128, D], F32, "x1r") for _ in range(3)]
            accC = [sb(st, [128, D], F32, "accC") for _ in range(3)]
            outb = [sb(st, [128, D], F32, "outb") for _ in range(3)]
            lnb2 = (sb(st, [128, 2, 6], F32, "stats2"), sb(st, [128, 2], F32, "mv2"), sb(st, [128, 1], F32, "lnv2"),
                    sb(st, [128, 1], F32, "rstd2"), sb(st, [128, 1], F32, "nmr2"), sb(st, [128, D], F32, "xn2"))
            for T in range(NT):
                pp = T % 3
                S.dma("sp", lambda e: e.dma_start(out=x1r[pp].t[:], in_=x1buf[T * 128:(T + 1) * 128, :]), "x1r%d" % pp, reads=[r_x1buf], writes=[x1r[pp].r])
                for k in range(4):
                    yg = ygs[pp][k]
                    S.dma("pool", lambda e: e.indirect_dma_start(out=yg.t[:], out_offset=None, in_=ybuf, in_offset=bass.IndirectOffsetOnAxis(ap=dest.t[:, T, k:k + 1], axis=0)), "yg%d_%d" % (pp, k), reads=[r_ybuf, dest.r], writes=[yg.r])
                a = accC[pp]
                S.op("act", lambda e: e.activation(out=a.t[:], in_=x1r[pp].t[:], func=AF.Copy, scale=ALPHA), reads=[x1r[pp].r], writes=[a.r])
                for k in range(4):
                    yg = ygs[pp][k]
                    S.op("dve", lambda e: e.scalar_tensor_tensor(out=a.t[:], in0=yg.t[:], scalar=gates.t[:, T, k:k + 1], in1=a.t[:], op0=ALU.mult, op1=ALU.add), reads=[yg.r, gates.r, a.r], writes=[a.r])
                layer_norm(lnb2, a, ln2g, ln2b, outb[pp])
                S.dma("sp", lambda e: e.dma_start(out=out_d[T * 128:(T + 1) * 128, :], in_=outb[pp].t[:]), "outb%d" % pp, reads=[outb[pp].r], writes=[r_out])
            S.finish("sp", [r_out])
            S.barrier()
    return nc


def rope_tables():
    inv = 1.0 / (10000.0 ** (np.arange(0, 64, 2, dtype=np.float32) / 64.0))
    ang = np.arange(SEQ, dtype=np.float32)[:, None] * inv[None, :].astype(np.float32)
    cos = np.cos(ang).astype(np.float32)
    sin = np.sin(ang).astype(np.float32)
    return np.concatenate([cos, cos], 1), np.concatenate([-sin, sin], 1)


def prep_weights(w_in, b_in, sinks, w_out, b_out, ln1_g, ln1_b, w_router, b_router, w1, b1, w2, b2, ln2_g, ln2_b):
    f = lambda a: np.ascontiguousarray(np.asarray(a, dtype=np.float32))
    rc, rs = rope_tables()
    w1 = np.asarray(w1)[0]
    b1 = np.asarray(b1)[0]
    m = {
        "w_in": f(w_in[0]), "b_in": f(b_in[0]).reshape(1, -1), "sinks": f(sinks[0]).reshape(1, -1),
        "w_out": f(w_out[0]), "b_out": f(b_out[0]).reshape(1, -1),
        "ln1_g": f(ln1_g[0]).reshape(1, -1), "ln1_b": f(ln1_b[0]).reshape(1, -1),
        "w_router": f(np.asarray(w_router[0]).reshape(8, 128, NE).transpose(1, 0, 2)), "b_router": f(b_router[0]).reshape(1, -1),
        "w1g": f(w1[:, :, 0::2]), "w1l": f(w1[:, :, 1::2]),
        "b1g": f(b1[:, 0::2].reshape(NE, 8, 128).transpose(2, 0, 1)),
        "b1l": f(b1[:, 1::2].reshape(NE, 8, 128).transpose(2, 0, 1)),
        "w2": f(w2[0]), "b2": f(b2[0]),
        "ln2_g": f(ln2_g[0]).reshape(1, -1), "ln2_b": f(ln2_b[0]).reshape(1, -1),
        "ropeC": f(rc.reshape(16, 128, 64).transpose(1, 0, 2)), "ropeS": f(rs.reshape(16, 128, 64).transpose(1, 0, 2)),
    }
    return m


def kernel(x, w_in, b_in, sinks, w_out, b_out, ln1_g, ln1_b, w_router, b_router, w1, b1, w2, b2, ln2_g, ln2_b):
    x = np.asarray(x, dtype=np.float32)
    B = x.shape[0]
    nseq = B // N_CORES
    wm = prep_weights(w_in, b_in, sinks, w_out, b_out, ln1_g, ln1_b, w_router, b_router, w1, b1, w2, b2, ln2_g, ln2_b)
    nc = build(nseq, 1536)
    in_maps = []
    for c in range(N_CORES):
        m = dict(wm)
        m["x"] = np.ascontiguousarray(x[c * nseq:(c + 1) * nseq].reshape(nseq * SEQ, D))
        in_maps.append(m)
    res = run_bass_kernel_spmd(nc, in_maps, core_ids=list(range(N_CORES)))
    out = np.concatenate([r["out"].reshape(nseq, SEQ, D) for r in res.results], axis=0)
    return out.astype(np.float32)
```

```python
import numpy as np
from contextlib import ExitStack
import concourse.bass as bass
import concourse.mybir as mybir
from concourse.bass_utils import run_bass_kernel_spmd

F32 = mybir.dt.float32
BF16 = mybir.dt.bfloat16
U32 = mybir.dt.uint32
I32 = mybir.dt.int32
AF = mybir.ActivationFunctionType
ALU = mybir.AluOpType
AX = mybir.AxisListType

D = 1024
SEQ = 2048
NE = 32
ALPHA = float(2.0 ** 0.25)
EPS = 1e-5
NEG = -30000.0
SINV = float(1.0 / 1.702)
UMAX = float(11.914 / (1.0 + np.exp(-11.914)))
N_CORES = 8


class Res:
    __slots__ = ("name", "w", "r", "multi", "excl")

    def __init__(self, name, multi=False, excl=False):
        self.excl = excl
        self.name = name
        self.w = {}
        self.r = {}
        self.multi = multi


class Buf:
    __slots__ = ("t", "r")

    def __init__(self, t, name):
        self.t = t
        self.r = Res(name)


class Sched:
    def __init__(self, nc, stack):
        self.nc = nc
        self.stack = stack
        self.eng = {"pe": nc.tensor, "act": nc.scalar, "dve": nc.vector, "pool": nc.gpsimd, "sp": nc.sync}
        self.sem = {}
        self.cnt = {}
        self.waited = {k: {} for k in self.eng}
        for k in self.eng:
            self.sem[k] = stack.enter_context(nc.semaphore("s_" + k))
            self.cnt[k] = 0
        self.dsem = {}
        self.dcnt = {}
        self.nwait = 0
        self.nins = 0

    def _wait(self, e, deps):
        for key, (sh, val) in deps.items():
            if self.waited[e].get(key, 0) >= val:
                continue
            self.eng[e].wait_ge(sh, val)
            self.waited[e][key] = val
            self.nwait += 1

    def _deps(self, e, reads, writes, pe_order=False):
        deps = {}

        def add(key, sh, val):
            if key == "pe" and e == "pe" and not pe_order:
                return
            if key not in deps or deps[key][1] < val:
                deps[key] = (sh, val)

        for r in reads:
            for key, (sh, val) in r.w.items():
                add(key, sh, val)
            if r.excl:
                for key, (sh, val) in r.r.items():
                    if key != e:
                        add(key, sh, val)
        for w in writes:
            if not w.multi:
                for key, (sh, val) in w.w.items():
                    add(key, sh, val)
            for key, (sh, val) in w.r.items():
                add(key, sh, val)
        return deps

    def _record(self, key, sh, val, reads, writes):
        for r in reads:
            r.r[key] = (sh, val)
        for w in writes:
            if w.multi:
                w.w[key] = (sh, val)
            else:
                w.w = {key: (sh, val)}
                w.r = {}

    def op(self, e, fn, reads=(), writes=(), pe_order=False):
        deps = self._deps(e, reads, writes, pe_order)
        self._wait(e, deps)
        ins = fn(self.eng[e])
        self.cnt[e] += 1
        self.nins += 1
        ins.then_inc(self.sem[e], 1)
        self._record(e, self.sem[e], self.cnt[e], reads, writes)
        return ins

    def dma(self, q, fn, dname, reads=(), writes=()):
        if dname not in self.dsem:
            self.dsem[dname] = self.stack.enter_context(self.nc.semaphore("d_" + dname))
            self.dcnt[dname] = 0
        key = "d_" + dname
        deps = self._deps(q, reads, writes)
        deps.pop(key, None)
        self._wait(q, deps)
        ins = fn(self.eng[q])
        self.dcnt[dname] += 16
        self.nins += 1
        ins.then_inc(self.dsem[dname], 16)
        self._record(key, self.dsem[dname], self.dcnt[dname], reads, writes)
        return ins

    def barrier(self):
        allev = {}
        for k in self.eng:
            if self.cnt[k] > 0:
                allev[k] = (self.sem[k], self.cnt[k])
        for dn in self.dsem:
            if self.dcnt[dn] > 0:
                allev["d_" + dn] = (self.dsem[dn], self.dcnt[dn])
        for e in self.eng:
            deps = {k: v for k, v in allev.items() if k != e}
            self._wait(e, deps)

    def finish(self, e, resources):
        deps = {}
        for r in resources:
            for key, (sh, val) in list(r.w.items()) + list(r.r.items()):
                if key not in deps or deps[key][1] < val:
                    deps[key] = (sh, val)
        self._wait(e, deps)


def build(NSEQ, C, dbg=False):
    NT = NSEQ * 16
    NTOK = NSEQ * SEQ
    NS = C // 128
    NROW = NE * C + 1024
    nc = bass.Bass("TRN2", target_bir_lowering=False)

    def din(name, shape, dt=F32):
        return nc.dram_tensor(name, shape, dt, kind="ExternalInput").ap()

    x_d = din("x", [NTOK, D])
    w_in_d = din("w_in", [D, 2304])
    b_in_d = din("b_in", [1, 2304])
    sinks_d = din("sinks", [1, 8])
    w_out_d = din("w_out", [D, D])
    b_out_d = din("b_out", [1, D])
    ln1g_d = din("ln1_g", [1, D])
    ln1b_d = din("ln1_b", [1, D])
    w_r_d = din("w_router", [128, 8, NE])
    b_r_d = din("b_router", [1, NE])
    w1g_d = din("w1g", [NE, D, D])
    w1l_d = din("w1l", [NE, D, D])
    b1g_d = din("b1g", [128, NE, 8])
    b1l_d = din("b1l", [128, NE, 8])
    w2_d = din("w2", [NE, D, D])
    b2_d = din("b2", [NE, D])
    ln2g_d = din("ln2_g", [1, D])
    ln2b_d = din("ln2_b", [1, D])
    ropeC_d = din("ropeC", [128, 16, 64])
    ropeS_d = din("ropeS", [128, 16, 64])
    out_d = nc.dram_tensor("out", [NTOK, D], F32, kind="ExternalOutput").ap()
    x1buf = nc.dram_tensor("x1buf", [NTOK, D], F32, kind="ExternalOutput" if dbg else "Internal").ap()
    xbuf = nc.dram_tensor("xbuf", [NROW, D], BF16, kind="Internal").ap()
    ybuf = nc.dram_tensor("ybuf", [NROW, D], F32, kind="Internal").ap()
    r_x1buf, r_xbuf, r_ybuf, r_out = Res("x1buf", True), Res("xbuf", True), Res("ybuf", True), Res("out", True)

    with ExitStack() as st0:
        S = Sched(nc, st0)
        uid = [0]

        def sb(st, shape, dt=F32, name="t"):
            uid[0] += 1
            nm = "%s_%d" % (name, uid[0])
            return Buf(st.enter_context(nc.sbuf_tensor(nm, shape, dt)), nm)

        pb = []
        for i in range(8):
            if i in (5, 6):
                t = st0.enter_context(nc.psum_tensor("pb%d" % i, [128, 1024], BF16))
            else:
                t = st0.enter_context(nc.psum_tensor("pb%d" % i, [128, 512], F32))
            pb.append(Buf(t, "pb%d" % i))
            pb[-1].r.excl = True
        bigc = [0]

        def nb():
            b = pb[bigc[0] % 3]
            bigc[0] += 1
            return b

        bigs = [0]

        def nbs():
            b = pb[(0, 1, 2, 7)[bigs[0] % 4]]
            bigs[0] += 1
            return b

        oc = [0]

        def nob():
            b = pb[3 + oc[0] % 2]
            oc[0] += 1
            return b

        gates = sb(st0, [128, NT, 4], F32, "gates")
        dest = sb(st0, [128, NT, 4], U32, "dest")
        ident_b = sb(st0, [128, 128], BF16, "identb")
        ident_f = sb(st0, [128, 128], F32, "identf")
        ones2 = sb(st0, [2, 128], BF16, "ones2")
        m10 = sb(st0, [2, 1], F32, "m10")

        S.op("pool", lambda e: e.memset(ident_f.t[:], 0.0), writes=[ident_f.r])
        S.op("pool", lambda e: e.affine_select(out=ident_f.t[:], in_=ident_f.t[:], pattern=[[-1, 128]], compare_op=ALU.not_equal, fill=1.0, base=0, channel_multiplier=1), reads=[ident_f.r], writes=[ident_f.r])
        S.op("dve", lambda e: e.tensor_copy(out=ident_b.t[:], in_=ident_f.t[:]), reads=[ident_f.r], writes=[ident_b.r])
        S.op("dve", lambda e: e.memset(ones2.t[:], 1.0), writes=[ones2.r])
        S.op("dve", lambda e: e.memset(m10.t[:], 0.0), writes=[m10.r])
        S.op("dve", lambda e: e.memset(m10.t[0:1, :], 1.0), reads=[m10.r], writes=[m10.r])

        def make_hilo(st, dst, src_row, n, tag):
            stg = sb(st, [2, n], F32, "hl_s" + tag)
            hb = sb(st, [2, n], BF16, "hl_b" + tag)
            hf = sb(st, [2, n], F32, "hl_f" + tag)
            lo = sb(st, [2, n], F32, "hl_l" + tag)
            S.dma("sp", lambda e: e.dma_start(out=stg.t[:], in_=src_row.partition_broadcast(2)), "hl" + tag, writes=[stg.r])
            hilo_compute(dst, stg, hb, hf, lo, n)

        def hilo_compute(dst, stg, hb, hf, lo, n):
            S.op("dve", lambda e: e.tensor_copy(out=hb.t[:, 0:n], in_=stg.t[:, 0:n]), reads=[stg.r], writes=[hb.r])
            S.op("dve", lambda e: e.tensor_copy(out=hf.t[:, 0:n], in_=hb.t[:, 0:n]), reads=[hb.r], writes=[hf.r])
            S.op("dve", lambda e: e.tensor_tensor(out=lo.t[:, 0:n], in0=stg.t[:, 0:n], in1=hf.t[:, 0:n], op=ALU.subtract), reads=[stg.r, hf.r], writes=[lo.r])
            S.op("dve", lambda e: e.tensor_tensor(out=hf.t[:, 0:n], in0=hf.t[:, 0:n], in1=lo.t[:, 0:n], op=ALU.subtract), reads=[hf.r, lo.r], writes=[hf.r])
            S.op("dve", lambda e: e.scalar_tensor_tensor(out=dst.t[:, 0:n], in0=hf.t[:, 0:n], scalar=m10.t[:, 0:1], in1=lo.t[:, 0:n], op0=ALU.mult, op1=ALU.add), reads=[hf.r, lo.r, m10.r], writes=[dst.r])

        def layer_norm(st_bufs, yln, gB, bB, outb, eng_gb="pool"):
            stats, mv, lnv, rstd, nmr, xn = st_bufs
            S.op("dve", lambda e: e.bn_stats(out=stats.t[:, 0, :], in_=yln.t[:, 0:512]), reads=[yln.r], writes=[stats.r])
            S.op("dve", lambda e: e.bn_stats(out=stats.t[:, 1, :], in_=yln.t[:, 512:1024]), reads=[yln.r], writes=[stats.r])
            S.op("dve", lambda e: e.bn_aggr(out=mv.t[:], in_=stats.t[:].rearrange("p a b -> p (a b)")), reads=[stats.r], writes=[mv.r])
            S.op("act", lambda e: e.activation(out=lnv.t[:], in_=mv.t[:, 1:2], func=AF.Ln, bias=EPS, scale=1.0), reads=[mv.r], writes=[lnv.r])
            S.op("act", lambda e: e.activation(out=rstd.t[:], in_=lnv.t[:], func=AF.Exp, scale=-0.5), reads=[lnv.r], writes=[rstd.r])
            S.op("dve", lambda e: e.tensor_scalar(out=nmr.t[:], in0=mv.t[:, 0:1], scalar1=-1.0, scalar2=rstd.t[:, 0:1], op0=ALU.mult, op1=ALU.mult), reads=[mv.r, rstd.r], writes=[nmr.r])
            S.op("act", lambda e: e.activation(out=xn.t[:], in_=yln.t[:], func=AF.Identity, scale=rstd.t[:, 0:1], bias=nmr.t[:, 0:1]), reads=[yln.r, rstd.r, nmr.r], writes=[xn.r])
            S.op(eng_gb, lambda e: e.tensor_tensor(out=xn.t[:], in0=xn.t[:], in1=gB.t[:], op=ALU.mult), reads=[xn.r, gB.r], writes=[xn.r])
            S.op(eng_gb, lambda e: e.tensor_tensor(out=outb.t[:], in0=xn.t[:], in1=bB.t[:], op=ALU.add), reads=[xn.r, bB.r], writes=[outb.r])

        with ExitStack() as st:
            w_in = sb(st, [128, 8, 2304], BF16, "w_in")
            w_out = sb(st, [128, 8, D], BF16, "w_out")
            w_r = sb(st, [128, 8, NE], F32, "w_r")
            bin2 = sb(st, [2, 2304], BF16, "bin2")
            bout2 = sb(st, [2, D], BF16, "bout2")
            ropeC = sb(st, [128, 16, 64], F32, "ropeC")
            ropeS = sb(st, [128, 16, 64], F32, "ropeS")
            ln1g = sb(st, [128, D], F32, "ln1g")
            ln1b = sb(st, [128, D], F32, "ln1b")
            expsink = sb(st, [128, 8], F32, "expsink")
            brB = sb(st, [128, NE], F32, "brB")
            carryD = sb(st, [128, NE], F32, "carryD")
            carryI = sb(st, [128, NE], I32, "carryI")
            tri4 = sb(st, [128, 512], BF16, "tri4")
            atri4 = sb(st, [128, 512], BF16, "atri4")
            Lst = sb(st, [128, 128], BF16, "Lst")
            onesm = sb(st, [128, 128], BF16, "onesm")
            elig = sb(st, [128, 4, 64], F32, "elig")
            stt = ExitStack()
            trif = sb(stt, [128, 512], F32, "trif")

            w_in_v = w_in_d.rearrange("(k p) c -> p k c", p=128)
            w_out_v = w_out_d.rearrange("(k p) c -> p k c", p=128)
            for k in range(8):
                for h2 in range(2):
                    S.dma("pool", lambda e: e.dma_start(out=w_in.t[:, k, h2 * 1152:(h2 + 1) * 1152], in_=w_in_v[:, k, h2 * 1152:(h2 + 1) * 1152]), "w_in", writes=[w_in.r])
                S.dma("pool", lambda e: e.dma_start(out=w_out.t[:, k, :], in_=w_out_v[:, k, :]), "w_out", writes=[w_out.r])
            S.dma("sp", lambda e: e.dma_start(out=w_r.t[:], in_=w_r_d), "w_r", writes=[w_r.r])
            S.dma("sp", lambda e: e.dma_start(out=ropeC.t[:], in_=ropeC_d), "ropeC", writes=[ropeC.r])
            S.dma("sp", lambda e: e.dma_start(out=ropeS.t[:], in_=ropeS_d), "ropeS", writes=[ropeS.r])
            S.dma("sp", lambda e: e.dma_start(out=ln1g.t[:], in_=ln1g_d[0].partition_broadcast(128)), "ln1g", writes=[ln1g.r])
            S.dma("sp", lambda e: e.dma_start(out=ln1b.t[:], in_=ln1b_d[0].partition_broadcast(128)), "ln1b", writes=[ln1b.r])
            S.dma("sp", lambda e: e.dma_start(out=expsink.t[:], in_=sinks_d[0].partition_broadcast(128)), "sinks", writes=[expsink.r])
            S.dma("sp", lambda e: e.dma_start(out=brB.t[:], in_=b_r_d[0].partition_broadcast(128)), "brB", writes=[brB.r])
            S.op("act", lambda e: e.activation(out=expsink.t[:], in_=expsink.t[:], func=AF.Exp), reads=[expsink.r], writes=[expsink.r])
            make_hilo(stt, bin2, b_in_d[0], 2304, "bin")
            make_hilo(stt, bout2, b_out_d[0], D, "bout")
            S.op("pool", lambda e: e.iota(carryI.t[:], pattern=[[C, NE]], base=0, channel_multiplier=0), writes=[carryI.r])
            S.op("dve", lambda e: e.tensor_copy(out=carryD.t[:], in_=carryI.t[:]), reads=[carryI.r], writes=[carryD.r])
            S.op("pool", lambda e: e.memset(trif.t[:], 0.0), writes=[trif.r])
            S.op("pool", lambda e: e.affine_select(out=trif.t[:], in_=trif.t[:], pattern=[[0, 4], [1, 128]], compare_op=ALU.is_ge, fill=NEG, base=0, channel_multiplier=-1), reads=[trif.r], writes=[trif.r])
            S.op("dve", lambda e: e.tensor_copy(out=tri4.t[:], in_=trif.t[:]), reads=[trif.r], writes=[tri4.r])
            S.op("pool", lambda e: e.memset(trif.t[:], 0.0), reads=[trif.r], writes=[trif.r])
            S.op("pool", lambda e: e.affine_select(out=trif.t[:], in_=trif.t[:], pattern=[[0, 4], [-1, 128]], compare_op=ALU.is_gt, fill=NEG, base=0, channel_multiplier=1), reads=[trif.r], writes=[trif.r])
            S.op("dve", lambda e: e.tensor_copy(out=atri4.t[:], in_=trif.t[:]), reads=[trif.r], writes=[atri4.r])
            S.op("pool", lambda e: e.memset(trif.t[:, 0:128], 1.0), reads=[trif.r], writes=[trif.r])
            S.op("pool", lambda e: e.affine_select(out=trif.t[:, 0:128], in_=trif.t[:, 0:128], pattern=[[1, 128]], compare_op=ALU.is_gt, fill=0.0, base=0, channel_multiplier=-1), reads=[trif.r], writes=[trif.r])
            S.op("dve", lambda e: e.tensor_copy(out=Lst.t[:], in_=trif.t[:, 0:128]), reads=[trif.r], writes=[Lst.r])
            S.op("dve", lambda e: e.memset(onesm.t[:], 1.0), writes=[onesm.r])
            S.op("dve", lambda e: e.memset(elig.t[:], 0.0), writes=[elig.r])
            for j in range(4, 8):
                S.op("dve", lambda e: e.memset(elig.t[:, j - 4, :].rearrange("p (h b) -> p h b", h=8)[:, :, j:8], -1e30), reads=[elig.r], writes=[elig.r])

            S.barrier()
            stt.close()
            kT_a = sb(st, [128, SEQ], BF16, "kT_a")
            kT_b = sb(st, [128, 4, SEQ], BF16, "kT_b")
            Va = sb(st, [128, 16, 2, 65], BF16, "Va")
            Vb = sb(st, [128, 16, 8, 65], BF16, "Vb")
            r_kv = [Res("kv%d" % t) for t in range(16)]
            S.op("pool", lambda e: e.memset(Va.t[:], 1.0), writes=r_kv)
            S.op("pool", lambda e: e.memset(Vb.t[:], 1.0), writes=r_kv)
            kms = sb(st, [128, 4, 8], F32, "kms")
            kmT = sb(st, [128, 4, 8], BF16, "kmT")
            S.op("dve", lambda e: e.memset(kms.t[:], 0.0), writes=[kms.r])
            S.op("dve", lambda e: e.memset(kmT.t[:], 0.0), writes=[kmT.r])
            xbs = [sb(st, [128, D], BF16, "xb") for _ in range(2)]
            xTs = [sb(st, [128, D], BF16, "xT") for _ in range(2)]
            m1s = [sb(st, [128, 512], F32, "m1") for _ in range(2)]
            m2s = [sb(st, [128, 512], F32, "m2") for _ in range(2)]
            rqa = [sb(st, [128, 512], BF16, "rqa") for _ in range(2)]
            rqb = [sb(st, [128, 512], BF16, "rqb") for _ in range(2)]
            rkb = [sb(st, [128, 512], BF16, "rkb") for _ in range(2)]
            rka = [sb(st, [128, 128], BF16, "rka") for _ in range(2)]
            qTa = [sb(st, [128, 4, 512], BF16, "qTa") for _ in range(1)]
            qTb = [sb(st, [128, 4, 512], BF16, "qTb") for _ in range(1)]
            sel = [sb(st, [128, 4, 64], F32, "sel") for _ in range(2)]
            gm = sb(st, [128, 64], F32, "gm")
            top = sb(st, [128, 8, 8], F32, "top")
            pts = [sb(st, [128, 512], BF16, "pt") for _ in range(6)]
            ptc = [0]
            den = sb(st, [128, 4], F32, "den")
            rden = sb(st, [128, 4], F32, "rden")
            accs = [sb(st, [128, 4, 65], F32, "acc") for _ in range(2)]
            o_t = [sb(st, [128, 4, D], BF16, "o_t") for _ in range(1)]
            oT = sb(st, [128, D], BF16, "oT")
            xres = [sb(st, [128, D], F32, "xres") for _ in range(1)]
            ylns = [sb(st, [128, D], F32, "yln") for _ in range(2)]
            lnb = (sb(st, [128, 2, 6], F32, "stats"), sb(st, [128, 2], F32, "mv"), sb(st, [128, 1], F32, "lnv"),
                   sb(st, [128, 1], F32, "rstd"), sb(st, [128, 1], F32, "nmr"), sb(st, [128, D], F32, "xn"))
            x1s = [sb(st, [128, D], F32, "x1") for _ in range(2)]
            x1bs = [sb(st, [128, D], BF16, "x1b") for _ in range(2)]
            x1T = sb(st, [128, D], F32, "x1T")
            lg = sb(st, [128, NE], F32, "lg")
            top8 = sb(st, [128, 8], F32, "top8")
            ntop = sb(st, [128, 1], F32, "ntop")
            gex = sb(st, [128, 4], F32, "gex")
            gsum = sb(st, [128, 1], F32, "gsum")
            Mb = sb(st, [128, NE], BF16, "Mb")
            posD = sb(st, [128, NE], F32, "posD")
            junk = sb(st, [128, NE], F32, "junk")
            destf = sb(st, [128, 4], F32, "destf")

            def npt():
                b = pts[ptc[0] % 6]
                ptc[0] += 1
                return b

            def rope(src_bank, H, t, m1, m2, out_view_fn):
                n = H * 64
                src = src_bank.t[:, 0:n].rearrange("p (h c) -> p h c", h=H)
                Cb = ropeC.t[:, t, :].unsqueeze(1).broadcast_to([128, H, 64])
                Sb1 = ropeS.t[:, t, 0:32].unsqueeze(1).broadcast_to([128, H, 32])
                Sb2 = ropeS.t[:, t, 32:64].unsqueeze(1).broadcast_to([128, H, 32])
                m1v = m1.t[:, 0:n].rearrange("p (h c) -> p h c", h=H)
                m2v = m2.t[:, 0:n].rearrange("p (h c) -> p h c", h=H)
                S.op("dve", lambda e: e.tensor_tensor(out=m1v, in0=src, in1=Cb, op=ALU.mult), reads=[src_bank.r, ropeC.r], writes=[m1.r])
                S.op("dve", lambda e: e.tensor_tensor(out=m2v[:, :, 0:32], in0=src[:, :, 32:64], in1=Sb1, op=ALU.mult), reads=[src_bank.r, ropeS.r], writes=[m2.r])
                S.op("dve", lambda e: e.tensor_tensor(out=m2v[:, :, 32:64], in0=src[:, :, 0:32], in1=Sb2, op=ALU.mult), reads=[src_bank.r, ropeS.r], writes=[m2.r])
                return m1v, m2v

            def load_x(Tg):
                xb_ = xbs[Tg % 2]
                S.dma("pool", lambda e: e.dma_start(out=xb_.t[:], in_=x_d[Tg * 128:(Tg + 1) * 128, :]), "xb%d" % (Tg % 2), writes=[xb_.r])

            for s in range(NSEQ):
                for c in range(4):
                    cb = (s * 4 + c) % 2
                    qa_c, qb_c, sel_c, o_c = qTa[0], qTb[0], sel[cb], o_t[0]
                    for u in range(4):
                        t = 4 * c + u
                        T = s * 16 + t
                        pp = T % 2
                        xb, xT = xbs[pp], xTs[pp]
                        if T == 0:
                            load_x(0)
                        for k in range(8):
                            S.op("pe", lambda e: e.transpose(out=pb[5].t[:, k * 128:(k + 1) * 128], in_=xb.t[:, k * 128:(k + 1) * 128], identity=ident_b.t[:]), reads=[xb.r, ident_b.r], writes=[pb[5].r])
                        S.op("act", lambda e: e.activation(out=xT.t[:], in_=pb[5].t[:], func=AF.Copy), reads=[pb[5].r], writes=[xT.r])
                        if T + 1 < NT:
                            load_x(T + 1)

                        def proj(col0, n):
                            bank = nb()
                            for k in range(8):
                                S.op("pe", lambda e: e.matmul(bank.t[:, 0:n], lhsT=xT.t[:, k * 128:(k + 1) * 128], rhs=w_in.t[:, k, col0:col0 + n], start=(k == 0), stop=False), reads=[xT.r, w_in.r], writes=[bank.r])
                            S.op("pe", lambda e: e.matmul(bank.t[:, 0:n], lhsT=ones2.t[:, :], rhs=bin2.t[:, col0:col0 + n], start=False, stop=True), reads=[ones2.r, bin2.r], writes=[bank.r])
                            return bank

                        bank = proj(0, 512)
                        m1v, m2v = rope(bank, 8, t, m1s[0], m2s[0], None)
                        ov = rqa[pp].t[:].rearrange("p (i two c) -> p two i c", i=4, two=2)
                        S.op("pool", lambda e: e.tensor_tensor(out=ov, in0=m1s[0].t[:].rearrange("p (two i c) -> p two i c", two=2, i=4), in1=m2s[0].t[:].rearrange("p (two i c) -> p two i c", two=2, i=4), op=ALU.add), reads=[m1s[0].r, m2s[0].r], writes=[rqa[pp].r])
                        bank = proj(768, 512)
                        rope(bank, 8, t, m1s[1], m2s[1], None)
                        S.op("pool", lambda e: e.tensor_tensor(out=rqb[pp].t[:], in0=m1s[1].t[:], in1=m2s[1].t[:], op=ALU.add), reads=[m1s[1].r, m2s[1].r], writes=[rqb[pp].r])
                        bank = proj(1280, 512)
                        rope(bank, 8, t, m1s[0], m2s[0], None)
                        S.op("pool", lambda e: e.tensor_tensor(out=rkb[pp].t[:], in0=m1s[0].t[:], in1=m2s[0].t[:], op=ALU.add), reads=[m1s[0].r, m2s[0].r], writes=[rkb[pp].r])
                        for i in range(4):
                            S.op("pe", lambda e: e.transpose(out=pb[6].t[:, i * 128:(i + 1) * 128], in_=rqa[pp].t[:, i * 128:(i + 1) * 128], identity=ident_b.t[:]), reads=[rqa[pp].r, ident_b.r], writes=[pb[6].r])
                        for i in range(4):
                            S.op("pe", lambda e: e.transpose(out=pb[6].t[:, 512 + i * 128:512 + (i + 1) * 128], in_=rqb[pp].t[:, i * 128:(i + 1) * 128], identity=ident_b.t[:]), reads=[rqb[pp].r, ident_b.r], writes=[pb[6].r])
                        S.op("act", lambda e: e.activation(out=qa_c.t[:, u, :], in_=pb[6].t[:, 0:512], func=AF.Copy), reads=[pb[6].r], writes=[qa_c.r])
                        S.op("act", lambda e: e.activation(out=qb_c.t[:, :, u * 128:(u + 1) * 128], in_=pb[6].t[:, 512:1024].rearrange("p (i q) -> p i q", i=4), func=AF.Copy), reads=[pb[6].r], writes=[qb_c.r])
                        bank = proj(512, 256)
                        rope(bank, 2, t, m1s[1], m2s[1], None)
                        S.op("pool", lambda e: e.tensor_tensor(out=rka[pp].t[:], in0=m1s[1].t[:, 0:128], in1=m2s[1].t[:, 0:128], op=ALU.add), reads=[m1s[1].r, m2s[1].r], writes=[rka[pp].r])
                        S.op("act", lambda e: e.activation(out=Va.t[:, t, :, 0:64], in_=bank.t[:, 128:256].rearrange("p (h c) -> p h c", h=2), func=AF.Copy), reads=[bank.r], writes=[r_kv[t]])
                        bank = proj(1792, 512)
                        S.op("act", lambda e: e.activation(out=Vb.t[:, t, :, 0:64], in_=bank.t[:, 0:512].rearrange("p (h c) -> p h c", h=8), func=AF.Copy), reads=[bank.r], writes=[r_kv[t]])

                        for i in range(4):
                            S.op("pe", lambda e: e.transpose(out=pb[5].t[:, i * 128:(i + 1) * 128], in_=rkb[pp].t[:, i * 128:(i + 1) * 128], identity=ident_b.t[:]), reads=[rkb[pp].r, ident_b.r], writes=[pb[5].r])
                        S.op("pe", lambda e: e.transpose(out=pb[5].t[:, 512:640], in_=rka[pp].t[:, 0:128], identity=ident_b.t[:]), reads=[rka[pp].r, ident_b.r], writes=[pb[5].r])
                        S.op("act", lambda e: e.activation(out=kT_b.t[:, :, t * 128:(t + 1) * 128], in_=pb[5].t[:, 0:512].rearrange("p (i q) -> p i q", i=4), func=AF.Copy), reads=[pb[5].r], writes=[r_kv[t]])
                        S.op("act", lambda e: e.activation(out=kT_a.t[:, t * 128:(t + 1) * 128], in_=pb[5].t[:, 512:640], func=AF.Copy), reads=[pb[5].r], writes=[r_kv[t]])
                    kvc = [r_kv[4 * c + u] for u in range(4)]
                    S.op("dve", lambda e: e.tensor_reduce(out=kms.t[:, :, 2 * c:2 * c + 2], in_=kT_b.t[:, :, c * 512:(c + 1) * 512].rearrange("p i (b k) -> p i b k", b=2), axis=AX.X, op=ALU.add), reads=kvc, writes=[kms.r])
                    S.op("act", lambda e: e.activation(out=kmT.t[:, :, 2 * c:2 * c + 2], in_=kms.t[:, :, 2 * c:2 * c + 2], func=AF.Copy, scale=1.0 / 256.0), reads=[kms.r], writes=[kmT.r])
                    if c >= 2:
                        for u in range(4):
                            for h in range(8):
                                i, ph = h // 2, (h % 2) * 64
                                S.op("pe", lambda e: e.matmul(pb[7].t[:, (u * 8 + h) * 8:(u * 8 + h) * 8 + 8], lhsT=qb_c.t[ph:ph + 64, i, u * 128:(u + 1) * 128], rhs=kmT.t[ph:ph + 64, i, 0:8], start=True, stop=True), reads=[qb_c.r, kmT.r], writes=[pb[7].r], pe_order=True)
                        for u in range(4):
                            j = 2 * c + u // 2
                            S.op("dve", lambda e: e.tensor_tensor(out=gm.t[:], in0=pb[7].t[:, u * 64:(u + 1) * 64], in1=elig.t[:, j - 4, :], op=ALU.add), reads=[pb[7].r, elig.r], writes=[gm.r])
                            for h in range(8):
                                S.op("dve", lambda e: e.max(out=top.t[:, h, :], in_=gm.t[:, h * 8:(h + 1) * 8]), reads=[gm.r], writes=[top.r])
                            S.op("dve", lambda e: e.tensor_tensor(out=sel_c.t[:, u, :].rearrange("p (h b) -> p h b", h=8), in0=gm.t[:].rearrange("p (h b) -> p h b", h=8), in1=top.t[:, :, 2:3].broadcast_to([128, 8, 8]), op=ALU.is_ge), reads=[gm.r, top.r], writes=[sel_c.r])

                    def swa_s1(u, g):
                        qt = 4 * c + u
                        ph = g * 64
                        pt_prev = None
                        if qt >= 1:
                            bank = nbs()
                            S.op("pe", lambda e: e.matmul(bank.t[:, 0:512], lhsT=kT_a.t[ph:ph + 64, (qt - 1) * 128:qt * 128], rhs=qa_c.t[ph:ph + 64, u, :], start=True, stop=False), reads=[r_kv[qt - 1], qa_c.r], writes=[bank.r])
                            S.op("pe", lambda e: e.matmul(bank.t[:, 0:512], lhsT=ident_b.t[:], rhs=atri4.t[:], start=False, stop=True), reads=[ident_b.r, atri4.r], writes=[bank.r])
                            pt_prev = npt()
                            S.op("act", lambda e: e.activation(out=pt_prev.t[:], in_=bank.t[:, 0:512], func=AF.Exp, scale=0.125), reads=[bank.r], writes=[pt_prev.r])
                        bank = nbs()
                        S.op("pe", lambda e: e.matmul(bank.t[:, 0:512], lhsT=kT_a.t[ph:ph + 64, qt * 128:(qt + 1) * 128], rhs=qa_c.t[ph:ph + 64, u, :], start=True, stop=False), reads=[r_kv[qt], qa_c.r], writes=[bank.r])
                        S.op("pe", lambda e: e.matmul(bank.t[:, 0:512], lhsT=ident_b.t[:], rhs=tri4.t[:], start=False, stop=True), reads=[ident_b.r, tri4.r], writes=[bank.r])
                        pt_cur = npt()
                        S.op("act", lambda e: e.activation(out=pt_cur.t[:], in_=bank.t[:, 0:512], func=AF.Exp, scale=0.125), reads=[bank.r], writes=[pt_cur.r])
                        return pt_prev, pt_cur

                    def swa_s2(u, g, pt_prev, pt_cur):
                        qt = 4 * c + u
                        ob = nob()
                        for hh in range(4):
                            reg = ob.t[:, hh * 65:(hh + 1) * 65]
                            if pt_prev is not None:
                                S.op("pe", lambda e: e.matmul(reg, lhsT=pt_prev.t[:, hh * 128:(hh + 1) * 128], rhs=Va.t[:, qt - 1, g, :], start=True, stop=False), reads=[pt_prev.r, r_kv[qt - 1]], writes=[ob.r])
                            S.op("pe", lambda e: e.matmul(reg, lhsT=pt_cur.t[:, hh * 128:(hh + 1) * 128], rhs=Va.t[:, qt, g, :], start=(pt_prev is None), stop=True), reads=[pt_cur.r, r_kv[qt]], writes=[ob.r])
                        ov = ob.t[:, 0:260].rearrange("p (h c) -> p h c", h=4)
                        S.op("dve", lambda e: e.tensor_tensor(out=den.t[:].unsqueeze(2), in0=ov[:, :, 64:65], in1=expsink.t[:, 4 * g:4 * g + 4].unsqueeze(2), op=ALU.add), reads=[ob.r, expsink.r], writes=[den.r])
                        S.op("dve", lambda e: e.reciprocal(out=rden.t[:], in_=den.t[:]), reads=[den.r], writes=[rden.r])
                        S.op("dve", lambda e: e.tensor_tensor(out=o_c.t[:, u, g * 256:(g + 1) * 256].rearrange("p (h c) -> p h c", h=4), in0=ov[:, :, 0:64], in1=rden.t[:].unsqueeze(2).broadcast_to([128, 4, 64]), op=ALU.mult), reads=[ob.r, rden.r], writes=[o_c.r])

                    def moba_s1(h, blk):
                        i, ph = h // 2, (h % 2) * 64
                        ptk = {}
                        for kt in (2 * blk, 2 * blk + 1):
                            r = kt - 4 * c
                            q0 = max(r, 0) * 128
                            n = 512 - q0
                            bank = nbs()
                            S.op("pe", lambda e: e.matmul(bank.t[:, 0:n], lhsT=kT_b.t[ph:ph + 64, i, kt * 128:(kt + 1) * 128], rhs=qb_c.t[ph:ph + 64, i, q0:512], start=True, stop=(r < 0)), reads=[r_kv[kt], qb_c.r], writes=[bank.r])
                            if r >= 0:
                                S.op("pe", lambda e: e.matmul(bank.t[:, 0:128], lhsT=ident_b.t[:], rhs=tri4.t[:, 0:128], start=False, stop=True), reads=[ident_b.r, tri4.r], writes=[bank.r])
                            p = npt()
                            S.op("act", lambda e: e.activation(out=p.t[:, q0:512], in_=bank.t[:, 0:n], func=AF.Exp, scale=0.125), reads=[bank.r], writes=[p.r])
                            ptk[kt] = p
                        return ptk

                    def moba_s2(h, blk, ptk):
                        acc = accs[h % 2]
                        ob = nob()
                        us = []
                        for u in range(4):
                            kts = [kt for kt in (2 * blk, 2 * blk + 1) if kt <= 4 * c + u]
                            if not kts:
                                continue
                            us.append(u)
                            for ki, kt in enumerate(kts):
                                S.op("pe", lambda e: e.matmul(ob.t[:, u * 65:(u + 1) * 65], lhsT=ptk[kt].t[:, u * 128:(u + 1) * 128], rhs=Vb.t[:, kt, h, :], start=(ki == 0), stop=(ki == len(kts) - 1)), reads=[ptk[kt].r, r_kv[kt]], writes=[ob.r])
                        first = (blk == 0)
                        need_sel = [(blk != 2 * c + u // 2) and (2 * c + u // 2 >= 4) for u in us]
                        if not any(need_sel):
                            u0, u1 = us[0], us[-1] + 1
                            if first:
                                S.op("dve", lambda e: e.tensor_copy(out=acc.t[:, u0:u1, :], in_=ob.t[:, u0 * 65:u1 * 65].rearrange("p (u c) -> p u c", c=65)), reads=[ob.r], writes=[acc.r])
                            else:
                                S.op("dve", lambda e: e.tensor_tensor(out=acc.t[:, u0:u1, :], in0=ob.t[:, u0 * 65:u1 * 65].rearrange("p (u c) -> p u c", c=65), in1=acc.t[:, u0:u1, :], op=ALU.add), reads=[ob.r, acc.r], writes=[acc.r])
                        else:
                            for u, ns in zip(us, need_sel):
                                reg = ob.t[:, u * 65:(u + 1) * 65]
                                sc = sel_c.t[:, u, h * 8 + blk:h * 8 + blk + 1]
                                if ns and first:
                                    S.op("dve", lambda e: e.tensor_scalar(out=acc.t[:, u, :], in0=reg, scalar1=sc, scalar2=None, op0=ALU.mult), reads=[ob.r, sel_c.r], writes=[acc.r])
                                elif ns:
                                    S.op("dve", lambda e: e.scalar_tensor_tensor(out=acc.t[:, u, :], in0=reg, scalar=sc, in1=acc.t[:, u, :], op0=ALU.mult, op1=ALU.add), reads=[ob.r, sel_c.r, acc.r], writes=[acc.r])
                                elif first:
                                    S.op("dve", lambda e: e.tensor_copy(out=acc.t[:, u, :], in_=reg), reads=[ob.r], writes=[acc.r])
                                else:
                                    S.op("dve", lambda e: e.tensor_tensor(out=acc.t[:, u, :], in0=reg, in1=acc.t[:, u, :], op=ALU.add), reads=[ob.r, acc.r], writes=[acc.r])
                        if blk == 2 * c + 1:
                            S.op("dve", lambda e: e.reciprocal(out=rden.t[:].unsqueeze(2), in_=acc.t[:, :, 64:65]), reads=[acc.r], writes=[rden.r])
                            S.op("dve", lambda e: e.tensor_tensor(out=o_c.t[:, :, 512 + h * 64:512 + (h + 1) * 64], in0=acc.t[:, :, 0:64], in1=rden.t[:].unsqueeze(2).broadcast_to([128, 4, 64]), op=ALU.mult), reads=[acc.r, rden.r], writes=[o_c.r])

                    items = [("s", u, g) for u in range(4) for g in range(2)] + [("m", h, blk) for h in range(8) for blk in range(2 * c + 2)]

                    def s1(it):
                        return swa_s1(it[1], it[2]) if it[0] == "s" else moba_s1(it[1], it[2])

                    def s2(it, st1):
                        if it[0] == "s":
                            swa_s2(it[1], it[2], st1[0], st1[1])
                        else:
                            moba_s2(it[1], it[2], st1)

                    nxt = s1(items[0])
                    for ii, it in enumerate(items):
                        cur = nxt
                        if ii + 1 < len(items):
                            nxt = s1(items[ii + 1])
                        s2(it, cur)

                    def a5_s1(u):
                        T = s * 16 + 4 * c + u
                        xr, yl = xres[0], ylns[T % 2]
                        S.dma("sp", lambda e: e.dma_start(out=xr.t[:], in_=x_d[T * 128:(T + 1) * 128, :]), "xres0", writes=[xr.r])
                        for k in range(8):
                            S.op("pe", lambda e: e.transpose(out=pb[6].t[:, k * 128:(k + 1) * 128], in_=o_c.t[:, u, k * 128:(k + 1) * 128], identity=ident_b.t[:]), reads=[o_c.r, ident_b.r], writes=[pb[6].r])
                        S.op("act", lambda e: e.activation(out=oT.t[:], in_=pb[6].t[:], func=AF.Copy), reads=[pb[6].r], writes=[oT.r])
                        for hf in range(2):
                            bank = nb()
                            for k in range(8):
                                S.op("pe", lambda e: e.matmul(bank.t[:, 0:512], lhsT=oT.t[:, k * 128:(k + 1) * 128], rhs=w_out.t[:, k, hf * 512:(hf + 1) * 512], start=(k == 0), stop=False), reads=[oT.r, w_out.r], writes=[bank.r])
                            S.op("pe", lambda e: e.matmul(bank.t[:, 0:512], lhsT=ones2.t[:, :], rhs=bout2.t[:, hf * 512:(hf + 1) * 512], start=False, stop=True), reads=[ones2.r, bout2.r], writes=[bank.r])
                            S.op("dve", lambda e: e.scalar_tensor_tensor(out=yl.t[:, hf * 512:(hf + 1) * 512], in0=xr.t[:, hf * 512:(hf + 1) * 512], scalar=ALPHA, in1=bank.t[:, 0:512], op0=ALU.mult, op1=ALU.add), reads=[xr.r, bank.r], writes=[yl.r])

                    def a5_s2(u):
                        T = s * 16 + 4 * c + u
                        pp = T % 2
                        yl, x1, x1b = ylns[pp], x1s[pp], x1bs[pp]
                        layer_norm(lnb, yl, ln1g, ln1b, x1)
                        S.dma("sp", lambda e: e.dma_start(out=x1buf[T * 128:(T + 1) * 128, :], in_=x1.t[:]), "x1st%d" % pp, reads=[x1.r], writes=[r_x1buf])
                        S.op("act", lambda e: e.activation(out=x1b.t[:], in_=x1.t[:], func=AF.Copy), reads=[x1.r], writes=[x1b.r])
                        for rr in range(2):
                            for k in range(4):
                                kk = rr * 4 + k
                                S.op("pe", lambda e: e.transpose(out=pb[7].t[:, k * 128:(k + 1) * 128], in_=x1.t[:, kk * 128:(kk + 1) * 128], identity=ident_f.t[:]), reads=[x1.r, ident_f.r], writes=[pb[7].r])
                            S.op("act", lambda e: e.activation(out=x1T.t[:, rr * 512:(rr + 1) * 512], in_=pb[7].t[:, 0:512], func=AF.Copy), reads=[pb[7].r], writes=[x1T.r])
                        for k in range(8):
                            S.op("pe", lambda e: e.matmul(pb[7].t[:, 0:NE], lhsT=x1T.t[:, k * 128:(k + 1) * 128], rhs=w_r.t[:, k, :], start=(k == 0), stop=(k == 7)), reads=[x1T.r, w_r.r], writes=[pb[7].r])
                        S.op("dve", lambda e: e.tensor_tensor(out=lg.t[:], in0=pb[7].t[:, 0:NE], in1=brB.t[:], op=ALU.add), reads=[pb[7].r, brB.r], writes=[lg.r])
                        S.op("dve", lambda e: e.max(out=top8.t[:], in_=lg.t[:]), reads=[lg.r], writes=[top8.r])
                        S.op("dve", lambda e: e.tensor_scalar(out=ntop.t[:], in0=top8.t[:, 0:1], scalar1=-1.0, scalar2=None, op0=ALU.mult), reads=[top8.r], writes=[ntop.r])
                        S.op("act", lambda e: e.activation(out=gex.t[:], in_=top8.t[:, 0:4], func=AF.Exp, bias=ntop.t[:, 0:1], scale=1.0, accum_out=gsum.t[:, 0:1]), reads=[top8.r, ntop.r], writes=[gex.r, gsum.r])
                        S.op("dve", lambda e: e.reciprocal(out=gsum.t[:], in_=gsum.t[:]), reads=[gsum.r], writes=[gsum.r])
                        S.op("dve", lambda e: e.tensor_scalar(out=gates.t[:, T, :], in0=gex.t[:], scalar1=gsum.t[:, 0:1], scalar2=None, op0=ALU.mult), reads=[gex.r, gsum.r], writes=[gates.r])
                        S.op("dve", lambda e: e.tensor_scalar(out=Mb.t[:], in0=lg.t[:], scalar1=top8.t[:, 3:4], scalar2=None, op0=ALU.is_ge), reads=[lg.r, top8.r], writes=[Mb.r])
                        S.op("pe", lambda e: e.matmul(pb[7].t[:, 64:64 + NE], lhsT=Lst.t[:], rhs=Mb.t[:], start=True, stop=True), reads=[Lst.r, Mb.r], writes=[pb[7].r])
                        S.op("pe", lambda e: e.matmul(pb[7].t[:, 128:128 + NE], lhsT=onesm.t[:], rhs=Mb.t[:], start=True, stop=True), reads=[onesm.r, Mb.r], writes=[pb[7].r])
                        S.op("dve", lambda e: e.tensor_tensor(out=posD.t[:], in0=pb[7].t[:, 64:64 + NE], in1=carryD.t[:], op=ALU.add), reads=[pb[7].r, carryD.r], writes=[posD.r])
                        S.op("dve", lambda e: e.tensor_tensor(out=carryD.t[:], in0=pb[7].t[:, 128:128 + NE], in1=carryD.t[:], op=ALU.add), reads=[pb[7].r, carryD.r], writes=[carryD.r])
                        for k in range(4):
                            S.op("dve", lambda e: e.scalar_tensor_tensor(out=junk.t[:], in0=lg.t[:], scalar=top8.t[:, k:k + 1], in1=posD.t[:], op0=ALU.is_equal, op1=ALU.mult, accum_out=destf.t[:, k:k + 1]), reads=[lg.r, top8.r, posD.r], writes=[junk.r, destf.r])
                        S.op("dve", lambda e: e.tensor_copy(out=dest.t[:, T, :], in_=destf.t[:]), reads=[destf.r], writes=[dest.r])
                        for k in range(4):
                            S.dma("pool", lambda e: e.indirect_dma_start(out=xbuf, out_offset=bass.IndirectOffsetOnAxis(ap=dest.t[:, T, k:k + 1], axis=0), in_=x1b.t[:], in_offset=None), "x1b%d" % pp, reads=[x1b.r, dest.r], writes=[r_xbuf])

                    a5_s1(0)
                    for u in range(4):
                        if u + 1 < 4:
                            a5_s1(u + 1)
                        a5_s2(u)
            S.barrier()

        if dbg == "A":
            S.finish("sp", [r_x1buf])
            S.barrier()
            return nc

        with ExitStack() as st:
            b1g = sb(st, [128, NE, 8], F32, "b1g")
            b1l = sb(st, [128, NE, 8], F32, "b1l")
            S.dma("sp", lambda e: e.dma_start(out=b1g.t[:], in_=b1g_d), "b1g", writes=[b1g.r])
            S.dma("sp", lambda e: e.dma_start(out=b1l.t[:], in_=b1l_d), "b1l", writes=[b1l.r])
            S.op("dve", lambda e: e.tensor_scalar(out=b1g.t[:], in0=b1g.t[:], scalar1=1.702, scalar2=None, op0=ALU.mult), reads=[b1g.r], writes=[b1g.r])
            S.op("dve", lambda e: e.tensor_scalar(out=b1l.t[:], in0=b1l.t[:], scalar1=7.0, scalar2=SINV, op0=ALU.add, op1=ALU.mult), reads=[b1l.r], writes=[b1l.r])
            wg = [sb(st, [128, 8, D], BF16, "wg") for _ in range(2)]
            wl = [sb(st, [128, 8, D], BF16, "wl") for _ in range(2)]
            w2 = [sb(st, [128, 8, D], BF16, "w2") for _ in range(2)]
            b2B = [sb(st, [128, D], F32, "b2B") for _ in range(2)]
            xeT = [sb(st, [128, 8, C], BF16, "xeT") for _ in range(2)]
            xss = [sb(st, [128, D], BF16, "xs") for _ in range(4)]
            actT = [sb(st, [128, 8, 512], BF16, "actT") for _ in range(2)]
            glu = [sb(st, [128, 512], F32, "glu") for _ in range(2)]
            sg = [sb(st, [128, 512], F32, "sg") for _ in range(2)]
            la = [sb(st, [128, 512], F32, "la") for _ in range(2)]
            ysb = [sb(st, [128, D], F32, "ysb") for _ in range(2)]
            xsc = [0]
            ysc = [0]
            hc = [0]
            chunks = []
            q0 = 0
            while q0 < C:
                n = min(512, C - q0)
                chunks.append((q0, n))
                q0 += n

            def load_w(e_):
                pp = e_ % 2
                for (wb, wd, nm) in ((wg[pp], w1g_d, "wg"), (wl[pp], w1l_d, "wl"), (w2[pp], w2_d, "w2")):
                    src = wd[e_].rearrange("(k p) f -> p k f", p=128)
                    for k in range(8):
                        S.dma("pool", lambda e: e.dma_start(out=wb.t[:, k, :], in_=src[:, k, :]), "%s%d" % (nm, pp), writes=[wb.r])
                S.dma("sp", lambda e: e.dma_start(out=b2B[pp].t[:], in_=b2_d[e_].partition_broadcast(128)), "b2B%d" % pp, writes=[b2B[pp].r])

            def prep_x(e_):
                pp = e_ % 2
                for s_ in range(NS):
                    xs = xss[xsc[0] % 4]
                    xsc[0] += 1
                    row0 = e_ * C + s_ * 128
                    S.dma("sp", lambda e: e.dma_start(out=xs.t[:], in_=xbuf[row0:row0 + 128, :]), "xs%d" % ((xsc[0] - 1) % 4), reads=[r_xbuf], writes=[xs.r])
                    for k in range(8):
                        S.op("pe", lambda e: e.transpose(out=pb[5].t[:, k * 128:(k + 1) * 128], in_=xs.t[:, k * 128:(k + 1) * 128], identity=ident_b.t[:]), reads=[xs.r, ident_b.r], writes=[pb[5].r])
                    S.op("act", lambda e: e.activation(out=xeT[pp].t[:, :, s_ * 128:(s_ + 1) * 128], in_=pb[5].t[:].rearrange("p (k q) -> p k q", k=8), func=AF.Copy), reads=[pb[5].r], writes=[xeT[pp].r])

            load_w(0)
            prep_x(0)
            for e_ in range(NE):
                pp = e_ % 2
                if e_ + 1 < NE:
                    load_w(e_ + 1)
                for ci, (q0, n) in enumerate(chunks):
                    aT = actT[ci % 2]
                    for j in range(8):
                        hb = hc[0] % 2
                        hc[0] += 1
                        bg, bl = pb[hb * 2], pb[hb * 2 + 1]
                        for k in range(8):
                            S.op("pe", lambda e: e.matmul(bg.t[:, 0:n], lhsT=wg[pp].t[:, k, j * 128:(j + 1) * 128], rhs=xeT[pp].t[:, k, q0:q0 + n], start=(k == 0), stop=(k == 7)), reads=[wg[pp].r, xeT[pp].r], writes=[bg.r])
                        for k in range(8):
                            S.op("pe", lambda e: e.matmul(bl.t[:, 0:n], lhsT=wl[pp].t[:, k, j * 128:(j + 1) * 128], rhs=xeT[pp].t[:, k, q0:q0 + n], start=(k == 0), stop=(k == 7)), reads=[wl[pp].r, xeT[pp].r], writes=[bl.r])
                        S.op("act", lambda e: e.activation(out=glu[hb].t[:, 0:n], in_=bg.t[:, 0:n], func=AF.Silu, scale=1.702, bias=b1g.t[:, e_, j:j + 1]), reads=[bg.r, b1g.r], writes=[glu[hb].r])
                        S.op("act", lambda e: e.activation(out=la[hb].t[:, 0:n], in_=bl.t[:, 0:n], func=AF.Relu, scale=SINV, bias=b1l.t[:, e_, j:j + 1]), reads=[bl.r, b1l.r], writes=[la[hb].r])
                        S.op("dve", lambda e: e.tensor_scalar(out=sg[hb].t[:, 0:n], in0=la[hb].t[:, 0:n], scalar1=14.0 * SINV, scalar2=-6.0 * SINV, op0=ALU.min, op1=ALU.add), reads=[la[hb].r], writes=[sg[hb].r])
                        S.op("dve", lambda e: e.scalar_tensor_tensor(out=aT.t[:, j, 0:n], in0=glu[hb].t[:, 0:n], scalar=UMAX, in1=sg[hb].t[:, 0:n], op0=ALU.min, op1=ALU.mult), reads=[glu[hb].r, sg[hb].r], writes=[aT.r])
                    for sl in range(n // 128):
                        yb = ysb[ysc[0] % 2]
                        yi = ysc[0] % 2
                        ysc[0] += 1
                        for hf in range(2):
                            bank = pb[4] if hf == 0 else pb[7]
                            for j in range(8):
                                S.op("pe", lambda e: e.matmul(bank.t[:, 0:512], lhsT=aT.t[:, j, sl * 128:(sl + 1) * 128], rhs=w2[pp].t[:, j, hf * 512:(hf + 1) * 512], start=(j == 0), stop=(j == 7)), reads=[aT.r, w2[pp].r], writes=[bank.r])
                            S.op("dve", lambda e: e.tensor_tensor(out=yb.t[:, hf * 512:(hf + 1) * 512], in0=bank.t[:, 0:512], in1=b2B[pp].t[:, hf * 512:(hf + 1) * 512], op=ALU.add), reads=[bank.r, b2B[pp].r], writes=[yb.r])
                        row0 = e_ * C + q0 + sl * 128
                        S.dma("sp", lambda e: e.dma_start(out=ybuf[row0:row0 + 128, :], in_=yb.t[:]), "ysb%d" % yi, reads=[yb.r], writes=[r_ybuf])
                    if e_ + 1 < NE and ci == min(1, len(chunks) - 1):
                        prep_x(e_ + 1)
            S.barrier()

        with ExitStack() as st:
            ln2g = sb(st, [128, D], F32, "ln2g")
            ln2b = sb(st, [128, D], F32, "ln2b")
            S.dma("sp", lambda e: e.dma_start(out=ln2g.t[:], in_=ln2g_d[0].partition_broadcast(128)), "ln2g", writes=[ln2g.r])
            S.dma("sp", lambda e: e.dma_start(out=ln2b.t[:], in_=ln2b_d[0].partition_broadcast(128)), "ln2b", writes=[ln2b.r])
            ygs = [[sb(st, [128, D], F32, "yg") for _ in range(4)] for _ in range(3)]
            x1r = [sb(st, [128, D], F32, "x1r") for _ in range(3)]
            accC = [sb(st, [128, D], F32, "accC") for _ in range(3)]
            outb = [sb(st, [128, D], F32, "outb") for _ in range(3)]
            lnb2 = (sb(st, [128, 2, 6], F32, "stats2"), sb(st, [128, 2], F32, "mv2"), sb(st, [128, 1], F32, "lnv2"),
                    sb(st, [128, 1], F32, "rstd2"), sb(st, [128, 1], F32, "nmr2"), sb(st, [128, D], F32, "xn2"))
            def c_load(T):
                pp = T % 3
                S.dma("sp", lambda e: e.dma_start(out=x1r[pp].t[:], in_=x1buf[T * 128:(T + 1) * 128, :]), "x1r%d" % pp, reads=[r_x1buf], writes=[x1r[pp].r])
                for k in range(4):
                    yg = ygs[pp][k]
                    S.dma("pool", lambda e: e.indirect_dma_start(out=yg.t[:], out_offset=None, in_=ybuf, in_offset=bass.IndirectOffsetOnAxis(ap=dest.t[:, T, k:k + 1], axis=0)), "yg%d_%d" % (pp, k), reads=[r_ybuf, dest.r], writes=[yg.r])

            def c_comp(T):
                pp = T % 3
                a = accC[pp]
                S.op("act", lambda e: e.activation(out=a.t[:], in_=x1r[pp].t[:], func=AF.Copy, scale=ALPHA), reads=[x1r[pp].r], writes=[a.r])
                for k in range(4):
                    yg = ygs[pp][k]
                    S.op("dve", lambda e: e.scalar_tensor_tensor(out=a.t[:], in0=yg.t[:], scalar=gates.t[:, T, k:k + 1], in1=a.t[:], op0=ALU.mult, op1=ALU.add), reads=[yg.r, gates.r, a.r], writes=[a.r])
                layer_norm(lnb2, a, ln2g, ln2b, outb[pp])
                S.dma("sp", lambda e: e.dma_start(out=out_d[T * 128:(T + 1) * 128, :], in_=outb[pp].t[:]), "outb%d" % pp, reads=[outb[pp].r], writes=[r_out])

            for T in range(min(3, NT)):
                c_load(T)
            for T in range(NT):
                c_comp(T)
                if T + 3 < NT:
                    c_load(T + 3)
            S.finish("sp", [r_out])
            S.barrier()
    return nc


def rope_tables():
    inv = 1.0 / (10000.0 ** (np.arange(0, 64, 2, dtype=np.float32) / 64.0))
    ang = np.arange(SEQ, dtype=np.float32)[:, None] * inv[None, :].astype(np.float32)
    cos = np.cos(ang).astype(np.float32)
    sin = np.sin(ang).astype(np.float32)
    return np.concatenate([cos, cos], 1), np.concatenate([-sin, sin], 1)


def prep_weights(w_in, b_in, sinks, w_out, b_out, ln1_g, ln1_b, w_router, b_router, w1, b1, w2, b2, ln2_g, ln2_b):
    f = lambda a: np.ascontiguousarray(np.asarray(a, dtype=np.float32))
    rc, rs = rope_tables()
    w1 = np.asarray(w1)[0]
    b1 = np.asarray(b1)[0]
    m = {
        "w_in": f(w_in[0]), "b_in": f(b_in[0]).reshape(1, -1), "sinks": f(sinks[0]).reshape(1, -1),
        "w_out": f(w_out[0]), "b_out": f(b_out[0]).reshape(1, -1),
        "ln1_g": f(ln1_g[0]).reshape(1, -1), "ln1_b": f(ln1_b[0]).reshape(1, -1),
        "w_router": f(np.asarray(w_router[0]).reshape(8, 128, NE).transpose(1, 0, 2)), "b_router": f(b_router[0]).reshape(1, -1),
        "w1g": f(w1[:, :, 0::2]), "w1l": f(w1[:, :, 1::2]),
        "b1g": f(b1[:, 0::2].reshape(NE, 8, 128).transpose(2, 0, 1)),
        "b1l": f(b1[:, 1::2].reshape(NE, 8, 128).transpose(2, 0, 1)),
        "w2": f(w2[0]), "b2": f(b2[0]),
        "ln2_g": f(ln2_g[0]).reshape(1, -1), "ln2_b": f(ln2_b[0]).reshape(1, -1),
        "ropeC": f(rc.reshape(16, 128, 64).transpose(1, 0, 2)), "ropeS": f(rs.reshape(16, 128, 64).transpose(1, 0, 2)),
    }
    return m


def kernel(x, w_in, b_in, sinks, w_out, b_out, ln1_g, ln1_b, w_router, b_router, w1, b1, w2, b2, ln2_g, ln2_b):
    x = np.asarray(x, dtype=np.float32)
    B = x.shape[0]
    nseq = B // N_CORES
    wm = prep_weights(w_in, b_in, sinks, w_out, b_out, ln1_g, ln1_b, w_router, b_router, w1, b1, w2, b2, ln2_g, ln2_b)
    nc = build(nseq, 1536)
    in_maps = []
    for c in range(N_CORES):
        m = dict(wm)
        m["x"] = np.ascontiguousarray(x[c * nseq:(c + 1) * nseq].reshape(nseq * SEQ, D))
        in_maps.append(m)
    res = run_bass_kernel_spmd(nc, in_maps, core_ids=list(range(N_CORES)))
    out = np.concatenate([r["out"].reshape(nseq, SEQ, D) for r in res.results], axis=0)
    return out.astype(np.float32)
```
